# Optimizing a Trainium2 kernel written in Bass

```python
import jax, jax.numpy as jnp
from jax import lax
import numpy as np

D_MODEL = 1024
BATCH = 8
SEQ = 2048
DEPTH = 4

CTX_LEN = 256
GRID_W = 64
HEAD_DIM = 64
ROPE_THETA = 10000.0
NORM_EPS = 1e-6
NEG_INF = -1e30
Q_BLOCK = 128

NA_HEADS = 4
NA_KH = 8
NA_KW = 16
GB_Q_HEADS = 4
GB_KV_HEADS = 2
WC_Q_HEADS = 4
WC_KV_HEADS = 2
WC_WINDOW = 128
MLA_HEADS = 4
MLA_Q_RANK = 256
MLA_KV_RANK = 128
MLA_NOPE = 64
MLA_ROPE = 32
MLA_V = 64

N_BRANCH = 4
BRANCH_W = 256
MIX_W = N_BRANCH * BRANCH_W

D_FF = 3584
N_EXPERTS = 8
TOP_K = 2
N_DENSE = (DEPTH + 1) // 2
N_MOE = DEPTH // 2
ADA_W = 6 * D_MODEL

IN_SIZES = (NA_HEADS * HEAD_DIM, NA_HEADS * HEAD_DIM, NA_HEADS * HEAD_DIM,
            GB_Q_HEADS * HEAD_DIM, GB_KV_HEADS * HEAD_DIM, GB_KV_HEADS * HEAD_DIM,
            WC_Q_HEADS * HEAD_DIM, WC_KV_HEADS * HEAD_DIM, WC_KV_HEADS * HEAD_DIM,
            MLA_Q_RANK, MLA_KV_RANK + MLA_ROPE,
            N_BRANCH * D_MODEL)
IN_SPLITS = tuple(sum(IN_SIZES[: i + 1]) for i in range(len(IN_SIZES) - 1))
IN_W = sum(IN_SIZES)

kernel_name = 'hybrid_gated_mixer_dit_block'


def rmsnorm(x, g):
    xf = x.astype(jnp.float32)
    y = xf * lax.rsqrt(jnp.mean(xf * xf, axis=-1, keepdims=True) + NORM_EPS)
    return (y * g.astype(jnp.float32)).astype(x.dtype)


def modulate(xn, shift, scale):
    return xn * (1 + scale) + shift


def _axis_angles(pos, dim):
    inv = ROPE_THETA ** (-jnp.arange(0, dim, 2, dtype=jnp.float32) / dim)
    return pos.astype(jnp.float32)[:, None] * inv[None, :]


def axial_rope(n_tokens, rot_dim):
    t = jnp.arange(n_tokens)
    half = rot_dim // 2
    ang = jnp.concatenate([_axis_angles(t // GRID_W, half), _axis_angles(t % GRID_W, half)], axis=-1)
    return jnp.cos(ang), jnp.sin(ang)


def apply_rope(x, cos, sin):
    d2 = x.shape[-1] // 2
    x1, x2 = x[..., :d2], x[..., d2:]
    c, s = cos.astype(x.dtype), sin.astype(x.dtype)
    return jnp.concatenate([x1 * c - x2 * s, x1 * s + x2 * c], axis=-1)


def group_heads(q, n_kv):
    B, T, H, d = q.shape
    return q.reshape(B, T, n_kv, H // n_kv, d)


def dense_blocked_attn(q, k, v, scale):
    B, T, Hkv, G, dq = q.shape
    nb = T // Q_BLOCK
    qb = jnp.moveaxis(q.reshape(B, nb, Q_BLOCK, Hkv, G, dq), 1, 0)

    def one_block(qblk):
        s = jnp.einsum('bqhgd,bkhd->bhgqk', qblk, k, preferred_element_type=jnp.float32) * scale
        p = jax.nn.softmax(s, axis=-1).astype(v.dtype)
        return jnp.einsum('bhgqk,bkhd->bqhgd', p, v)

    out = lax.map(one_block, qb)
    return jnp.moveaxis(out, 0, 1).reshape(B, T, -1)


def sink_attn(q, k, v, sink, scale):
    B, T, Hkv, G, d = q.shape
    s = jnp.einsum('bqhgd,bkhd->bhgqk', q, k, preferred_element_type=jnp.float32) * scale
    s_sink = jnp.broadcast_to(sink.astype(jnp.float32).reshape(Hkv, G)[:, :, None, None], s.shape[:-1] + (1,))
    p = jax.nn.softmax(jnp.concatenate([s, s_sink], axis=-1), axis=-1)[..., :-1].astype(v.dtype)
    return jnp.einsum('bhgqk,bkhd->bqhgd', p, v).reshape(B, T, -1)


def window_sink_attn(q, k, v, kc, vc, sink, scale):
    B, S, Hkv, G, d = q.shape
    nb = S // Q_BLOCK
    nk = 3 * Q_BLOCK
    pad = ((0, 0), (Q_BLOCK, Q_BLOCK), (0, 0), (0, 0))
    kp = jnp.pad(k, pad).reshape(B, nb + 2, Q_BLOCK, Hkv, d)
    vp = jnp.pad(v, pad).reshape(B, nb + 2, Q_BLOCK, Hkv, d)
    kband = jnp.concatenate([kp[:, :-2], kp[:, 1:-1], kp[:, 2:]], axis=2)
    vband = jnp.concatenate([vp[:, :-2], vp[:, 1:-1], vp[:, 2:]], axis=2)
    qb = q.reshape(B, nb, Q_BLOCK, Hkv, G, d)
    qpos = jnp.arange(nb)[:, None] * Q_BLOCK + jnp.arange(Q_BLOCK)[None, :]
    kpos = jnp.arange(nb)[:, None] * Q_BLOCK - Q_BLOCK + jnp.arange(nk)[None, :]
    ok = ((jnp.abs(qpos[:, :, None] - kpos[:, None, :]) <= WC_WINDOW)
          & (kpos >= 0)[:, None, :] & (kpos < S)[:, None, :])
    s_loc = jnp.einsum('bnqhgd,bnkhd->bnhgqk', qb, kband, preferred_element_type=jnp.float32) * scale
    s_loc = jnp.where(ok[None, :, None, None], s_loc, NEG_INF)
    s_ctx = jnp.einsum('bnqhgd,bchd->bnhgqc', qb, kc, preferred_element_type=jnp.float32) * scale
    s_sink = jnp.broadcast_to(sink.astype(jnp.float32).reshape(Hkv, G)[:, :, None, None], s_loc.shape[:-1] + (1,))
    p = jax.nn.softmax(jnp.concatenate([s_loc, s_ctx, s_sink], axis=-1), axis=-1).astype(v.dtype)
    o = (jnp.einsum('bnhgqk,bnkhd->bnqhgd', p[..., :nk], vband)
         + jnp.einsum('bnhgqc,bchd->bnqhgd', p[..., nk:nk + kc.shape[1]], vc))
    return o.reshape(B, S, -1)


def natten_attn(q, k, v, kc, vc, rpb, scale):
    B, S, H, d = q.shape
    rows = S // GRID_W
    kh = min(NA_KH, rows)
    r = jnp.arange(rows)
    krow = jnp.clip(r - kh // 2, 0, rows - kh)[:, None] + jnp.arange(kh)[None, :]
    col = jnp.arange(GRID_W)
    cstart = jnp.clip(col - NA_KW // 2, 0, GRID_W - NA_KW)
    col_ok = (col[None, :] >= cstart[:, None]) & (col[None, :] < cstart[:, None] + NA_KW)
    dr = krow - r[:, None] + NA_KH - 1
    dc = jnp.clip(col[None, :] - col[:, None] + NA_KW - 1, 0, 2 * NA_KW - 2)
    bias = rpb.astype(jnp.float32)[:, dr[:, None, :, None], dc[None, :, None, :]]
    qg = q.reshape(B, rows, GRID_W, H, d)
    kwin = k.reshape(B, rows, GRID_W, H, d)[:, krow]
    vwin = v.reshape(B, rows, GRID_W, H, d)[:, krow]
    s_loc = jnp.einsum('brqhd,brjkhd->bhrqjk', qg, kwin, preferred_element_type=jnp.float32) * scale + bias[None]
    s_loc = jnp.where(col_ok[:, None, :], s_loc, NEG_INF)
    n_loc = kh * GRID_W
    s_loc = s_loc.reshape(B, H, rows, GRID_W, n_loc)
    s_ctx = jnp.einsum('brqhd,bchd->bhrqc', qg, kc, preferred_element_type=jnp.float32) * scale
    p = jax.nn.softmax(jnp.concatenate([s_loc, s_ctx], axis=-1), axis=-1).astype(v.dtype)
    p_loc = p[..., :n_loc].reshape(B, H, rows, GRID_W, kh, GRID_W)
    o = (jnp.einsum('bhrqjk,brjkhd->brqhd', p_loc, vwin)
         + jnp.einsum('bhrqc,bchd->brqhd', p[..., n_loc:], vc))
    return o.reshape(B, S, H * d)


def mixer_features(h, w_in_l, gb_qn, gb_kn, mla_qn, mla_kvn, mla_wqb_l, mla_wkvb_l, rope):
    B, T, _ = h.shape
    p = jnp.einsum('btd,de->bte', h, w_in_l)
    aq, ak, av, bq, bk, bv, cq, ck, cv, dqa, dkva, gate = jnp.split(p, IN_SPLITS, axis=-1)
    aq, ak, av = (t.reshape(B, T, NA_HEADS, HEAD_DIM) for t in (aq, ak, av))
    bq = rmsnorm(bq.reshape(B, T, GB_Q_HEADS, HEAD_DIM), gb_qn)
    bk = rmsnorm(bk.reshape(B, T, GB_KV_HEADS, HEAD_DIM), gb_kn)
    bv = bv.reshape(B, T, GB_KV_HEADS, HEAD_DIM)
    cq = cq.reshape(B, T, WC_Q_HEADS, HEAD_DIM)
    ck = ck.reshape(B, T, WC_KV_HEADS, HEAD_DIM)
    cv = cv.reshape(B, T, WC_KV_HEADS, HEAD_DIM)
    dq = jnp.einsum('btr,re->bte', rmsnorm(dqa, mla_qn), mla_wqb_l).reshape(B, T, MLA_HEADS, MLA_NOPE + MLA_ROPE)
    dkv_c, dk_pe = jnp.split(dkva, [MLA_KV_RANK], axis=-1)
    dkv = jnp.einsum('btr,re->bte', rmsnorm(dkv_c, mla_kvn), mla_wkvb_l).reshape(B, T, MLA_HEADS, MLA_NOPE + MLA_V)
    dk_nope, dv = jnp.split(dkv, [MLA_NOPE], axis=-1)
    dq_nope, dq_pe = jnp.split(dq, [MLA_NOPE], axis=-1)
    if rope is not None:
        cos_h, sin_h, cos_r, sin_r = rope
        ch, sh = cos_h[:, None, :], sin_h[:, None, :]
        bq, bk = apply_rope(bq, ch, sh), apply_rope(bk, ch, sh)
        cq, ck = apply_rope(cq, ch, sh), apply_rope(ck, ch, sh)
        dq_pe = apply_rope(dq_pe, cos_r[:, None, :], sin_r[:, None, :])
        dk_pe = apply_rope(dk_pe, cos_r, sin_r)
    dq = jnp.concatenate([dq_nope, dq_pe], axis=-1)
    dk = jnp.concatenate([dk_nope, jnp.broadcast_to(dk_pe[:, :, None, :], (B, T, MLA_HEADS, MLA_ROPE))], axis=-1)
    return (aq, ak, av), (bq, bk, bv), (cq, ck, cv), (dq, dk, dv), gate


def merge_branches(outs, gate, w_branch_l, w_out_l):
    B, T = gate.shape[:2]
    o = jnp.stack([t.reshape(B, T, BRANCH_W) for t in outs], axis=2)
    y = jnp.einsum('btnw,nwd->btnd', o, w_branch_l)
    g = jax.nn.sigmoid(gate.reshape(B, T, N_BRANCH, D_MODEL))
    return jnp.einsum('btd,de->bte', jnp.sum(g * y, axis=2), w_out_l)


def token_mixers(h, hc, w_in_l, rpb_l, gb_qn, gb_kn, sink_l, mla_qn, mla_kvn, mla_wqb_l, mla_wkvb_l,
                 w_branch_l, w_out_l, rope, with_ctx_out):
    scale = HEAD_DIM ** -0.5
    mla_scale = (MLA_NOPE + MLA_ROPE) ** -0.5
    (aq, ak, av), (bq, bk, bv), (cq, ck, cv), (dq, dk, dv), gate = mixer_features(
        h, w_in_l, gb_qn, gb_kn, mla_qn, mla_kvn, mla_wqb_l, mla_wkvb_l, rope)
    (aqc, akc, avc), (bqc, bkc, bvc), (cqc, ckc, cvc), (dqc, dkc, dvc), gate_c = mixer_features(
        hc, w_in_l, gb_qn, gb_kn, mla_qn, mla_kvn, mla_wqb_l, mla_wkvb_l, None)
    cat = lambda a, b: jnp.concatenate([a, b], axis=1)
    oa = natten_attn(aq, ak, av, akc, avc, rpb_l, scale)
    ob = dense_blocked_attn(group_heads(bq, GB_KV_HEADS), cat(bk, bkc), cat(bv, bvc), scale)
    oc = window_sink_attn(group_heads(cq, WC_KV_HEADS), ck, cv, ckc, cvc, sink_l, scale)
    od = dense_blocked_attn(group_heads(dq, MLA_HEADS), cat(dk, dkc), cat(dv, dvc), mla_scale)
    mix = merge_branches((oa, ob, oc, od), gate, w_branch_l, w_out_l)
    if not with_ctx_out:
        return mix, None
    oac = dense_blocked_attn(group_heads(aqc, NA_HEADS), akc, avc, scale)
    obc = dense_blocked_attn(group_heads(bqc, GB_KV_HEADS), bkc, bvc, scale)
    occ = sink_attn(group_heads(cqc, WC_KV_HEADS), ckc, cvc, sink_l, scale)
    odc = dense_blocked_attn(group_heads(dqc, MLA_HEADS), dkc, dvc, mla_scale)
    mix_c = merge_branches((oac, obc, occ, odc), gate_c, w_branch_l, w_out_l)
    return mix, mix_c


def swiglu(h, w1, w3, w2):
    a = jnp.einsum('btd,df->btf', h, w1)
    b = jnp.einsum('btd,df->btf', h, w3)
    return jnp.einsum('btf,fd->btd', jax.nn.silu(a) * b, w2)


def moe_swiglu(h, router, w1, w3, w2):
    logits = jnp.einsum('btd,de->bte', h, router, preferred_element_type=jnp.float32)
    top_val, top_idx = lax.top_k(logits, TOP_K)
    top_w = jax.nn.softmax(top_val, axis=-1)
    combine = jnp.einsum('btk,btke->bte', top_w,
                         jax.nn.one_hot(top_idx, N_EXPERTS, dtype=jnp.float32)).astype(h.dtype)
    out = jnp.zeros_like(h)
    for e in range(N_EXPERTS):
        out = out + combine[..., e:e + 1] * swiglu(h, w1[e], w3[e], w2[e])
    return out


def setup_inputs(seed: int = 0) -> dict:
    key = jax.random.key(seed)
    ks = jax.random.split(key, 32)
    D = D_MODEL

    def nrm(k, shape, s):
        return jax.random.normal(k, shape, jnp.float32) * s

    return {
        'x': nrm(ks[0], (BATCH, SEQ, D), 1.0),
        'c': nrm(ks[1], (BATCH, D), 1.0),
        'ctx': nrm(ks[2], (BATCH, CTX_LEN, D), 1.0),
        'c_ctx': nrm(ks[3], (D,), 1.0),
        'norm1_g': 1.0 + nrm(ks[4], (DEPTH, D), 0.02),
        'norm2_g': 1.0 + nrm(ks[5], (DEPTH, D), 0.02),
        'w_ada': nrm(ks[6], (DEPTH, D, ADA_W), 0.5 * D ** -0.5),
        'b_ada': nrm(ks[7], (DEPTH, ADA_W), 0.02),
        'w_in': nrm(ks[8], (DEPTH, D, IN_W), D ** -0.5),
        'na_rpb': nrm(ks[9], (DEPTH, NA_HEADS, 2 * NA_KH - 1, 2 * NA_KW - 1), 0.1),
        'gb_qnorm': 1.0 + nrm(ks[10], (DEPTH, HEAD_DIM), 0.02),
        'gb_knorm': 1.0 + nrm(ks[11], (DEPTH, HEAD_DIM), 0.02),
        'wc_sink': nrm(ks[12], (DEPTH, WC_Q_HEADS), 0.5),
        'mla_qnorm': 1.0 + nrm(ks[13], (DEPTH, MLA_Q_RANK), 0.02),
        'mla_kvnorm': 1.0 + nrm(ks[14], (DEPTH, MLA_KV_RANK), 0.02),
        'mla_wqb': nrm(ks[15], (DEPTH, MLA_Q_RANK, MLA_HEADS * (MLA_NOPE + MLA_ROPE)), MLA_Q_RANK ** -0.5),
        'mla_wkvb': nrm(ks[16], (DEPTH, MLA_KV_RANK, MLA_HEADS * (MLA_NOPE + MLA_V)), MLA_KV_RANK ** -0.5),
        'w_branch': nrm(ks[17], (DEPTH, N_BRANCH, BRANCH_W, D), BRANCH_W ** -0.5),
        'w_out': nrm(ks[18], (DEPTH, D, D), D ** -0.5),
        'ffn_w1': nrm(ks[19], (N_DENSE, D, D_FF), D ** -0.5),
        'ffn_w3': nrm(ks[20], (N_DENSE, D, D_FF), D ** -0.5),
        'ffn_w2': nrm(ks[21], (N_DENSE, D_FF, D), D_FF ** -0.5),
        'moe_router': nrm(ks[22], (N_MOE, D, N_EXPERTS), D ** -0.5),
        'moe_w1': nrm(ks[23], (N_MOE, N_EXPERTS, D, D_FF), D ** -0.5),
        'moe_w3': nrm(ks[24], (N_MOE, N_EXPERTS, D, D_FF), D ** -0.5),
        'moe_w2': nrm(ks[25], (N_MOE, N_EXPERTS, D_FF, D), D_FF ** -0.5),
        'final_g': 1.0 + nrm(ks[26], (D,), 0.02),
    }


def reference(x, c, ctx, c_ctx, norm1_g, norm2_g, w_ada, b_ada, w_in, na_rpb, gb_qnorm, gb_knorm, wc_sink,
              mla_qnorm, mla_kvnorm, mla_wqb, mla_wkvb, w_branch, w_out, ffn_w1, ffn_w3, ffn_w2,
              moe_router, moe_w1, moe_w3, moe_w2, final_g):
    S = x.shape[1]
    C = ctx.shape[1]
    rope = (*axial_rope(S, HEAD_DIM), *axial_rope(S, MLA_ROPE))
    silu_c = jax.nn.silu(c)
    silu_cc = jax.nn.silu(c_ctx)
    xc = ctx
    for l in range(DEPTH):
        with_ctx = l < DEPTH - 1
        mod = jnp.einsum('bd,de->be', silu_c, w_ada[l]) + b_ada[l]
        mod_c = jnp.einsum('d,de->e', silu_cc, w_ada[l]) + b_ada[l]
        sh1, sc1, g1, sh2, sc2, g2 = jnp.split(mod[:, None, :], 6, axis=-1)
        sh1c, sc1c, g1c, sh2c, sc2c, g2c = jnp.split(mod_c, 6, axis=-1)
        h = modulate(rmsnorm(x, norm1_g[l]), sh1, sc1)
        hc = modulate(rmsnorm(xc, norm1_g[l]), sh1c, sc1c)
        mix, mix_c = token_mixers(h, hc, w_in[l], na_rpb[l], gb_qnorm[l], gb_knorm[l], wc_sink[l],
                                  mla_qnorm[l], mla_kvnorm[l], mla_wqb[l], mla_wkvb[l],
                                  w_branch[l], w_out[l], rope, with_ctx)
        x = x + g1 * mix
        h2 = modulate(rmsnorm(x, norm2_g[l]), sh2, sc2)
        if with_ctx:
            xc = xc + g1c * mix_c
            h2c = modulate(rmsnorm(xc, norm2_g[l]), sh2c, sc2c)
            h2 = jnp.concatenate([h2c, h2], axis=1)
        if l % 2 == 0:
            f = swiglu(h2, ffn_w1[l // 2], ffn_w3[l // 2], ffn_w2[l // 2])
        else:
            f = moe_swiglu(h2, moe_router[l // 2], moe_w1[l // 2], moe_w3[l // 2], moe_w2[l // 2])
        if with_ctx:
            xc = xc + g2c * f[:, :C]
            f = f[:, C:]
        x = x + g2 * f
    return rmsnorm(x, final_g)
```

```python
import numpy as np
from contextlib import ExitStack
import concourse.bass as bass
import concourse.mybir as mybir
from concourse.bass_utils import run_bass_kernel_spmd

F32 = mybir.dt.float32
BF16 = mybir.dt.bfloat16
AF = mybir.ActivationFunctionType
ALU = mybir.AluOpType
AX = mybir.AxisListType

D = 1024
S = 2048
C = 256
T = S + C
NT = T // 128
DEPTH = 4
IN_W = 6304
DFF = 3584
NEXP = 8
EPS = 1e-6
NEG = -30000.0
NDMA = 12
NSTG = 3

OFF = dict(aq=0, ak=256, av=512, bq=768, bk=1024, bv=1152, cq=1280, ck=1536, cv=1664,
           dqa=1792, dkva=2048, gate=2208)


class Prog:
    CE = ['pe', 'act', 'dve', 'pool']

    def __init__(self, nc, es):
        self.nc = nc
        self.streams = {e: [] for e in self.CE + ['sp']}
        self.sems = {}
        for e in self.CE:
            self.sems[e] = es.enter_context(nc.semaphore("s_" + e))
        for i in range(NDMA):
            self.sems['d%d' % i] = es.enter_context(nc.semaphore("s_d%d" % i))
        self.cnt = {k: 0 for k in self.sems}
        self.known = {e: {k: 0 for k in self.sems} for e in self.streams}
        self.lastw = {}
        self.readers = {}
        self.dma_rr = 0
        self.n = 0

    def _deps(self, eng, reads, writes):
        deps = {}

        def add(d):
            if d is not None:
                if deps.get(d[0], 0) < d[1]:
                    deps[d[0]] = d[1]
        for k in reads:
            add(self.lastw.get(k))
        for k in writes:
            add(self.lastw.get(k))
            for r in self.readers.get(k, ()):
                add(r)
        waits = []
        kn = self.known[eng]
        for s, i in deps.items():
            if kn[s] < i:
                kn[s] = i
                waits.append((s, i))
        return waits

    def _commit(self, tok, reads, writes):
        for k in writes:
            self.lastw[k] = tok
            self.readers[k] = []
        for k in reads:
            self.readers.setdefault(k, []).append(tok)

    def op(self, eng, fn, reads=(), writes=()):
        waits = self._deps(eng, reads, writes)
        self.cnt[eng] += 1
        tok = (eng, self.cnt[eng])
        self.known[eng][eng] = max(self.known[eng][eng], 0)
        self.streams[eng].append((waits, fn, eng))
        self._commit(tok, reads, writes)
        self.n += 1

    def dma(self, fn, reads=(), writes=()):
        s = 'd%d' % self.dma_rr
        self.dma_rr = (self.dma_rr + 1) % NDMA
        waits = self._deps('sp', reads, writes)
        if self.known['sp'][s] < self.cnt[s]:
            self.known['sp'][s] = self.cnt[s]
            waits.append((s, self.cnt[s]))
        self.cnt[s] += 1
        tok = (s, self.cnt[s])
        self.streams['sp'].append((waits, fn, s))
        self._commit(tok, reads, writes)
        self.n += 1

    def barrier(self):
        snap = dict(self.cnt)
        for e in self.streams:
            waits = []
            for s, i in snap.items():
                if self.known[e][s] < i:
                    self.known[e][s] = i
                    waits.append((s, i))
            if waits:
                self.streams[e].append((waits, None, None))

    def emit(self):
        nc = self.nc
        sems = self.sems

        def mult(s):
            return 16 if s[0] == 'd' else 1

        def run(engine, stream):
            for waits, fn, inc in stream:
                for s, i in waits:
                    engine.wait_ge(sems[s], i * mult(s))
                if fn is None:
                    continue
                ins = fn(engine)
                ins.then_inc(sems[inc], mult(inc))

        with nc.Block() as block:
            @block.tensor
            def _(e):
                run(e, self.streams['pe'])

            @block.scalar
            def _(e):
                run(e, self.streams['act'])

            @block.vector
            def _(e):
                run(e, self.streams['dve'])

            @block.gpsimd
            def _(e):
                run(e, self.streams['pool'])

            @block.sync
            def _(e):
                run(e, self.streams['sp'])


def _rope_tables():
    def ang(pos, dim):
        inv = (10000.0 ** (-np.arange(0, dim, 2, dtype=np.float32) / dim)).astype(np.float32)
        return pos.astype(np.float32)[:, None] * inv[None, :]
    t = np.arange(S)
    out = []
    for rot in (64, 32):
        half = rot // 2
        a = np.concatenate([ang(t // 64, half), ang(t % 64, half)], axis=-1).astype(np.float32)
        out += [np.cos(a).astype(np.float32), np.sin(a).astype(np.float32)]
    return out


def _natten_plan():
    rows = S // 64
    kh = 8
    pats = {}
    plan = []
    for i in range(16):
        lst = []
        for j in range(16):
            sig = []
            anyv = False
            for a in range(2):
                for b in range(2):
                    r = 2 * i + b
                    rk = 2 * j + a
                    st = min(max(r - kh // 2, 0), rows - kh)
                    if st <= rk < st + kh:
                        sig.append(rk - r + 7)
                        anyv = True
                    else:
                        sig.append(-1)
            if anyv:
                sig = tuple(sig)
                if sig not in pats:
                    pats[sig] = len(pats)
                lst.append((j, pats[sig]))
        plan.append(lst)
    return plan, pats


NAT_PLAN, NAT_PATS = _natten_plan()
NPAT = len(NAT_PATS)


def _natten_bias(rpb):
    col = np.arange(64)
    cstart = np.clip(col - 8, 0, 48)
    col_ok = (col[None, :] >= cstart[:, None]) & (col[None, :] < cstart[:, None] + 16)
    dc = np.clip(col[None, :] - col[:, None] + 15, 0, 30)
    out = np.full((DEPTH, 128, NPAT * 4, 128), NEG, dtype=np.float32)
    for sig, pid in NAT_PATS.items():
        for a in range(2):
            for b in range(2):
                dr = sig[a * 2 + b]
                if dr < 0:
                    continue
                g = rpb[:, :, dr, :][:, :, dc]
                g = np.where(col_ok[None, None], g, np.float32(NEG))
                g = np.transpose(g, (0, 3, 1, 2))
                out[:, a * 64:(a + 1) * 64, pid * 4:(pid + 1) * 4, b * 64:(b + 1) * 64] = g
    return out


def _maskC():
    kk = np.arange(128)[:, None]
    qq = np.arange(128)[None, :]
    m0 = np.where(qq <= kk, 0.0, NEG).astype(np.float32)
    m1 = np.where(kk <= qq, 0.0, NEG).astype(np.float32)
    return np.stack([m0, m1], axis=1)


def build(n_layers=DEPTH, dbg=None):
    nc = bass.Bass("TRN2", target_bir_lowering=False, dynamic_dma_scratch_size=256)
    es = ExitStack()
    with es:
        _build_body(nc, es, n_layers, dbg)
    return nc


def _build_body(nc, es, n_layers, dbg):
    def din(name, shape):
        return nc.dram_tensor(name, list(shape), F32, kind="ExternalInput").ap()

    xin = din("xin", [T, D])
    cvec = din("cvec", [2, D])
    norm1_g = din("norm1_g", [DEPTH, D])
    norm2_g = din("norm2_g", [DEPTH, D])
    w_ada = din("w_ada", [DEPTH, D, 6 * D])
    b_ada = din("b_ada", [DEPTH, 6 * D])
    w_in = din("w_in", [DEPTH, D, IN_W])
    biasA = din("biasA", [DEPTH, 128, NPAT * 4, 128])
    smallp = din("smallp", [DEPTH, 516])
    mla_wqb = din("mla_wqb", [DEPTH, 256, 384])
    mla_wkvb = din("mla_wkvb", [DEPTH, 128, 512])
    w_branch = din("w_branch", [DEPTH, 4, 256, D])
    w_out = din("w_out", [DEPTH, D, D])
    ffn_w1 = din("ffn_w1", [2, D, DFF])
    ffn_w3 = din("ffn_w3", [2, D, DFF])
    ffn_w2 = din("ffn_w2", [2, DFF, D])
    moe_router = din("moe_router", [2, D, NEXP])
    moe_w1 = din("moe_w1", [2, NEXP, D, DFF])
    moe_w3 = din("moe_w3", [2, NEXP, D, DFF])
    moe_w2 = din("moe_w2", [2, NEXP, DFF, D])
    final_g = din("final_g", [1, D])
    c_ident = din("c_ident", [128, 128])
    c_ropeH = din("c_ropeH", [2, S, 32])
    c_ropeR = din("c_ropeR", [2, S, 16])
    c_maskC = din("c_maskC", [128, 2, 128])
    out = nc.dram_tensor("out", [S, D], F32, kind="ExternalOutput").ap()
    xd = nc.dram_tensor("xd", [T, D], F32, kind="Internal").ap()
    md = nc.dram_tensor("md", [4, T, D], BF16, kind="Internal").ap()
    dbg_out = None
    if dbg is not None:
        dbg_out = nc.dram_tensor("dbg", [T, D], F32, kind="ExternalOutput").ap()

    P = Prog(nc, es)

    def sb(name, shape, dt):
        return es.enter_context(nc.sbuf_tensor(name, list(shape), dt))[:]

    ps = [es.enter_context(nc.psum_tensor("ps%d" % i, [128, 512], F32))[:] for i in range(8)]

    IDF = sb("idf", [128, 128], F32)
    IDB = sb("idb", [128, 128], BF16)
    ROPEH = sb("ropeh", [128, 2, 16, 32], F32)
    ROPER = sb("roper", [128, 2, 16, 16], F32)
    MASKC = sb("maskc", [128, 2, 128], BF16)
    CV = sb("cv", [128, 2, 8], F32)
    SL = sb("sl", [128, 2, 8, 128], F32)
    MOD = sb("mod", [128, 4, D], F32)
    SMP = sb("smp", [128, 516], F32)
    SKE = sb("ske", [128, 4], F32)
    HT = sb("ht", [128, 8, T], BF16)
    STG = sb("stg", [128, NSTG, 2048], F32)
    XT = sb("xt", [128, D], F32)
    HB = sb("hb", [128, D], F32)
    PB = HB
    STAT = sb("stat", [128, 32], F32)
    PBUF = sb("pbuf", [128, D], F32)
    TMPF = sb("tmpf", [128, 4, 256], F32)
    PB16 = sb("pb16", [128, 512], BF16)
    QT = sb("qt", [128, 4, 128], BF16)
    OB = sb("ob", [128, 256], BF16)
    OT = sb("ot", [128, 2, 128], BF16)
    DTT = sb("dtt", [128, 2, 128], BF16)
    MT8 = sb("mt8", [128, 8, 128], BF16)
    RT = sb("rt", [128, 8, NEXP], F32)
    RTB = sb("rtb", [128, 8, 16], BF16)
    COMB = sb("comb", [128, NT, NEXP], F32)
    RS = sb("rs", [128, 64], F32)
    UT = sb("ut", [128, 4, 512], BF16)
    SA = sb("sa", [128, 2, 512], BF16)
    R4 = sb("r4", [128, 12288], BF16)
    R2 = sb("r2", [128, 36864], BF16)

    KT = R2[:, 0:9216].rearrange("p (h t) -> p h t", h=4)
    VA = R2[:, 9216:13896].rearrange("p (t h d) -> p t h d", t=NT, h=4)
    BIAS = R2[:, 13896:13896 + NPAT * 4 * 128].rearrange("p (n q) -> p n q", q=128)
    XS = R2.bitcast(F32).rearrange("p (t d) -> p t d", t=NT)
    b0 = 13896 + NPAT * 4 * 128
    WG = R2[:, b0:b0 + 8192].rearrange("p (k c) -> p k c", k=8)
    WKV = WG
    WQ = R2[:, b0 + 8192:b0 + 10240].rearrange("p (k c) -> p k c", k=8)
    WBR = R2[:, b0 + 10240:b0 + 12288].rearrange("p (k c) -> p k c", k=2)
    WQB = R2[:, b0 + 12288:b0 + 13056].rearrange("p (k c) -> p k c", k=2)
    WKVB = R2[:, b0 + 13056:b0 + 13568]
    SIG = R2[:, b0 + 13568:b0 + 14592]
    MTL = R2[:, b0 + 14592:b0 + 15616]
    PT = R2[:, b0 + 15616:b0 + 16640].rearrange("p (a g q) -> p a g q", a=2, g=4)
    assert b0 + 16640 <= 36864
    WO = R4[:, 0:8192].rearrange("p (k c) -> p k c", k=8)
    M4 = MOD[:, 2:4, :].rearrange("p a d -> p (a d)").bitcast(BF16).rearrange("p (a d) -> p a d", a=4)
    H2F = PBUF.rearrange("p (k q) -> p k q", k=8)
    W1B = R4[:, 0:4096].rearrange("p (k c) -> p k c", k=8)
    W3B = R4[:, 4096:8192].rearrange("p (k c) -> p k c", k=8)
    W2B = R4[:, 8192:12288].rearrange("p (k c) -> p k c", k=4)

    def MM(o, lhsT, rhs, start, stop, r, w):
        P.op('pe', lambda e: e.matmul(o, lhsT, rhs, start=start, stop=stop), r, w)

    def TR(o, i, ident, r, w):
        P.op('pe', lambda e: e.transpose(o, i, ident), r, w)

    def ACT(o, i, func, r, w, **kw):
        P.op('act', lambda e: e.activation(o, i, func, **kw), r, w)

    def TT(eng, o, a, b, op, r, w):
        P.op(eng, lambda e: e.tensor_tensor(o, a, b, op), r, w)

    def TS(eng, o, a, s1, s2, op0, op1, r, w):
        if op1 is None:
            P.op(eng, lambda e: e.tensor_scalar(o, a, s1, None, op0), r, w)
        else:
            P.op(eng, lambda e: e.tensor_scalar(o, a, s1, s2, op0, op1), r, w)

    def STT(o, a, s, b, op0, op1, r, w):
        P.op('dve', lambda e: e.scalar_tensor_tensor(o, a, s, b, op0, op1), r, w)

    def CP(eng, o, i, r, w):
        if eng == 'act':
            P.op('act', lambda e: e.activation(o, i, AF.Copy), r, w)
        else:
            P.op(eng, lambda e: e.tensor_copy(o, i), r, w)

    def RED(o, i, op, r, w):
        P.op('dve', lambda e: e.tensor_reduce(o, i, AX.X, op), r, w)

    def RCP(o, i, r, w):
        P.op('dve', lambda e: e.reciprocal(o, i), r, w)

    def DMA(o, i, r, w):
        P.dma(lambda e: e.dma_start(out=o, in_=i), r, w)

    stg_rr = [0]

    def load_w(src, kc, ncols, dst, dkey, cast=True, use=None):
        pcw = 2048 // kc
        c0 = 0
        while c0 < ncols:
            cw = min(pcw, ncols - c0)
            s = stg_rr[0]
            stg_rr[0] = (s + 1) % NSTG
            sv = STG[:, s, 0:kc * cw].rearrange("p (k c) -> p k c", k=kc)
            DMA(sv, src[:, c0:c0 + cw].rearrange("(k p) c -> p k c", p=128), [], [('stg', s)])
            if cast:
                CP('pool', dst[:, :, c0:c0 + cw], sv, [('stg', s)], [dkey])
            else:
                use(sv, c0, cw, ('stg', s))
            c0 += cw

    import os
    KS = int(os.environ.get("KSETUP", "99"))
    if KS > 0:
        DMA(IDF, c_ident, [], ['idf'])
        CP('dve', IDB, IDF, ['idf'], ['idb'])
    if KS > 1:
        DMA(ROPEH[:, 0], c_ropeH[0].rearrange("(t p) d -> p t d", p=128), [], ['rope'])
        DMA(ROPEH[:, 1], c_ropeH[1].rearrange("(t p) d -> p t d", p=128), [], ['rope'])
        DMA(ROPER[:, 0], c_ropeR[0].rearrange("(t p) d -> p t d", p=128), [], ['rope'])
        DMA(ROPER[:, 1], c_ropeR[1].rearrange("(t p) d -> p t d", p=128), [], ['rope'])
    if KS > 2:
        DMA(PBUF[:, 0:256].rearrange("p (a q) -> p a q", a=2), c_maskC, [], ['pbuf'])
        CP('dve', MASKC, PBUF[:, 0:256].rearrange("p (a q) -> p a q", a=2), ['pbuf'], ['maskc'])
    if KS > 3:
        P.dma(lambda e: e.dma_start(out=CV, in_=cvec.rearrange("w (k p) -> p w k", p=128),
                                    allow_slow_non_contiguous=True), [], ['cv'])
    if KS > 4:
        ACT(CV, CV, AF.Silu, ['cv'], ['cv'])
    if KS > 5:
        for wch in range(2):
            CP('dve', SL[:, wch], CV[:, wch, :].rearrange("p (k o) -> p k o", o=1).to_broadcast([128, 8, 128]),
               ['cv'], ['sl'])

    def rope_view(tab, which, t, nh, half):
        return tab[:, which, t - 2, :].rearrange("p (o d) -> p o d", o=1).to_broadcast([128, nh, half])

    def rope(dst, src, tab, t, nh, half, skey, dkey):
        c = rope_view(tab, 0, t, nh, half)
        s = rope_view(tab, 1, t, nh, half)
        x1 = src[:, :, 0:half]
        x2 = src[:, :, half:2 * half]
        t1 = TMPF[:, 0, 0:nh * half].rearrange("p (h d) -> p h d", h=nh)
        t2 = TMPF[:, 1, 0:nh * half].rearrange("p (h d) -> p h d", h=nh)
        t3 = TMPF[:, 2, 0:nh * half].rearrange("p (h d) -> p h d", h=nh)
        t4 = TMPF[:, 3, 0:nh * half].rearrange("p (h d) -> p h d", h=nh)
        TT('dve', t1, x1, c, ALU.mult, [skey, 'rope'], ['tf0'])
        TT('pool', t2, x2, s, ALU.mult, [skey, 'rope'], ['tf1'])
        TT('dve', t3, x1, s, ALU.mult, [skey, 'rope'], ['tf2'])
        TT('pool', t4, x2, c, ALU.mult, [skey, 'rope'], ['tf3'])
        TT('dve', dst[:, :, 0:half], t1, t2, ALU.subtract, ['tf0', 'tf1'], [dkey])
        TT('dve', dst[:, :, half:2 * half], t3, t4, ALU.add, ['tf2', 'tf3'], [dkey])

    def head_rms(src, nh, hd, gain, skey):
        sq = TMPF[:, 0:1, :].rearrange("p a c -> p (a c)")[:, 0:nh * hd].rearrange("p (h d) -> p h d", h=nh)
        TT('dve', sq, src, src, ALU.mult, [skey], ['tf0'])
        RED(RS[:, 0:nh], sq, ALU.add, ['tf0'], ['rs'])
        ACT(RS[:, 8:8 + nh], RS[:, 0:nh], AF.Sqrt, ['rs'], ['rs'], bias=EPSB, scale=1.0 / hd)
        RCP(RS[:, 16:16 + nh], RS[:, 8:8 + nh], ['rs'], ['rs'])
        TT('dve', src, src, RS[:, 16:16 + nh].rearrange("p (h o) -> p h o", o=1).to_broadcast([128, nh, hd]),
           ALU.mult, [skey, 'rs'], [skey])
        TT('dve', src, src, gain.rearrange("p (o d) -> p o d", o=1).to_broadcast([128, nh, hd]),
           ALU.mult, [skey, 'smp'], [skey])

    EPSB = sb("epsb", [128, 1], F32)
    P.op('dve', lambda e: e.memset(EPSB, EPS), [], ['epsb'])

    def mod_tiles(l, col0, kind, slot, gain_src=None, which=(0, 1)):
        DMA(PB, b_ada[l:l + 1, col0:col0 + D].partition_broadcast(128), [], ['hb'])

        def use(sv, c0, cw, skey):
            for w in which:
                bank = ps[6 + w]
                for k in range(8):
                    MM(bank[:, 0:cw], SL[:, w, k, :], sv[:, k, :], k == 0, k == 7,
                       ['sl', skey], [('ps', 6 + w)])
                TT('dve', MOD[:, slot + w, c0:c0 + cw], bank[:, 0:cw], PB[:, c0:c0 + cw], ALU.add,
                   [('ps', 6 + w), 'hb'], [('mod', slot + w)])
        load_w(w_ada[l][:, col0:col0 + D], 8, D, None, None, cast=False, use=use)
        if kind == 'scale':
            DMA(PB, gain_src.partition_broadcast(128), [], ['hb'])
            for w in which:
                STT(MOD[:, slot + w], MOD[:, slot + w], 1.0, PB, ALU.add, ALU.mult,
                    [('mod', slot + w), 'hb'], [('mod', slot + w)])

    def norm_tile(t, src_ap, src_keys, gslot, sslot, router=False, xkey=None):
        w = 0 if t >= 2 else 1
        ACT(HB, src_ap, AF.Square, src_keys, ['hb'], accum_out=STAT[:, 0:1])
        ACT(STAT[:, 1:2], STAT[:, 0:1], AF.Sqrt, ['hb'], ['stat'], bias=EPSB, scale=1.0 / D)
        RCP(STAT[:, 2:3], STAT[:, 1:2], ['stat'], ['stat'])
        STT(HB, src_ap, STAT[:, 2:3], MOD[:, gslot + w], ALU.mult, ALU.mult,
            src_keys + ['stat', ('mod', gslot + w)], ['hb'])
        TT('dve', HB, HB, MOD[:, sslot + w], ALU.add, ['hb', ('mod', sslot + w)], ['hb'])
        for half in range(2):
            bank = ps[half]
            for kk in range(4):
                k = half * 4 + kk
                TR(bank[:, kk * 128:(kk + 1) * 128], HB[:, k * 128:(k + 1) * 128], IDF,
                   ['hb', 'idf'], [('ps', half)])
            pv = bank.rearrange("p (k q) -> p k q", k=4)
            CP('act' if half == 0 else 'dve', HT[:, half * 4:half * 4 + 4, t * 128:(t + 1) * 128], pv,
               [('ps', half)], [('ht', t)])
            if router:
                CP('dve' if half == 0 else 'act', H2F[:, half * 4:half * 4 + 4, :], pv,
                   [('ps', half)], ['h2f'])


    class _Stop(Exception):
        pass
    KSTOP = int(os.environ.get('KSTOP', '0'))

    def ck(n):
        if KSTOP == n:
            raise _Stop()

    def x_src(l):
        return xin if l == 0 else xd

    def kv_tiles(m, t, lastl):
        if t < 2:
            return [(0, None), (1, None)]
        i = t - 2
        if m == 0:
            return [(j + 2, ('A', pid)) for (j, pid) in NAT_PLAN[i]] + [(0, None), (1, None)]
        if m == 2:
            lst = []
            if i - 1 >= 0:
                lst.append((t - 1, ('C', 0)))
            lst.append((t, None))
            if i + 1 < 16:
                lst.append((t + 1, ('C', 1)))
            return lst + [(0, None), (1, None)]
        return [(j, None) for j in range(NT)]

    GQN = SMP[:, 0:64]
    GKN = SMP[:, 64:128]
    MQN = SMP[:, 128:384]
    MKVN = SMP[:, 384:512]

    def evac_scaled(dst, src_ps, pkey, dkey, scale):
        P.op('act', lambda e: e.activation(dst, src_ps, AF.Copy, scale=scale), [pkey], [dkey])

    def transposes_to(dst_fn, src16, nblk, width, skey, dkey, bank_i=1):
        pb = ps[bank_i].bitcast(BF16)
        for b in range(nblk):
            TR(pb[0:width, b * 128:(b + 1) * 128], src16[:, b * width:(b + 1) * width], IDB,
               [skey, 'idb'], [('ps', bank_i)])
        for b in range(nblk):
            CP('act' if b % 2 == 0 else 'dve', dst_fn(b), pb[0:width, b * 128:(b + 1) * 128],
               [('ps', bank_i)], [dkey])

    def proj(bank_i, t, wview, ncols, wkey):
        for k in range(8):
            MM(ps[bank_i][:, 0:ncols], HT[:, k, t * 128:(t + 1) * 128], wview[:, k, 0:ncols], k == 0, k == 7,
               [('ht', t), wkey], [('ps', bank_i)])

    def layer(l):
        lastl = (l == DEPTH - 1)
        q_tiles = list(range(2, NT)) if lastl else list(range(NT))
        DMA(SMP, smallp[l:l + 1, :].partition_broadcast(128), [], ['smp'])
        ACT(SKE, SMP[:, 512:516], AF.Exp, ['smp'], ['ske'])
        mod_tiles(l, 1 * D, 'scale', 0, norm1_g[l:l + 1, :])
        mod_tiles(l, 0 * D, 'shift', 2)
        ck(1)
        for t in range(NT):
            DMA(XT, x_src(l)[t * 128:(t + 1) * 128, :], [], ['xt'])
            norm_tile(t, XT, ['xt'], 0, 2)
        P.barrier()
        ck(2)

        for m in [int(c_) for c_ in os.environ.get('KMIX', '0123')]:
            nh = 4
            nkv = 4 if m in (0, 3) else 2
            hd = 128 if m == 3 else 64
            if m == 0:
                load_w(w_in[l][:, OFF['ak']:OFF['ak'] + 512], 8, 512, WKV, 'wg')
                DMA_bias = True
                for c0 in range(0, NPAT * 4, 16):
                    c1 = min(c0 + 16, NPAT * 4)
                    s_ = stg_rr[0]
                    stg_rr[0] = (s_ + 1) % NSTG
                    sv = STG[:, s_, 0:(c1 - c0) * 128].rearrange("p (n q) -> p n q", q=128)
                    DMA(sv, biasA[l][:, c0:c1, :], [], [('stg', s_)])
                    CP('pool', BIAS[:, c0:c1, :], sv, [('stg', s_)], ['bias'])
                kvw = 512
            elif m == 1:
                load_w(w_in[l][:, OFF['bk']:OFF['bk'] + 256], 8, 256, WKV, 'wg')
                kvw = 256
            elif m == 2:
                load_w(w_in[l][:, OFF['ck']:OFF['ck'] + 256], 8, 256, WKV, 'wg')
                kvw = 256
            else:
                load_w(w_in[l][:, OFF['dkva']:OFF['dkva'] + 160], 8, 160, WKV, 'wg')
                load_w(mla_wkvb[l], 1, 512, WKVB.rearrange("p (k c) -> p k c", k=1), 'wkvb')
                kvw = 160
            if m == 0:
                ck(3)
            P.op('dve', lambda e: e.memset(VA[:, :, :, 64:65], 1.0), [], ['va'])
            for t in range(NT):
                lat = t >= 2
                proj(0, t, WKV, kvw, 'wg')
                CP('act', PBUF[:, 0:kvw], ps[0][:, 0:kvw], [('ps', 0)], ['pbuf'])
                if m == 0:
                    CP('dve', PB16[:, 0:256], PBUF[:, 0:256], ['pbuf'], ['pb16'])
                    CP('pool', VA[:, t, :, 0:64], PBUF[:, 256:512].rearrange("p (h d) -> p h d", h=4), ['pbuf'], ['va'])
                    transposes_to(lambda b: KT[0:64, b, t * 128:(t + 1) * 128], PB16, 4, 64, 'pb16', 'kt')
                elif m in (1, 2):
                    kview = PBUF[:, 0:128].rearrange("p (h d) -> p h d", h=2)
                    if m == 1:
                        head_rms(kview, 2, 64, GKN, 'pbuf')
                    k16 = PB16[:, 0:128].rearrange("p (h d) -> p h d", h=2)
                    if lat:
                        rope(k16, kview, ROPEH, t, 2, 32, 'pbuf', 'pb16')
                    else:
                        CP('dve', k16, kview, ['pbuf'], ['pb16'])
                    CP('pool', VA[:, t, 0:2, 0:64], PBUF[:, 128:256].rearrange("p (h d) -> p h d", h=2), ['pbuf'], ['va'])
                    transposes_to(lambda b: KT[0:64, b, t * 128:(t + 1) * 128], PB16, 2, 64, 'pb16', 'kt')
                else:
                    cview = PBUF[:, 0:128].rearrange("p (h d) -> p h d", h=1)
                    head_rms(cview, 1, 128, MKVN, 'pbuf')
                    CP('dve', PB16[:, 0:128], PBUF[:, 0:128], ['pbuf'], ['pb16'])
                    transposes_to(lambda b: DTT[:, 0, :], PB16, 1, 128, 'pb16', 'dtt')
                    ck(14)
                    MM(ps[2][:, 0:512], DTT[:, 0, :], WKVB, True, True, ['dtt', 'wkvb'], [('ps', 2)])
                    ck(15)
                    kf = PB16[:, 0:512].rearrange("p (h d) -> p h d", h=4)
                    P.op('dve', lambda e: e.memset(PB16[:, 0:512], 0.0), [], ['pb16'])
                    CP('act', PBUF[:, 512:1024], ps[2], [('ps', 2)], ['pbuf2'])
                    dkv = PBUF[:, 512:1024].rearrange("p (h d) -> p h d", h=4)
                    CP('pool', kf[:, :, 0:64], dkv[:, :, 0:64], ['pbuf2'], ['pb16'])
                    CP('pool', VA[:, t, :, 0:64], dkv[:, :, 64:128], ['pbuf2'], ['va'])
                    ck(16)
                    pe_src = PBUF[:, 128:160].rearrange("p (h d) -> p h d", h=1)
                    pe_dst = PBUF[:, 160:192].rearrange("p (h d) -> p h d", h=1)
                    if lat:
                        rope(pe_dst, pe_src, ROPER, t, 1, 16, 'pbuf', 'pbuf')
                    else:
                        CP('dve', pe_dst, pe_src, ['pbuf'], ['pbuf'])
                    ck(17)
                    CP('dve', kf[:, :, 64:96], pe_dst.to_broadcast([128, 4, 32]), ['pbuf'], ['pb16'])
                    ck(18)
                    transposes_to(lambda b: KT[:, b, t * 128:(t + 1) * 128], PB16, 4, 128, 'pb16', 'kt')
            if m == 0:
                ck(4)
            if m == 3:
                ck(10)
            qoff = [OFF['aq'], OFF['bq'], OFF['cq'], OFF['dqa']][m]
            load_w(w_in[l][:, qoff:qoff + 256], 8, 256, WQ, 'wq')
            if m == 3:
                load_w(mla_wqb[l], 2, 384, WQB, 'wqb')
            load_w(w_in[l][:, OFF['gate'] + m * D:OFF['gate'] + (m + 1) * D], 8, D, WG, 'wg')
            load_w(w_branch[l, m], 2, D, WBR, 'wbr')
            for t in q_tiles:
                lat = t >= 2
                proj(0, t, WQ, 256, 'wq')
                qscale = 0.125 if m != 3 else float(96 ** -0.5)
                if m == 3:
                    evac_scaled(PBUF[:, 0:256], ps[0][:, 0:256], ('ps', 0), 'pbuf', 1.0)
                    qv = PBUF[:, 0:256].rearrange("p (h d) -> p h d", h=1)
                    head_rms(qv, 1, 256, MQN, 'pbuf')
                    CP('dve', PB16[:, 0:256], PBUF[:, 0:256], ['pbuf'], ['pb16'])
                    transposes_to(lambda b: DTT[:, b, :], PB16, 2, 128, 'pb16', 'dtt')
                    for k in range(2):
                        MM(ps[2][:, 0:384], DTT[:, k, :], WQB[:, k, :], k == 0, k == 1, ['dtt', 'wqb'], [('ps', 2)])
                    evac_scaled(PBUF[:, 0:384], ps[2][:, 0:384], ('ps', 2), 'pbuf', qscale)
                    q4 = PBUF[:, 0:384].rearrange("p (h d) -> p h d", h=4)
                    q16 = PB16[:, 0:512].rearrange("p (h d) -> p h d", h=4)
                    P.op('dve', lambda e: e.memset(PB16[:, 0:512], 0.0), [], ['pb16'])
                    CP('pool', q16[:, :, 0:64], q4[:, :, 0:64], ['pbuf'], ['pb16'])
                    if lat:
                        rope(q16[:, :, 64:96], q4[:, :, 64:96], ROPER, t, 4, 16, 'pbuf', 'pb16')
                    else:
                        CP('dve', q16[:, :, 64:96], q4[:, :, 64:96], ['pbuf'], ['pb16'])
                    transposes_to(lambda b: QT[:, b, :], PB16, 4, 128, 'pb16', 'qt')
                else:
                    evac_scaled(PBUF[:, 0:256], ps[0][:, 0:256], ('ps', 0), 'pbuf', qscale)
                    q4 = PBUF[:, 0:256].rearrange("p (h d) -> p h d", h=4)
                    q16 = PB16[:, 0:256].rearrange("p (h d) -> p h d", h=4)
                    if m == 1:
                        head_rms(q4, 4, 64, GQN, 'pbuf')
                        P.op('dve', lambda e: e.tensor_scalar(PBUF[:, 0:256], PBUF[:, 0:256], 0.125, None, ALU.mult),
                             ['pbuf'], ['pbuf'])
                    if lat and m in (1, 2):
                        rope(q16, q4, ROPEH, t, 4, 32, 'pbuf', 'pb16')
                    else:
                        CP('dve', q16, q4, ['pbuf'], ['pb16'])
                    transposes_to(lambda b: QT[0:64, b, :], PB16, 4, 64, 'pb16', 'qt')
                kts = kv_tiles(m, t, lastl)
                OPS = ps[4][:, 0:260].rearrange("p (h d) -> p h d", h=4)
                grp_i = 0
                for h in range(4):
                    kvh = h if nkv == 4 else h // 2
                    for g0 in range(0, len(kts), 4):
                        grp = kts[g0:g0 + 4]
                        sb_i = 2 + (grp_i % 2)
                        pt_i = grp_i % 2
                        grp_i += 1
                        Sv = ps[sb_i].rearrange("p (g q) -> p g q", g=4)
                        for gi, (kt, bspec) in enumerate(grp):
                            MM(Sv[:, gi, :], KT[0:hd, kvh, kt * 128:(kt + 1) * 128], QT[0:hd, h, :], True, bspec is None,
                               ['kt', 'qt'], [('ps', sb_i)])
                            if bspec is not None:
                                brhs = BIAS[:, bspec[1] * 4 + h, :] if bspec[0] == 'A' else MASKC[:, bspec[1], :]
                                MM(Sv[:, gi, :], IDB, brhs, False, True, ['idb', 'bias', 'maskc'], [('ps', sb_i)])
                        ng = len(grp)
                        ACT(PT[:, pt_i, 0:ng, :], Sv[:, 0:ng, :], AF.Exp, [('ps', sb_i)], [('pt', pt_i)])
                        for gi, (kt, bspec) in enumerate(grp):
                            first = (g0 == 0 and gi == 0)
                            last = (g0 + gi == len(kts) - 1)
                            MM(OPS[:, h, :], PT[:, pt_i, gi, :], VA[:, kt, kvh, :], first, last,
                               [('pt', pt_i), 'va'], [('ps', 4)])
                den = RS[:, 32:36]
                if m == 2:
                    TT('dve', den, OPS[:, :, 64], SKE, ALU.add, [('ps', 4), 'ske'], ['rs2'])
                else:
                    CP('dve', den, OPS[:, :, 64], [('ps', 4)], ['rs2'])
                RCP(RS[:, 36:40], den, ['rs2'], ['rs2'])
                TT('dve', OB.rearrange("p (h d) -> p h d", h=4), OPS[:, :, 0:64],
                   RS[:, 36:40].rearrange("p (h o) -> p h o", o=1).to_broadcast([128, 4, 64]), ALU.mult,
                   [('ps', 4), 'rs2'], ['ob'])
                transposes_to(lambda b: OT[:, b, :], OB, 2, 128, 'ob', 'ot', bank_i=1)
                for hf in range(2):
                    cs = slice(hf * 512, (hf + 1) * 512)
                    for k in range(8):
                        MM(ps[5], HT[:, k, t * 128:(t + 1) * 128], WG[:, k, cs], k == 0, k == 7,
                           [('ht', t), 'wg'], [('ps', 5)])
                    for k in range(2):
                        MM(ps[6], OT[:, k, :], WBR[:, k, cs], k == 0, k == 1, ['ot', 'wbr'], [('ps', 6)])
                    ACT(SIG[:, cs], ps[5], AF.Sigmoid, [('ps', 5)], ['sig'])
                    TT('dve', MTL[:, cs], ps[6], SIG[:, cs], ALU.mult, [('ps', 6), 'sig'], ['mtl'])
                DMA(md[m, t * 128:(t + 1) * 128, :], MTL, ['mtl'], [('md', m, t)])
                if m == 0 and t == 0:
                    ck(5)
                if m == 3 and t == 0:
                    ck(11)
                if m == 3 and t == 2:
                    ck(12)
            ck(6 + m)
        P.barrier()

        mod_tiles(l, 2 * D, 'gate', 0, which=(0,) if lastl else (0, 1))
        load_w(w_out[l], 8, D, WO, 'wo')
        for t in q_tiles:
            w = 0 if t >= 2 else 1
            DMA(M4, md[:, t * 128:(t + 1) * 128, :].rearrange("m p d -> p m d"),
                [('md', mm_, t) for mm_ in range(4)], ['m4'])
            DMA(XT, x_src(l)[t * 128:(t + 1) * 128, :], [], ['xt'])
            TT('dve', M4[:, 0, :], M4[:, 0, :], M4[:, 1, :], ALU.add, ['m4'], ['m4'])
            TT('pool', M4[:, 2, :], M4[:, 2, :], M4[:, 3, :], ALU.add, ['m4'], ['m4b'])
            TT('dve', M4[:, 0, :], M4[:, 0, :], M4[:, 2, :], ALU.add, ['m4', 'm4b'], ['m4'])
            pb = ps[1].bitcast(BF16)
            for k in range(8):
                TR(pb[:, k * 128:(k + 1) * 128], M4[:, 0, k * 128:(k + 1) * 128], IDB, ['m4', 'idb'], [('ps', 1)])
            CP('act', MT8, pb.rearrange("p (k q) -> p k q", k=8), [('ps', 1)], ['mt8'])
            for hf in range(2):
                cs = slice(hf * 512, (hf + 1) * 512)
                for k in range(8):
                    MM(ps[5 + hf], MT8[:, k, :], WO[:, k, cs], k == 0, k == 7, ['mt8', 'wo'], [('ps', 5 + hf)])
                TT('dve', HB[:, cs], ps[5 + hf], MOD[:, w, cs], ALU.mult, [('ps', 5 + hf), ('mod', w)], ['hb'])
                TT('pool', XS[:, t, cs], HB[:, cs], XT[:, cs], ALU.add, ['hb', 'xt'], [('x', t)])
        P.barrier()
        if dbg == ('xm', l):
            for t in q_tiles:
                DMA(dbg_out[t * 128:(t + 1) * 128, :], XS[:, t, :], [('x', t)], ['dbgo'])
            return True

        moe = (l % 2 == 1)
        mod_tiles(l, 4 * D, 'scale', 0, norm2_g[l:l + 1, :], which=(0,) if lastl else (0, 1))
        mod_tiles(l, 3 * D, 'shift', 2, which=(0,) if lastl else (0, 1))
        if moe:
            DMA(RT, moe_router[l // 2].rearrange("(k p) e -> p k e", p=128), [], ['rt'])
            P.op('dve', lambda e: e.memset(RTB, 0.0), [], ['rtb'])
            CP('dve', RTB[:, :, 0:8], RT, ['rt', 'rtb'], ['rtb'])
        for t in q_tiles:
            norm_tile(t, XS[:, t, :], [('x', t)], 0, 2, router=False)
            if moe:
                lg = ps[7][:, 0:NEXP]
                for k in range(8):
                    MM(ps[7][:, 0:16], HT[:, k, t * 128:(t + 1) * 128], RTB[:, k, :], k == 0, k == 7, [('ht', t), 'rtb'], [('ps', 7)])
                L = RS[:, 0:8]
                CP('dve', L, lg, [('ps', 7)], ['rs'])
                RED(RS[:, 8:9], L, ALU.max, ['rs'], ['rs'])
                TT('dve', RS[:, 16:24], L, RS[:, 8:9].to_broadcast([128, 8]), ALU.is_equal, ['rs'], ['rs'])
                STT(RS[:, 24:32], RS[:, 16:24], -1e30, L, ALU.mult, ALU.add, ['rs'], ['rs'])
                RED(RS[:, 9:10], RS[:, 24:32], ALU.max, ['rs'], ['rs'])
                TT('dve', RS[:, 40:48], RS[:, 24:32], RS[:, 9:10].to_broadcast([128, 8]), ALU.is_equal, ['rs'], ['rs'])
                TT('dve', RS[:, 10:11], RS[:, 9:10], RS[:, 8:9], ALU.subtract, ['rs'], ['rs'])
                ACT(RS[:, 11:12], RS[:, 10:11], AF.Exp, ['rs'], ['rs'])
                TS('dve', RS[:, 12:13], RS[:, 11:12], 1.0, None, ALU.add, None, ['rs'], ['rs'])
                RCP(RS[:, 13:14], RS[:, 12:13], ['rs'], ['rs'])
                TT('dve', RS[:, 14:15], RS[:, 11:12], RS[:, 13:14], ALU.mult, ['rs'], ['rs'])
                TT('dve', RS[:, 48:56], RS[:, 16:24], RS[:, 13:14].to_broadcast([128, 8]), ALU.mult, ['rs'], ['rs'])
                STT(COMB[:, t, :], RS[:, 40:48], RS[:, 14:15], RS[:, 48:56], ALU.mult, ALU.add, ['rs'], ['comb'])
                if t == 0:
                    ck(20)
        mod_tiles(l, 5 * D, 'gate', 0, which=(0,) if lastl else (0, 1))

        chunks = []
        if not lastl:
            chunks.append([0, 1])
        for c in range(4):
            chunks.append([2 + 4 * c + i for i in range(4)])
        nexp = NEXP if moe else 1
        for e_ in range(nexp):
            if moe:
                w1, w3, w2 = moe_w1[l // 2, e_], moe_w3[l // 2, e_], moe_w2[l // 2, e_]
            else:
                w1, w3, w2 = ffn_w1[l // 2], ffn_w3[l // 2], ffn_w2[l // 2]
            for g in range(DFF // 512):
                load_w(w1[:, g * 512:(g + 1) * 512], 8, 512, W1B, 'w1b')
                load_w(w3[:, g * 512:(g + 1) * 512], 8, 512, W3B, 'w3b')
                load_w(w2[g * 512:(g + 1) * 512, :], 4, D, W2B, 'w2b')
                for ch in chunks:
                    n = len(ch) * 128
                    t0 = ch[0] * 128
                    for fc in range(4):
                        ab = fc % 2
                        for k in range(8):
                            MM(ps[ab][:, 0:n], W1B[:, k, fc * 128:(fc + 1) * 128], HT[:, k, t0:t0 + n], k == 0, k == 7,
                               ['w1b'] + [('ht', t) for t in ch], [('ps', ab)])
                        for k in range(8):
                            MM(ps[2 + ab][:, 0:n], W3B[:, k, fc * 128:(fc + 1) * 128], HT[:, k, t0:t0 + n], k == 0, k == 7,
                               ['w3b'] + [('ht', t) for t in ch], [('ps', 2 + ab)])
                        ACT(SA[:, ab, 0:n], ps[ab][:, 0:n], AF.Silu, [('ps', ab)], [('sa', ab)])
                        TT('dve', UT[:, fc, 0:n], ps[2 + ab][:, 0:n], SA[:, ab, 0:n], ALU.mult,
                           [('ps', 2 + ab), ('sa', ab)], [('ut', fc)])
                    for ti, t in enumerate(ch):
                        w = 0 if t >= 2 else 1
                        ob_ = 4 + 2 * (ti % 2)
                        for hf in range(2):
                            cs = slice(hf * 512, (hf + 1) * 512)
                            for fc in range(4):
                                MM(ps[ob_ + hf], UT[:, fc, ti * 128:(ti + 1) * 128], W2B[:, fc, cs], fc == 0, fc == 3,
                                   [('ut', fc), 'w2b'], [('ps', ob_ + hf)])
                            if moe:
                                STT(HB[:, cs], ps[ob_ + hf], COMB[:, t, e_:e_ + 1], MOD[:, w, cs], ALU.mult, ALU.mult,
                                    [('ps', ob_ + hf), 'comb', ('mod', w)], [('hbh', hf)])
                            else:
                                TT('dve', HB[:, cs], ps[ob_ + hf], MOD[:, w, cs], ALU.mult,
                                   [('ps', ob_ + hf), ('mod', w)], [('hbh', hf)])
                            TT('pool', XS[:, t, cs], XS[:, t, cs], HB[:, cs], ALU.add, [('hbh', hf), ('x', t)], [('x', t)])
        P.barrier()
        if dbg == ('xf', l):
            for t in q_tiles:
                DMA(dbg_out[t * 128:(t + 1) * 128, :], XS[:, t, :], [('x', t)], ['dbgo'])
            return True
        if not lastl:
            for t in range(NT):
                DMA(xd[t * 128:(t + 1) * 128, :], XS[:, t, :], [('x', t)], [('xd', t)])
            P.barrier()
        return False

    stopped = False
    try:
        for l in range(n_layers):
            stopped = layer(l)
            if stopped:
                break
    except _Stop:
        stopped = True
    if not stopped and n_layers == DEPTH:
        DMA(MOD[:, 0, :], final_g.partition_broadcast(128), [], [('mod', 0)])
        for t in range(2, NT):
            src = XS[:, t, :]
            ACT(HB, src, AF.Square, [('x', t)], ['hb'], accum_out=STAT[:, 0:1])
            ACT(STAT[:, 1:2], STAT[:, 0:1], AF.Sqrt, ['hb'], ['stat'], bias=EPSB, scale=1.0 / D)
            RCP(STAT[:, 2:3], STAT[:, 1:2], ['stat'], ['stat'])
            STT(HB, src, STAT[:, 2:3], MOD[:, 0, :], ALU.mult, ALU.mult, [('x', t), 'stat', ('mod', 0)], ['hb'])
            DMA(out[(t - 2) * 128:(t - 1) * 128, :], HB, ['hb'], ['out'])
    elif not stopped:
        DMA(out[0:128, :], HB, [], ['out'])
    P.barrier()
    P.emit()
    print("instructions:", P.n, {k: len(v) for k, v in P.streams.items()})


_CACHE = {}


def make_in_maps(inp, n_cores=8):
    f = lambda a: np.ascontiguousarray(np.asarray(a), dtype=np.float32)
    cosH, sinH, cosR, sinR = _rope_tables()
    shared = {
        "norm1_g": f(inp["norm1_g"]), "norm2_g": f(inp["norm2_g"]), "w_ada": f(inp["w_ada"]),
        "b_ada": f(inp["b_ada"]), "w_in": f(inp["w_in"]), "biasA": _natten_bias(f(inp["na_rpb"])),
        "smallp": np.ascontiguousarray(np.concatenate(
            [f(inp["gb_qnorm"]), f(inp["gb_knorm"]), f(inp["mla_qnorm"]), f(inp["mla_kvnorm"]), f(inp["wc_sink"])],
            axis=1)),
        "mla_wqb": f(inp["mla_wqb"]), "mla_wkvb": f(inp["mla_wkvb"]), "w_branch": f(inp["w_branch"]),
        "w_out": f(inp["w_out"]), "ffn_w1": f(inp["ffn_w1"]), "ffn_w3": f(inp["ffn_w3"]), "ffn_w2": f(inp["ffn_w2"]),
        "moe_router": f(inp["moe_router"]), "moe_w1": f(inp["moe_w1"]), "moe_w3": f(inp["moe_w3"]),
        "moe_w2": f(inp["moe_w2"]), "final_g": f(inp["final_g"]).reshape(1, D),
        "c_ident": np.eye(128, dtype=np.float32),
        "c_ropeH": np.ascontiguousarray(np.stack([cosH, sinH])),
        "c_ropeR": np.ascontiguousarray(np.stack([cosR, sinR])),
        "c_maskC": _maskC(),
    }
    x = f(inp["x"]); ctx = f(inp["ctx"]); c = f(inp["c"]); cc = f(inp["c_ctx"])
    maps = []
    for b in range(n_cores):
        m = dict(shared)
        m["xin"] = np.ascontiguousarray(np.concatenate([ctx[b], x[b]], axis=0))
        m["cvec"] = np.ascontiguousarray(np.stack([c[b], cc]))
        maps.append(m)
    return maps


def kernel(**inputs):
    if "nc" not in _CACHE:
        _CACHE["nc"] = build(DEPTH)
    nc = _CACHE["nc"]
    maps = make_in_maps(inputs, 8)
    res = run_bass_kernel_spmd(nc, maps, core_ids=list(range(8)))
    return np.stack([np.asarray(r["out"], dtype=np.float32) for r in res.results], axis=0)
```

```python
import numpy as np
from contextlib import ExitStack
import concourse.bass as bass
import concourse.mybir as mybir
from concourse.bass_utils import run_bass_kernel_spmd

F32 = mybir.dt.float32
BF16 = mybir.dt.bfloat16
AF = mybir.ActivationFunctionType
ALU = mybir.AluOpType
AX = mybir.AxisListType

D = 1024
S = 2048
C = 256
T = S + C
NT = T // 128
DEPTH = 4
IN_W = 6304
DFF = 3584
NEXP = 8
EPS = 1e-6
NEG = -30000.0
NDMA = 12
NSTG = 3

OFF = dict(aq=0, ak=256, av=512, bq=768, bk=1024, bv=1152, cq=1280, ck=1536, cv=1664,
           dqa=1792, dkva=2048, gate=2208)


class Prog:
    CE = ['pe', 'act', 'dve', 'pool']

    def __init__(self, nc, es):
        self.nc = nc
        self.streams = {e: [] for e in self.CE + ['sp']}
        self.sems = {}
        for e in self.CE:
            self.sems[e] = es.enter_context(nc.semaphore("s_" + e))
        for i in range(NDMA):
            self.sems['d%d' % i] = es.enter_context(nc.semaphore("s_d%d" % i))
        self.cnt = {k: 0 for k in self.sems}
        self.known = {e: {k: 0 for k in self.sems} for e in self.streams}
        self.lastw = {}
        self.readers = {}
        self.dma_rr = 0
        self.n = 0
        self.last_real = {e: None for e in self.CE}
        self.pend = {e: False for e in self.CE}

    def _materialize(self, s):
        if s in self.pend and self.pend[s]:
            self.streams[s][self.last_real[s]][2] = s
            self.cnt[s] += 1
            self.pend[s] = False

    def _deps(self, eng, reads, writes):
        deps = {}

        def add(d):
            if d is not None:
                if d[0] == 'pe' and eng == 'pe':
                    return
                if deps.get(d[0], 0) < d[1]:
                    deps[d[0]] = d[1]
        for k in reads:
            add(self.lastw.get(k))
        for k in writes:
            add(self.lastw.get(k))
            for r in self.readers.get(k, ()):
                add(r)
        waits = []
        kn = self.known[eng]
        for s, i in deps.items():
            if kn[s] < i:
                if i > self.cnt[s]:
                    self._materialize(s)
                assert i <= self.cnt[s], (s, i, self.cnt[s])
                kn[s] = i
                waits.append((s, i))
        return waits

    def _commit(self, tok, reads, writes):
        for k in writes:
            self.lastw[k] = tok
            self.readers[k] = []
        for k in reads:
            self.readers.setdefault(k, []).append(tok)

    def op(self, eng, fn, reads=(), writes=()):
        waits = self._deps(eng, reads, writes)
        tok = (eng, self.cnt[eng] + 1)
        self.streams[eng].append([waits, fn, None])
        self.last_real[eng] = len(self.streams[eng]) - 1
        self.pend[eng] = True
        self._commit(tok, reads, writes)
        self.n += 1

    def dma(self, fn, reads=(), writes=()):
        s = 'd%d' % self.dma_rr
        self.dma_rr = (self.dma_rr + 1) % NDMA
        waits = self._deps('sp', reads, writes)
        if self.known['sp'][s] < self.cnt[s]:
            self.known['sp'][s] = self.cnt[s]
            waits.append((s, self.cnt[s]))
        self.cnt[s] += 1
        tok = (s, self.cnt[s])
        self.streams['sp'].append([waits, fn, s])
        self._commit(tok, reads, writes)
        self.n += 1

    def barrier(self):
        for e in self.CE:
            self._materialize(e)
        snap = dict(self.cnt)
        for e in self.streams:
            waits = []
            for s, i in snap.items():
                if self.known[e][s] < i:
                    self.known[e][s] = i
                    waits.append((s, i))
            if waits:
                self.streams[e].append([waits, None, None])

    def emit(self):
        nc = self.nc
        sems = self.sems

        def mult(s):
            return 16 if s[0] == 'd' else 1

        def run(engine, stream):
            for waits, fn, inc in stream:
                for s, i in waits:
                    engine.wait_ge(sems[s], i * mult(s))
                if fn is None:
                    continue
                ins = fn(engine)
                if inc is not None:
                    ins.then_inc(sems[inc], mult(inc))

        with nc.Block() as block:
            @block.tensor
            def _(e):
                run(e, self.streams['pe'])

            @block.scalar
            def _(e):
                run(e, self.streams['act'])

            @block.vector
            def _(e):
                run(e, self.streams['dve'])

            @block.gpsimd
            def _(e):
                run(e, self.streams['pool'])

            @block.sync
            def _(e):
                run(e, self.streams['sp'])


def _rope_tables():
    def ang(pos, dim):
        inv = (10000.0 ** (-np.arange(0, dim, 2, dtype=np.float32) / dim)).astype(np.float32)
        return pos.astype(np.float32)[:, None] * inv[None, :]
    t = np.arange(S)
    out = []
    for rot in (64, 32):
        half = rot // 2
        a = np.concatenate([ang(t // 64, half), ang(t % 64, half)], axis=-1).astype(np.float32)
        out += [np.cos(a).astype(np.float32), np.sin(a).astype(np.float32)]
    return out


def _natten_plan():
    rows = S // 64
    kh = 8
    pats = {}
    plan = []
    for i in range(16):
        lst = []
        for j in range(16):
            sig = []
            anyv = False
            for a in range(2):
                for b in range(2):
                    r = 2 * i + b
                    rk = 2 * j + a
                    st = min(max(r - kh // 2, 0), rows - kh)
                    if st <= rk < st + kh:
                        sig.append(rk - r + 7)
                        anyv = True
                    else:
                        sig.append(-1)
            if anyv:
                sig = tuple(sig)
                if sig not in pats:
                    pats[sig] = len(pats)
                lst.append((j, pats[sig]))
        plan.append(lst)
    return plan, pats


NAT_PLAN, NAT_PATS = _natten_plan()
NPAT = len(NAT_PATS)


def _natten_bias(rpb):
    col = np.arange(64)
    cstart = np.clip(col - 8, 0, 48)
    col_ok = (col[None, :] >= cstart[:, None]) & (col[None, :] < cstart[:, None] + 16)
    dc = np.clip(col[None, :] - col[:, None] + 15, 0, 30)
    out = np.full((DEPTH, 128, NPAT * 4, 128), NEG, dtype=np.float32)
    for sig, pid in NAT_PATS.items():
        for a in range(2):
            for b in range(2):
                dr = sig[a * 2 + b]
                if dr < 0:
                    continue
                g = rpb[:, :, dr, :][:, :, dc]
                g = np.where(col_ok[None, None], g, np.float32(NEG))
                g = np.transpose(g, (0, 3, 1, 2))
                out[:, a * 64:(a + 1) * 64, pid * 4:(pid + 1) * 4, b * 64:(b + 1) * 64] = g
    return out


def _maskC():
    kk = np.arange(128)[:, None]
    qq = np.arange(128)[None, :]
    m0 = np.where(qq <= kk, 0.0, NEG).astype(np.float32)
    m1 = np.where(kk <= qq, 0.0, NEG).astype(np.float32)
    return np.stack([m0, m1], axis=1)


def build(n_layers=DEPTH, dbg=None):
    nc = bass.Bass("TRN2", target_bir_lowering=False, dynamic_dma_scratch_size=256)
    es = ExitStack()
    with es:
        _build_body(nc, es, n_layers, dbg)
    return nc


def _build_body(nc, es, n_layers, dbg):
    def din(name, shape):
        return nc.dram_tensor(name, list(shape), F32, kind="ExternalInput").ap()

    xin = din("xin", [T, D])
    cvec = din("cvec", [2, D])
    norm1_g = din("norm1_g", [DEPTH, D])
    norm2_g = din("norm2_g", [DEPTH, D])
    w_ada = din("w_ada", [DEPTH, D, 6 * D])
    b_ada = din("b_ada", [DEPTH, 6 * D])
    w_in = din("w_in", [DEPTH, D, IN_W])
    biasA = din("biasA", [DEPTH, 128, NPAT * 4, 128])
    smallp = din("smallp", [DEPTH, 516])
    mla_wqb = din("mla_wqb", [DEPTH, 256, 384])
    mla_wkvb = din("mla_wkvb", [DEPTH, 128, 512])
    w_branch = din("w_branch", [DEPTH, 4, 256, D])
    w_out = din("w_out", [DEPTH, D, D])
    ffn_w1 = din("ffn_w1", [2, D, DFF])
    ffn_w3 = din("ffn_w3", [2, D, DFF])
    ffn_w2 = din("ffn_w2", [2, DFF, D])
    moe_router = din("moe_router", [2, D, NEXP])
    moe_w1 = din("moe_w1", [2, NEXP, D, DFF])
    moe_w3 = din("moe_w3", [2, NEXP, D, DFF])
    moe_w2 = din("moe_w2", [2, NEXP, DFF, D])
    final_g = din("final_g", [1, D])
    c_ident = din("c_ident", [128, 128])
    c_ropeH = din("c_ropeH", [2, S, 32])
    c_ropeR = din("c_ropeR", [2, S, 16])
    c_maskC = din("c_maskC", [128, 2, 128])
    out = nc.dram_tensor("out", [S, D], F32, kind="ExternalOutput").ap()
    xd = nc.dram_tensor("xd", [T, D], F32, kind="Internal").ap()
    md = nc.dram_tensor("md", [4, T, D], BF16, kind="Internal").ap()
    dbg_out = None
    if dbg is not None:
        dbg_out = nc.dram_tensor("dbg", [T, D], F32, kind="ExternalOutput").ap()

    P = Prog(nc, es)

    def sb(name, shape, dt):
        return es.enter_context(nc.sbuf_tensor(name, list(shape), dt))[:]

    ps = [es.enter_context(nc.psum_tensor("ps%d" % i, [128, 512], F32))[:] for i in range(8)]

    IDF = sb("idf", [128, 128], F32)
    IDB = sb("idb", [128, 128], BF16)
    ROPEH = sb("ropeh", [128, 2, 16, 32], F32)
    ROPER = sb("roper", [128, 2, 16, 16], F32)
    MASKC = sb("maskc", [128, 2, 128], BF16)
    CV = sb("cv", [128, 2, 8], F32)
    SL = sb("sl", [128, 2, 8, 128], F32)
    MOD = sb("mod", [128, 4, D], F32)
    SMP = sb("smp", [128, 516], F32)
    SKE = sb("ske", [128, 4], F32)
    HT = sb("ht", [128, 8, T], BF16)
    STG = sb("stg", [128, NSTG, 2048], F32)
    XT = sb("xt", [128, D], F32)
    HB = sb("hb", [128, D], F32)
    PB = HB
    STAT = sb("stat", [128, 32], F32)
    PBUF = sb("pbuf", [128, D], F32)
    TMPF = sb("tmpf", [128, 4, 256], F32)
    PB16 = sb("pb16", [128, 512], BF16)
    QT = sb("qt", [128, 4, 128], BF16)
    OB = sb("ob", [128, 256], BF16)
    OT = sb("ot", [128, 2, 128], BF16)
    DTT = sb("dtt", [128, 2, 128], BF16)
    MT8 = sb("mt8", [128, 8, 128], BF16)
    RT = sb("rt", [128, 8, NEXP], F32)
    RTB = sb("rtb", [128, 8, 16], BF16)
    COMB = sb("comb", [128, NT, NEXP], F32)
    RS = sb("rs", [128, 64], F32)
    UT = sb("ut", [128, 4, 512], BF16)
    SA = sb("sa", [128, 2, 512], BF16)
    R4 = sb("r4", [128, 12288], BF16)
    R2 = sb("r2", [128, 36864], BF16)

    KT = R2[:, 0:9216].rearrange("p (h t) -> p h t", h=4)
    VA = R2[:, 9216:13896].rearrange("p (t h d) -> p t h d", t=NT, h=4)
    BIAS = R2[:, 13896:13896 + NPAT * 4 * 128].rearrange("p (n q) -> p n q", q=128)
    XS = R2.bitcast(F32).rearrange("p (t d) -> p t d", t=NT)
    b0 = 13896 + NPAT * 4 * 128
    WG = R2[:, b0:b0 + 8192].rearrange("p (k c) -> p k c", k=8)
    WKV = WG
    WQ = R2[:, b0 + 8192:b0 + 10240].rearrange("p (k c) -> p k c", k=8)
    WBR = R2[:, b0 + 10240:b0 + 12288].rearrange("p (k c) -> p k c", k=2)
    WQB = R2[:, b0 + 12288:b0 + 13056].rearrange("p (k c) -> p k c", k=2)
    WKVB = R2[:, b0 + 13056:b0 + 13568]
    SIG = R2[:, b0 + 13568:b0 + 14592]
    MTL = R2[:, b0 + 14592:b0 + 15616]
    PT = R2[:, b0 + 15616:b0 + 16640].rearrange("p (a g q) -> p a g q", a=2, g=4)
    assert b0 + 16640 <= 36864
    WO = R4[:, 0:8192].rearrange("p (k c) -> p k c", k=8)
    M4 = MOD[:, 2:4, :].rearrange("p a d -> p (a d)").bitcast(BF16).rearrange("p (a d) -> p a d", a=4)
    H2F = PBUF.rearrange("p (k q) -> p k q", k=8)
    W1B = R4[:, 0:4096].rearrange("p (k c) -> p k c", k=8)
    W3B = R4[:, 4096:8192].rearrange("p (k c) -> p k c", k=8)
    W2B = R4[:, 8192:12288].rearrange("p (k c) -> p k c", k=4)

    def MM(o, lhsT, rhs, start, stop, r, w):
        P.op('pe', lambda e: e.matmul(o, lhsT, rhs, start=start, stop=stop), r, w)

    def TR(o, i, ident, r, w):
        P.op('pe', lambda e: e.transpose(o, i, ident), r, w)

    def ACT(o, i, func, r, w, **kw):
        P.op('act', lambda e: e.activation(o, i, func, **kw), r, w)

    def TT(eng, o, a, b, op, r, w):
        P.op(eng, lambda e: e.tensor_tensor(o, a, b, op), r, w)

    def TS(eng, o, a, s1, s2, op0, op1, r, w):
        if op1 is None:
            P.op(eng, lambda e: e.tensor_scalar(o, a, s1, None, op0), r, w)
        else:
            P.op(eng, lambda e: e.tensor_scalar(o, a, s1, s2, op0, op1), r, w)

    def STT(o, a, s, b, op0, op1, r, w):
        P.op('dve', lambda e: e.scalar_tensor_tensor(o, a, s, b, op0, op1), r, w)

    def CP(eng, o, i, r, w):
        if eng == 'act':
            P.op('act', lambda e: e.activation(o, i, AF.Copy), r, w)
        else:
            P.op(eng, lambda e: e.tensor_copy(o, i), r, w)

    def RED(o, i, op, r, w):
        P.op('dve', lambda e: e.tensor_reduce(o, i, AX.X, op), r, w)

    def RCP(o, i, r, w):
        P.op('dve', lambda e: e.reciprocal(o, i), r, w)

    def DMA(o, i, r, w):
        P.dma(lambda e: e.dma_start(out=o, in_=i), r, w)

    stg_rr = [0]

    def load_w(src, kc, ncols, dst, dkey, cast=True, use=None):
        pcw = 2048 // kc
        c0 = 0
        while c0 < ncols:
            cw = min(pcw, ncols - c0)
            s = stg_rr[0]
            stg_rr[0] = (s + 1) % NSTG
            sv = STG[:, s, 0:kc * cw].rearrange("p (k c) -> p k c", k=kc)
            DMA(sv, src[:, c0:c0 + cw].rearrange("(k p) c -> p k c", p=128), [], [('stg', s)])
            if cast:
                CP('pool', dst[:, :, c0:c0 + cw], sv, [('stg', s)], [dkey])
            else:
                use(sv, c0, cw, ('stg', s))
            c0 += cw

    import os
    KS = int(os.environ.get("KSETUP", "99"))
    if KS > 0:
        DMA(IDF, c_ident, [], ['idf'])
        CP('dve', IDB, IDF, ['idf'], ['idb'])
    if KS > 1:
        DMA(ROPEH[:, 0], c_ropeH[0].rearrange("(t p) d -> p t d", p=128), [], ['rope'])
        DMA(ROPEH[:, 1], c_ropeH[1].rearrange("(t p) d -> p t d", p=128), [], ['rope'])
        DMA(ROPER[:, 0], c_ropeR[0].rearrange("(t p) d -> p t d", p=128), [], ['rope'])
        DMA(ROPER[:, 1], c_ropeR[1].rearrange("(t p) d -> p t d", p=128), [], ['rope'])
    if KS > 2:
        DMA(PBUF[:, 0:256].rearrange("p (a q) -> p a q", a=2), c_maskC, [], ['pbuf'])
        CP('dve', MASKC, PBUF[:, 0:256].rearrange("p (a q) -> p a q", a=2), ['pbuf'], ['maskc'])
    if KS > 3:
        P.dma(lambda e: e.dma_start(out=CV, in_=cvec.rearrange("w (k p) -> p w k", p=128),
                                    allow_slow_non_contiguous=True), [], ['cv'])
    if KS > 4:
        ACT(CV, CV, AF.Silu, ['cv'], ['cv'])
    if KS > 5:
        for wch in range(2):
            CP('dve', SL[:, wch], CV[:, wch, :].rearrange("p (k o) -> p k o", o=1).to_broadcast([128, 8, 128]),
               ['cv'], ['sl'])

    def rope_view(tab, which, t, nh, half):
        return tab[:, which, t - 2, :].rearrange("p (o d) -> p o d", o=1).to_broadcast([128, nh, half])

    def rope(dst, src, tab, t, nh, half, skey, dkey):
        c = rope_view(tab, 0, t, nh, half)
        s = rope_view(tab, 1, t, nh, half)
        x1 = src[:, :, 0:half]
        x2 = src[:, :, half:2 * half]
        t1 = TMPF[:, 0, 0:nh * half].rearrange("p (h d) -> p h d", h=nh)
        t2 = TMPF[:, 1, 0:nh * half].rearrange("p (h d) -> p h d", h=nh)
        t3 = TMPF[:, 2, 0:nh * half].rearrange("p (h d) -> p h d", h=nh)
        t4 = TMPF[:, 3, 0:nh * half].rearrange("p (h d) -> p h d", h=nh)
        TT('dve', t1, x1, c, ALU.mult, [skey, 'rope'], ['tf0'])
        TT('pool', t2, x2, s, ALU.mult, [skey, 'rope'], ['tf1'])
        TT('dve', t3, x1, s, ALU.mult, [skey, 'rope'], ['tf2'])
        TT('pool', t4, x2, c, ALU.mult, [skey, 'rope'], ['tf3'])
        TT('dve', dst[:, :, 0:half], t1, t2, ALU.subtract, ['tf0', 'tf1'], [dkey])
        TT('dve', dst[:, :, half:2 * half], t3, t4, ALU.add, ['tf2', 'tf3'], [dkey])

    def head_rms(src, nh, hd, gain, skey):
        sq = TMPF[:, 0:1, :].rearrange("p a c -> p (a c)")[:, 0:nh * hd].rearrange("p (h d) -> p h d", h=nh)
        TT('dve', sq, src, src, ALU.mult, [skey], ['tf0'])
        RED(RS[:, 0:nh], sq, ALU.add, ['tf0'], ['rs'])
        ACT(RS[:, 8:8 + nh], RS[:, 0:nh], AF.Sqrt, ['rs'], ['rs'], bias=EPSB, scale=1.0 / hd)
        RCP(RS[:, 16:16 + nh], RS[:, 8:8 + nh], ['rs'], ['rs'])
        TT('dve', src, src, RS[:, 16:16 + nh].rearrange("p (h o) -> p h o", o=1).to_broadcast([128, nh, hd]),
           ALU.mult, [skey, 'rs'], [skey])
        TT('dve', src, src, gain.rearrange("p (o d) -> p o d", o=1).to_broadcast([128, nh, hd]),
           ALU.mult, [skey, 'smp'], [skey])

    EPSB = sb("epsb", [128, 1], F32)
    P.op('dve', lambda e: e.memset(EPSB, EPS), [], ['epsb'])

    def mod_tiles(l, col0, kind, slot, gain_src=None, which=(0, 1)):
        DMA(PB, b_ada[l:l + 1, col0:col0 + D].partition_broadcast(128), [], ['hb'])

        def use(sv, c0, cw, skey):
            for w in which:
                bank = ps[6 + w]
                for k in range(8):
                    MM(bank[:, 0:cw], SL[:, w, k, :], sv[:, k, :], k == 0, k == 7,
                       ['sl', skey], [('ps', 6 + w)])
                TT('dve', MOD[:, slot + w, c0:c0 + cw], bank[:, 0:cw], PB[:, c0:c0 + cw], ALU.add,
                   [('ps', 6 + w), 'hb'], [('mod', slot + w)])
        load_w(w_ada[l][:, col0:col0 + D], 8, D, None, None, cast=False, use=use)
        if kind == 'scale':
            DMA(PB, gain_src.partition_broadcast(128), [], ['hb'])
            for w in which:
                STT(MOD[:, slot + w], MOD[:, slot + w], 1.0, PB, ALU.add, ALU.mult,
                    [('mod', slot + w), 'hb'], [('mod', slot + w)])

    def norm_tile(t, src_ap, src_keys, gslot, sslot, router=False, xkey=None):
        w = 0 if t >= 2 else 1
        ACT(HB, src_ap, AF.Square, src_keys, ['hb'], accum_out=STAT[:, 0:1])
        ACT(STAT[:, 1:2], STAT[:, 0:1], AF.Sqrt, ['hb'], ['stat'], bias=EPSB, scale=1.0 / D)
        RCP(STAT[:, 2:3], STAT[:, 1:2], ['stat'], ['stat'])
        STT(HB, src_ap, STAT[:, 2:3], MOD[:, gslot + w], ALU.mult, ALU.mult,
            src_keys + ['stat', ('mod', gslot + w)], ['hb'])
        TT('dve', HB, HB, MOD[:, sslot + w], ALU.add, ['hb', ('mod', sslot + w)], ['hb'])
        for half in range(2):
            bank = ps[half]
            for kk in range(4):
                k = half * 4 + kk
                TR(bank[:, kk * 128:(kk + 1) * 128], HB[:, k * 128:(k + 1) * 128], IDF,
                   ['hb', 'idf'], [('ps', half)])
            pv = bank.rearrange("p (k q) -> p k q", k=4)
            CP('act' if half == 0 else 'dve', HT[:, half * 4:half * 4 + 4, t * 128:(t + 1) * 128], pv,
               [('ps', half)], [('ht', t)])
            if router:
                CP('dve' if half == 0 else 'act', H2F[:, half * 4:half * 4 + 4, :], pv,
                   [('ps', half)], ['h2f'])


    class _Stop(Exception):
        pass
    KSTOP = int(os.environ.get('KSTOP', '0'))

    def ck(n):
        if KSTOP == n:
            raise _Stop()

    def x_src(l):
        return xin if l == 0 else xd

    def kv_tiles(m, t, lastl):
        if t < 2:
            return [(0, None), (1, None)]
        i = t - 2
        if m == 0:
            return [(j + 2, ('A', pid)) for (j, pid) in NAT_PLAN[i]] + [(0, None), (1, None)]
        if m == 2:
            lst = []
            if i - 1 >= 0:
                lst.append((t - 1, ('C', 0)))
            lst.append((t, None))
            if i + 1 < 16:
                lst.append((t + 1, ('C', 1)))
            return lst + [(0, None), (1, None)]
        return [(j, None) for j in range(NT)]

    GQN = SMP[:, 0:64]
    GKN = SMP[:, 64:128]
    MQN = SMP[:, 128:384]
    MKVN = SMP[:, 384:512]

    def evac_scaled(dst, src_ps, pkey, dkey, scale):
        P.op('act', lambda e: e.activation(dst, src_ps, AF.Copy, scale=scale), [pkey], [dkey])

    def transposes_to(dst_fn, src16, nblk, width, skey, dkey, bank_i=1):
        pb = ps[bank_i].bitcast(BF16)
        for b in range(nblk):
            TR(pb[0:width, b * 128:(b + 1) * 128], src16[:, b * width:(b + 1) * width], IDB,
               [skey, 'idb'], [('ps', bank_i)])
        for b in range(nblk):
            CP('act' if b % 2 == 0 else 'dve', dst_fn(b), pb[0:width, b * 128:(b + 1) * 128],
               [('ps', bank_i)], [dkey])

    def proj(bank_i, t, wview, ncols, wkey):
        for k in range(8):
            MM(ps[bank_i][:, 0:ncols], HT[:, k, t * 128:(t + 1) * 128], wview[:, k, 0:ncols], k == 0, k == 7,
               [('ht', t), wkey], [('ps', bank_i)])

    def layer(l):
        lastl = (l == DEPTH - 1)
        q_tiles = list(range(2, NT)) if lastl else list(range(NT))
        DMA(SMP, smallp[l:l + 1, :].partition_broadcast(128), [], ['smp'])
        ACT(SKE, SMP[:, 512:516], AF.Exp, ['smp'], ['ske'])
        mod_tiles(l, 1 * D, 'scale', 0, norm1_g[l:l + 1, :])
        mod_tiles(l, 0 * D, 'shift', 2)
        ck(1)
        for t in range(NT):
            DMA(XT, x_src(l)[t * 128:(t + 1) * 128, :], [], ['xt'])
            norm_tile(t, XT, ['xt'], 0, 2)
        P.barrier()
        ck(2)

        for m in [int(c_) for c_ in os.environ.get('KMIX', '0123')]:
            nh = 4
            nkv = 4 if m in (0, 3) else 2
            hd = 128 if m == 3 else 64
            if m == 0:
                load_w(w_in[l][:, OFF['ak']:OFF['ak'] + 512], 8, 512, WKV, 'wg')
                DMA_bias = True
                for c0 in range(0, NPAT * 4, 16):
                    c1 = min(c0 + 16, NPAT * 4)
                    s_ = stg_rr[0]
                    stg_rr[0] = (s_ + 1) % NSTG
                    sv = STG[:, s_, 0:(c1 - c0) * 128].rearrange("p (n q) -> p n q", q=128)
                    DMA(sv, biasA[l][:, c0:c1, :], [], [('stg', s_)])
                    CP('pool', BIAS[:, c0:c1, :], sv, [('stg', s_)], ['bias'])
                kvw = 512
            elif m == 1:
                load_w(w_in[l][:, OFF['bk']:OFF['bk'] + 256], 8, 256, WKV, 'wg')
                kvw = 256
            elif m == 2:
                load_w(w_in[l][:, OFF['ck']:OFF['ck'] + 256], 8, 256, WKV, 'wg')
                kvw = 256
            else:
                load_w(w_in[l][:, OFF['dkva']:OFF['dkva'] + 160], 8, 160, WKV, 'wg')
                load_w(mla_wkvb[l], 1, 512, WKVB.rearrange("p (k c) -> p k c", k=1), 'wkvb')
                kvw = 160
            if m == 0:
                ck(3)
            P.op('dve', lambda e: e.memset(VA[:, :, :, 64:65], 1.0), [], ['va'])
            for t in range(NT):
                lat = t >= 2
                proj(0, t, WKV, kvw, 'wg')
                CP('act', PBUF[:, 0:kvw], ps[0][:, 0:kvw], [('ps', 0)], ['pbuf'])
                if m == 0:
                    CP('dve', PB16[:, 0:256], PBUF[:, 0:256], ['pbuf'], ['pb16'])
                    CP('pool', VA[:, t, :, 0:64], PBUF[:, 256:512].rearrange("p (h d) -> p h d", h=4), ['pbuf'], ['va'])
                    transposes_to(lambda b: KT[0:64, b, t * 128:(t + 1) * 128], PB16, 4, 64, 'pb16', 'kt')
                elif m in (1, 2):
                    kview = PBUF[:, 0:128].rearrange("p (h d) -> p h d", h=2)
                    if m == 1:
                        head_rms(kview, 2, 64, GKN, 'pbuf')
                    k16 = PB16[:, 0:128].rearrange("p (h d) -> p h d", h=2)
                    if lat:
                        rope(k16, kview, ROPEH, t, 2, 32, 'pbuf', 'pb16')
                    else:
                        CP('dve', k16, kview, ['pbuf'], ['pb16'])
                    CP('pool', VA[:, t, 0:2, 0:64], PBUF[:, 128:256].rearrange("p (h d) -> p h d", h=2), ['pbuf'], ['va'])
                    transposes_to(lambda b: KT[0:64, b, t * 128:(t + 1) * 128], PB16, 2, 64, 'pb16', 'kt')
                else:
                    cview = PBUF[:, 0:128].rearrange("p (h d) -> p h d", h=1)
                    head_rms(cview, 1, 128, MKVN, 'pbuf')
                    CP('dve', PB16[:, 0:128], PBUF[:, 0:128], ['pbuf'], ['pb16'])
                    transposes_to(lambda b: DTT[:, 0, :], PB16, 1, 128, 'pb16', 'dtt')
                    ck(14)
                    MM(ps[2][:, 0:512], DTT[:, 0, :], WKVB, True, True, ['dtt', 'wkvb'], [('ps', 2)])
                    ck(15)
                    kf = PB16[:, 0:512].rearrange("p (h d) -> p h d", h=4)
                    P.op('dve', lambda e: e.memset(PB16[:, 0:512], 0.0), [], ['pb16'])
                    CP('act', PBUF[:, 512:1024], ps[2], [('ps', 2)], ['pbuf2'])
                    dkv = PBUF[:, 512:1024].rearrange("p (h d) -> p h d", h=4)
                    CP('pool', kf[:, :, 0:64], dkv[:, :, 0:64], ['pbuf2'], ['pb16'])
                    CP('pool', VA[:, t, :, 0:64], dkv[:, :, 64:128], ['pbuf2'], ['va'])
                    ck(16)
                    pe_src = PBUF[:, 128:160].rearrange("p (h d) -> p h d", h=1)
                    pe_dst = PBUF[:, 160:192].rearrange("p (h d) -> p h d", h=1)
                    if lat:
                        rope(pe_dst, pe_src, ROPER, t, 1, 16, 'pbuf', 'pbuf')
                    else:
                        CP('dve', pe_dst, pe_src, ['pbuf'], ['pbuf'])
                    ck(17)
                    CP('dve', kf[:, :, 64:96], pe_dst.to_broadcast([128, 4, 32]), ['pbuf'], ['pb16'])
                    ck(18)
                    transposes_to(lambda b: KT[:, b, t * 128:(t + 1) * 128], PB16, 4, 128, 'pb16', 'kt')
            if m == 0:
                ck(4)
            if m == 3:
                ck(10)
            qoff = [OFF['aq'], OFF['bq'], OFF['cq'], OFF['dqa']][m]
            load_w(w_in[l][:, qoff:qoff + 256], 8, 256, WQ, 'wq')
            if m == 3:
                load_w(mla_wqb[l], 2, 384, WQB, 'wqb')
            load_w(w_in[l][:, OFF['gate'] + m * D:OFF['gate'] + (m + 1) * D], 8, D, WG, 'wg')
            load_w(w_branch[l, m], 2, D, WBR, 'wbr')
            for t in q_tiles:
                lat = t >= 2
                proj(0, t, WQ, 256, 'wq')
                qscale = 0.125 if m != 3 else float(96 ** -0.5)
                if m == 3:
                    evac_scaled(PBUF[:, 0:256], ps[0][:, 0:256], ('ps', 0), 'pbuf', 1.0)
                    qv = PBUF[:, 0:256].rearrange("p (h d) -> p h d", h=1)
                    head_rms(qv, 1, 256, MQN, 'pbuf')
                    CP('dve', PB16[:, 0:256], PBUF[:, 0:256], ['pbuf'], ['pb16'])
                    transposes_to(lambda b: DTT[:, b, :], PB16, 2, 128, 'pb16', 'dtt')
                    for k in range(2):
                        MM(ps[2][:, 0:384], DTT[:, k, :], WQB[:, k, :], k == 0, k == 1, ['dtt', 'wqb'], [('ps', 2)])
                    evac_scaled(PBUF[:, 0:384], ps[2][:, 0:384], ('ps', 2), 'pbuf', qscale)
                    q4 = PBUF[:, 0:384].rearrange("p (h d) -> p h d", h=4)
                    q16 = PB16[:, 0:512].rearrange("p (h d) -> p h d", h=4)
                    P.op('dve', lambda e: e.memset(PB16[:, 0:512], 0.0), [], ['pb16'])
                    CP('pool', q16[:, :, 0:64], q4[:, :, 0:64], ['pbuf'], ['pb16'])
                    if lat:
                        rope(q16[:, :, 64:96], q4[:, :, 64:96], ROPER, t, 4, 16, 'pbuf', 'pb16')
                    else:
                        CP('dve', q16[:, :, 64:96], q4[:, :, 64:96], ['pbuf'], ['pb16'])
                    transposes_to(lambda b: QT[:, b, :], PB16, 4, 128, 'pb16', 'qt')
                else:
                    evac_scaled(PBUF[:, 0:256], ps[0][:, 0:256], ('ps', 0), 'pbuf', qscale)
                    q4 = PBUF[:, 0:256].rearrange("p (h d) -> p h d", h=4)
                    q16 = PB16[:, 0:256].rearrange("p (h d) -> p h d", h=4)
                    if m == 1:
                        head_rms(q4, 4, 64, GQN, 'pbuf')
                        P.op('dve', lambda e: e.tensor_scalar(PBUF[:, 0:256], PBUF[:, 0:256], 0.125, None, ALU.mult),
                             ['pbuf'], ['pbuf'])
                    if lat and m in (1, 2):
                        rope(q16, q4, ROPEH, t, 4, 32, 'pbuf', 'pb16')
                    else:
                        CP('dve', q16, q4, ['pbuf'], ['pb16'])
                    transposes_to(lambda b: QT[0:64, b, :], PB16, 4, 64, 'pb16', 'qt')
                kts = kv_tiles(m, t, lastl)
                OPS = ps[4][:, 0:260].rearrange("p (h d) -> p h d", h=4)
                grp_i = 0
                for h in range(4):
                    kvh = h if nkv == 4 else h // 2
                    for g0 in range(0, len(kts), 4):
                        grp = kts[g0:g0 + 4]
                        sb_i = 2 + (grp_i % 2)
                        pt_i = grp_i % 2
                        grp_i += 1
                        Sv = ps[sb_i].rearrange("p (g q) -> p g q", g=4)
                        for gi, (kt, bspec) in enumerate(grp):
                            MM(Sv[:, gi, :], KT[0:hd, kvh, kt * 128:(kt + 1) * 128], QT[0:hd, h, :], True, bspec is None,
                               ['kt', 'qt'], [('ps', sb_i)])
                            if bspec is not None:
                                brhs = BIAS[:, bspec[1] * 4 + h, :] if bspec[0] == 'A' else MASKC[:, bspec[1], :]
                                MM(Sv[:, gi, :], IDB, brhs, False, True, ['idb', 'bias', 'maskc'], [('ps', sb_i)])
                        ng = len(grp)
                        ACT(PT[:, pt_i, 0:ng, :], Sv[:, 0:ng, :], AF.Exp, [('ps', sb_i)], [('pt', pt_i)])
                        for gi, (kt, bspec) in enumerate(grp):
                            first = (g0 == 0 and gi == 0)
                            last = (g0 + gi == len(kts) - 1)
                            MM(OPS[:, h, :], PT[:, pt_i, gi, :], VA[:, kt, kvh, :], first, last,
                               [('pt', pt_i), 'va'], [('ps', 4)])
                den = RS[:, 32:36]
                if m == 2:
                    TT('dve', den, OPS[:, :, 64], SKE, ALU.add, [('ps', 4), 'ske'], ['rs2'])
                else:
                    CP('dve', den, OPS[:, :, 64], [('ps', 4)], ['rs2'])
                RCP(RS[:, 36:40], den, ['rs2'], ['rs2'])
                TT('dve', OB.rearrange("p (h d) -> p h d", h=4), OPS[:, :, 0:64],
                   RS[:, 36:40].rearrange("p (h o) -> p h o", o=1).to_broadcast([128, 4, 64]), ALU.mult,
                   [('ps', 4), 'rs2'], ['ob'])
                transposes_to(lambda b: OT[:, b, :], OB, 2, 128, 'ob', 'ot', bank_i=1)
                for hf in range(2):
                    cs = slice(hf * 512, (hf + 1) * 512)
                    for k in range(8):
                        MM(ps[5], HT[:, k, t * 128:(t + 1) * 128], WG[:, k, cs], k == 0, k == 7,
                           [('ht', t), 'wg'], [('ps', 5)])
                    for k in range(2):
                        MM(ps[6], OT[:, k, :], WBR[:, k, cs], k == 0, k == 1, ['ot', 'wbr'], [('ps', 6)])
                    ACT(SIG[:, cs], ps[5], AF.Sigmoid, [('ps', 5)], ['sig'])
                    TT('dve', MTL[:, cs], ps[6], SIG[:, cs], ALU.mult, [('ps', 6), 'sig'], ['mtl'])
                DMA(md[m, t * 128:(t + 1) * 128, :], MTL, ['mtl'], [('md', m, t)])
                if m == 0 and t == 0:
                    ck(5)
                if m == 3 and t == 0:
                    ck(11)
                if m == 3 and t == 2:
                    ck(12)
            ck(6 + m)
        P.barrier()

        mod_tiles(l, 2 * D, 'gate', 0, which=(0,) if lastl else (0, 1))
        load_w(w_out[l], 8, D, WO, 'wo')
        for t in q_tiles:
            w = 0 if t >= 2 else 1
            DMA(M4, md[:, t * 128:(t + 1) * 128, :].rearrange("m p d -> p m d"),
                [('md', mm_, t) for mm_ in range(4)], ['m4'])
            DMA(XT, x_src(l)[t * 128:(t + 1) * 128, :], [], ['xt'])
            TT('dve', M4[:, 0, :], M4[:, 0, :], M4[:, 1, :], ALU.add, ['m4'], ['m4'])
            TT('pool', M4[:, 2, :], M4[:, 2, :], M4[:, 3, :], ALU.add, ['m4'], ['m4b'])
            TT('dve', M4[:, 0, :], M4[:, 0, :], M4[:, 2, :], ALU.add, ['m4', 'm4b'], ['m4'])
            pb = ps[1].bitcast(BF16)
            for k in range(8):
                TR(pb[:, k * 128:(k + 1) * 128], M4[:, 0, k * 128:(k + 1) * 128], IDB, ['m4', 'idb'], [('ps', 1)])
            CP('act', MT8, pb.rearrange("p (k q) -> p k q", k=8), [('ps', 1)], ['mt8'])
            for hf in range(2):
                cs = slice(hf * 512, (hf + 1) * 512)
                for k in range(8):
                    MM(ps[5 + hf], MT8[:, k, :], WO[:, k, cs], k == 0, k == 7, ['mt8', 'wo'], [('ps', 5 + hf)])
                TT('dve', HB[:, cs], ps[5 + hf], MOD[:, w, cs], ALU.mult, [('ps', 5 + hf), ('mod', w)], ['hb'])
                TT('pool', XS[:, t, cs], HB[:, cs], XT[:, cs], ALU.add, ['hb', 'xt'], [('x', t)])
        P.barrier()
        if dbg == ('xm', l):
            for t in q_tiles:
                DMA(dbg_out[t * 128:(t + 1) * 128, :], XS[:, t, :], [('x', t)], ['dbgo'])
            return True

        moe = (l % 2 == 1)
        mod_tiles(l, 4 * D, 'scale', 0, norm2_g[l:l + 1, :], which=(0,) if lastl else (0, 1))
        mod_tiles(l, 3 * D, 'shift', 2, which=(0,) if lastl else (0, 1))
        if moe:
            DMA(RT, moe_router[l // 2].rearrange("(k p) e -> p k e", p=128), [], ['rt'])
            P.op('dve', lambda e: e.memset(RTB, 0.0), [], ['rtb'])
            CP('dve', RTB[:, :, 0:8], RT, ['rt', 'rtb'], ['rtb'])
        for t in q_tiles:
            norm_tile(t, XS[:, t, :], [('x', t)], 0, 2, router=False)
            if moe:
                lg = ps[7][:, 0:NEXP]
                for k in range(8):
                    MM(ps[7][:, 0:16], HT[:, k, t * 128:(t + 1) * 128], RTB[:, k, :], k == 0, k == 7, [('ht', t), 'rtb'], [('ps', 7)])
                L = RS[:, 0:8]
                CP('dve', L, lg, [('ps', 7)], ['rs'])
                RED(RS[:, 8:9], L, ALU.max, ['rs'], ['rs'])
                TT('dve', RS[:, 16:24], L, RS[:, 8:9].to_broadcast([128, 8]), ALU.is_equal, ['rs'], ['rs'])
                STT(RS[:, 24:32], RS[:, 16:24], -1e30, L, ALU.mult, ALU.add, ['rs'], ['rs'])
                RED(RS[:, 9:10], RS[:, 24:32], ALU.max, ['rs'], ['rs'])
                TT('dve', RS[:, 40:48], RS[:, 24:32], RS[:, 9:10].to_broadcast([128, 8]), ALU.is_equal, ['rs'], ['rs'])
                TT('dve', RS[:, 10:11], RS[:, 9:10], RS[:, 8:9], ALU.subtract, ['rs'], ['rs'])
                ACT(RS[:, 11:12], RS[:, 10:11], AF.Exp, ['rs'], ['rs'])
                TS('dve', RS[:, 12:13], RS[:, 11:12], 1.0, None, ALU.add, None, ['rs'], ['rs'])
                RCP(RS[:, 13:14], RS[:, 12:13], ['rs'], ['rs'])
                TT('dve', RS[:, 14:15], RS[:, 11:12], RS[:, 13:14], ALU.mult, ['rs'], ['rs'])
                TT('dve', RS[:, 48:56], RS[:, 16:24], RS[:, 13:14].to_broadcast([128, 8]), ALU.mult, ['rs'], ['rs'])
                STT(COMB[:, t, :], RS[:, 40:48], RS[:, 14:15], RS[:, 48:56], ALU.mult, ALU.add, ['rs'], ['comb'])
                if t == 0:
                    ck(20)
        mod_tiles(l, 5 * D, 'gate', 0, which=(0,) if lastl else (0, 1))

        chunks = []
        if not lastl:
            chunks.append([0, 1])
        for c in range(4):
            chunks.append([2 + 4 * c + i for i in range(4)])
        nexp = NEXP if moe else 1
        for e_ in range(nexp):
            if moe:
                w1, w3, w2 = moe_w1[l // 2, e_], moe_w3[l // 2, e_], moe_w2[l // 2, e_]
            else:
                w1, w3, w2 = ffn_w1[l // 2], ffn_w3[l // 2], ffn_w2[l // 2]
            for g in range(DFF // 512):
                load_w(w1[:, g * 512:(g + 1) * 512], 8, 512, W1B, 'w1b')
                load_w(w3[:, g * 512:(g + 1) * 512], 8, 512, W3B, 'w3b')
                load_w(w2[g * 512:(g + 1) * 512, :], 4, D, W2B, 'w2b')
                for ch in chunks:
                    n = len(ch) * 128
                    t0 = ch[0] * 128
                    for fc in range(4):
                        ab = fc % 2
                        for k in range(8):
                            MM(ps[ab][:, 0:n], W1B[:, k, fc * 128:(fc + 1) * 128], HT[:, k, t0:t0 + n], k == 0, k == 7,
                               ['w1b'] + [('ht', t) for t in ch], [('ps', ab)])
                        for k in range(8):
                            MM(ps[2 + ab][:, 0:n], W3B[:, k, fc * 128:(fc + 1) * 128], HT[:, k, t0:t0 + n], k == 0, k == 7,
                               ['w3b'] + [('ht', t) for t in ch], [('ps', 2 + ab)])
                        ACT(SA[:, ab, 0:n], ps[ab][:, 0:n], AF.Silu, [('ps', ab)], [('sa', ab)])
                        TT('dve', UT[:, fc, 0:n], ps[2 + ab][:, 0:n], SA[:, ab, 0:n], ALU.mult,
                           [('ps', 2 + ab), ('sa', ab)], [('ut', fc)])
                    for ti, t in enumerate(ch):
                        w = 0 if t >= 2 else 1
                        ob_ = 4 + 2 * (ti % 2)
                        for hf in range(2):
                            cs = slice(hf * 512, (hf + 1) * 512)
                            for fc in range(4):
                                MM(ps[ob_ + hf], UT[:, fc, ti * 128:(ti + 1) * 128], W2B[:, fc, cs], fc == 0, fc == 3,
                                   [('ut', fc), 'w2b'], [('ps', ob_ + hf)])
                            if moe:
                                STT(HB[:, cs], ps[ob_ + hf], COMB[:, t, e_:e_ + 1], MOD[:, w, cs], ALU.mult, ALU.mult,
                                    [('ps', ob_ + hf), 'comb', ('mod', w)], [('hbh', hf)])
                            else:
                                TT('dve', HB[:, cs], ps[ob_ + hf], MOD[:, w, cs], ALU.mult,
                                   [('ps', ob_ + hf), ('mod', w)], [('hbh', hf)])
                            TT('pool', XS[:, t, cs], XS[:, t, cs], HB[:, cs], ALU.add, [('hbh', hf), ('x', t)], [('x', t)])
        P.barrier()
        if dbg == ('xf', l):
            for t in q_tiles:
                DMA(dbg_out[t * 128:(t + 1) * 128, :], XS[:, t, :], [('x', t)], ['dbgo'])
            return True
        if not lastl:
            for t in range(NT):
                DMA(xd[t * 128:(t + 1) * 128, :], XS[:, t, :], [('x', t)], [('xd', t)])
            P.barrier()
        return False

    stopped = False
    try:
        for l in range(n_layers):
            stopped = layer(l)
            if stopped:
                break
    except _Stop:
        stopped = True
    if not stopped and n_layers == DEPTH:
        DMA(MOD[:, 0, :], final_g.partition_broadcast(128), [], [('mod', 0)])
        for t in range(2, NT):
            src = XS[:, t, :]
            ACT(HB, src, AF.Square, [('x', t)], ['hb'], accum_out=STAT[:, 0:1])
            ACT(STAT[:, 1:2], STAT[:, 0:1], AF.Sqrt, ['hb'], ['stat'], bias=EPSB, scale=1.0 / D)
            RCP(STAT[:, 2:3], STAT[:, 1:2], ['stat'], ['stat'])
            STT(HB, src, STAT[:, 2:3], MOD[:, 0, :], ALU.mult, ALU.mult, [('x', t), 'stat', ('mod', 0)], ['hb'])
            DMA(out[(t - 2) * 128:(t - 1) * 128, :], HB, ['hb'], ['out'])
    elif not stopped:
        DMA(out[0:128, :], HB, [], ['out'])
    P.barrier()
    P.emit()
    print("instructions:", P.n, {k: len(v) for k, v in P.streams.items()})


_CACHE = {}


def make_in_maps(inp, n_cores=8):
    f = lambda a: np.ascontiguousarray(np.asarray(a), dtype=np.float32)
    cosH, sinH, cosR, sinR = _rope_tables()
    shared = {
        "norm1_g": f(inp["norm1_g"]), "norm2_g": f(inp["norm2_g"]), "w_ada": f(inp["w_ada"]),
        "b_ada": f(inp["b_ada"]), "w_in": f(inp["w_in"]), "biasA": _natten_bias(f(inp["na_rpb"])),
        "smallp": np.ascontiguousarray(np.concatenate(
            [f(inp["gb_qnorm"]), f(inp["gb_knorm"]), f(inp["mla_qnorm"]), f(inp["mla_kvnorm"]), f(inp["wc_sink"])],
            axis=1)),
        "mla_wqb": f(inp["mla_wqb"]), "mla_wkvb": f(inp["mla_wkvb"]), "w_branch": f(inp["w_branch"]),
        "w_out": f(inp["w_out"]), "ffn_w1": f(inp["ffn_w1"]), "ffn_w3": f(inp["ffn_w3"]), "ffn_w2": f(inp["ffn_w2"]),
        "moe_router": f(inp["moe_router"]), "moe_w1": f(inp["moe_w1"]), "moe_w3": f(inp["moe_w3"]),
        "moe_w2": f(inp["moe_w2"]), "final_g": f(inp["final_g"]).reshape(1, D),
        "c_ident": np.eye(128, dtype=np.float32),
        "c_ropeH": np.ascontiguousarray(np.stack([cosH, sinH])),
        "c_ropeR": np.ascontiguousarray(np.stack([cosR, sinR])),
        "c_maskC": _maskC(),
    }
    x = f(inp["x"]); ctx = f(inp["ctx"]); c = f(inp["c"]); cc = f(inp["c_ctx"])
    maps = []
    for b in range(n_cores):
        m = dict(shared)
        m["xin"] = np.ascontiguousarray(np.concatenate([ctx[b], x[b]], axis=0))
        m["cvec"] = np.ascontiguousarray(np.stack([c[b], cc]))
        maps.append(m)
    return maps


def kernel(**inputs):
    if "nc" not in _CACHE:
        _CACHE["nc"] = build(DEPTH)
    nc = _CACHE["nc"]
    maps = make_in_maps(inputs, 8)
    res = run_bass_kernel_spmd(nc, maps, core_ids=list(range(8)))
    return np.stack([np.asarray(r["out"], dtype=np.float32) for r in res.results], axis=0)
```

```python
import numpy as np
from contextlib import ExitStack
import concourse.bass as bass
import concourse.mybir as mybir
from concourse.bass_utils import run_bass_kernel_spmd

F32 = mybir.dt.float32
BF16 = mybir.dt.bfloat16
AF = mybir.ActivationFunctionType
ALU = mybir.AluOpType
AX = mybir.AxisListType

D = 1024
S = 2048
C = 256
T = S + C
NT = T // 128
DEPTH = 4
IN_W = 6304
DFF = 3584
NEXP = 8
EPS = 1e-6
NEG = -30000.0
NDMA = 12
NSTG = 3

OFF = dict(aq=0, ak=256, av=512, bq=768, bk=1024, bv=1152, cq=1280, ck=1536, cv=1664,
           dqa=1792, dkva=2048, gate=2208)


class Prog:
    CE = ['pe', 'act', 'dve', 'pool']

    def __init__(self, nc, es):
        self.nc = nc
        self.streams = {e: [] for e in self.CE + ['sp']}
        self.sems = {}
        for e in self.CE:
            self.sems[e] = es.enter_context(nc.semaphore("s_" + e))
        for i in range(NDMA):
            self.sems['d%d' % i] = es.enter_context(nc.semaphore("s_d%d" % i))
        self.cnt = {k: 0 for k in self.sems}
        self.known = {e: {k: 0 for k in self.sems} for e in self.streams}
        self.lastw = {}
        self.readers = {}
        self.dma_rr = 0
        self.n = 0
        self.last_real = {e: None for e in self.CE}
        self.pend = {e: False for e in self.CE}

    def _materialize(self, s):
        if s in self.pend and self.pend[s]:
            self.streams[s][self.last_real[s]][2] = s
            self.cnt[s] += 1
            self.pend[s] = False

    def _deps(self, eng, reads, writes):
        deps = {}

        def add(d):
            if d is not None:
                if d[0] == 'pe' and eng == 'pe':
                    return
                if deps.get(d[0], 0) < d[1]:
                    deps[d[0]] = d[1]
        for k in reads:
            add(self.lastw.get(k))
        for k in writes:
            add(self.lastw.get(k))
            for r in self.readers.get(k, ()):
                add(r)
        waits = []
        kn = self.known[eng]
        for s, i in deps.items():
            if kn[s] < i:
                if i > self.cnt[s]:
                    self._materialize(s)
                assert i <= self.cnt[s], (s, i, self.cnt[s])
                kn[s] = i
                waits.append((s, i))
        return waits

    def _commit(self, tok, reads, writes):
        for k in writes:
            self.lastw[k] = tok
            self.readers[k] = []
        for k in reads:
            self.readers.setdefault(k, []).append(tok)

    def op(self, eng, fn, reads=(), writes=()):
        waits = self._deps(eng, reads, writes)
        tok = (eng, self.cnt[eng] + 1)
        self.streams[eng].append([waits, fn, None])
        self.last_real[eng] = len(self.streams[eng]) - 1
        self.pend[eng] = True
        self._commit(tok, reads, writes)
        self.n += 1

    def dma(self, fn, reads=(), writes=()):
        s = 'd%d' % self.dma_rr
        self.dma_rr = (self.dma_rr + 1) % NDMA
        waits = self._deps('sp', reads, writes)
        if self.known['sp'][s] < self.cnt[s]:
            self.known['sp'][s] = self.cnt[s]
            waits.append((s, self.cnt[s]))
        self.cnt[s] += 1
        tok = (s, self.cnt[s])
        self.streams['sp'].append([waits, fn, s])
        self._commit(tok, reads, writes)
        self.n += 1

    def barrier(self):
        for e in self.CE:
            self._materialize(e)
        snap = dict(self.cnt)
        for e in self.streams:
            waits = []
            for s, i in snap.items():
                if self.known[e][s] < i:
                    self.known[e][s] = i
                    waits.append((s, i))
            if waits:
                self.streams[e].append([waits, None, None])

    def emit(self):
        nc = self.nc
        sems = self.sems

        def mult(s):
            return 16 if s[0] == 'd' else 1

        def run(engine, stream):
            for waits, fn, inc in stream:
                for s, i in waits:
                    engine.wait_ge(sems[s], i * mult(s))
                if fn is None:
                    continue
                ins = fn(engine)
                if inc is not None:
                    ins.then_inc(sems[inc], mult(inc))

        with nc.Block() as block:
            @block.tensor
            def _(e):
                run(e, self.streams['pe'])

            @block.scalar
            def _(e):
                run(e, self.streams['act'])

            @block.vector
            def _(e):
                run(e, self.streams['dve'])

            @block.gpsimd
            def _(e):
                run(e, self.streams['pool'])

            @block.sync
            def _(e):
                run(e, self.streams['sp'])


def _rope_tables():
    def ang(pos, dim):
        inv = (10000.0 ** (-np.arange(0, dim, 2, dtype=np.float32) / dim)).astype(np.float32)
        return pos.astype(np.float32)[:, None] * inv[None, :]
    t = np.arange(S)
    out = []
    for rot in (64, 32):
        half = rot // 2
        a = np.concatenate([ang(t // 64, half), ang(t % 64, half)], axis=-1).astype(np.float32)
        out += [np.cos(a).astype(np.float32), np.sin(a).astype(np.float32)]
    return out


def _natten_plan():
    rows = S // 64
    kh = 8
    pats = {}
    plan = []
    for i in range(16):
        lst = []
        for j in range(16):
            sig = []
            anyv = False
            for a in range(2):
                for b in range(2):
                    r = 2 * i + b
                    rk = 2 * j + a
                    st = min(max(r - kh // 2, 0), rows - kh)
                    if st <= rk < st + kh:
                        sig.append(rk - r + 7)
                        anyv = True
                    else:
                        sig.append(-1)
            if anyv:
                sig = tuple(sig)
                if sig not in pats:
                    pats[sig] = len(pats)
                lst.append((j, pats[sig]))
        plan.append(lst)
    return plan, pats


NAT_PLAN, NAT_PATS = _natten_plan()
NPAT = len(NAT_PATS)


def _natten_bias(rpb):
    col = np.arange(64)
    cstart = np.clip(col - 8, 0, 48)
    col_ok = (col[None, :] >= cstart[:, None]) & (col[None, :] < cstart[:, None] + 16)
    dc = np.clip(col[None, :] - col[:, None] + 15, 0, 30)
    out = np.full((DEPTH, 128, NPAT * 4, 128), NEG, dtype=np.float32)
    for sig, pid in NAT_PATS.items():
        for a in range(2):
            for b in range(2):
                dr = sig[a * 2 + b]
                if dr < 0:
                    continue
                g = rpb[:, :, dr, :][:, :, dc]
                g = np.where(col_ok[None, None], g, np.float32(NEG))
                g = np.transpose(g, (0, 3, 1, 2))
                out[:, a * 64:(a + 1) * 64, pid * 4:(pid + 1) * 4, b * 64:(b + 1) * 64] = g
    return out


def _maskC():
    kk = np.arange(128)[:, None]
    qq = np.arange(128)[None, :]
    m0 = np.where(qq <= kk, 0.0, NEG).astype(np.float32)
    m1 = np.where(kk <= qq, 0.0, NEG).astype(np.float32)
    return np.stack([m0, m1], axis=1)


def build(n_layers=DEPTH, dbg=None):
    nc = bass.Bass("TRN2", target_bir_lowering=False, dynamic_dma_scratch_size=256)
    es = ExitStack()
    with es:
        _build_body(nc, es, n_layers, dbg)
    return nc


def _build_body(nc, es, n_layers, dbg):
    def din(name, shape):
        return nc.dram_tensor(name, list(shape), F32, kind="ExternalInput").ap()

    xin = din("xin", [T, D])
    cvec = din("cvec", [2, D])
    norm1_g = din("norm1_g", [DEPTH, D])
    norm2_g = din("norm2_g", [DEPTH, D])
    w_ada = din("w_ada", [DEPTH, D, 6 * D])
    b_ada = din("b_ada", [DEPTH, 6 * D])
    w_in = din("w_in", [DEPTH, D, IN_W])
    biasA = din("biasA", [DEPTH, 128, NPAT * 4, 128])
    smallp = din("smallp", [DEPTH, 516])
    mla_wqb = din("mla_wqb", [DEPTH, 256, 384])
    mla_wkvb = din("mla_wkvb", [DEPTH, 128, 512])
    w_branch = din("w_branch", [DEPTH, 4, 256, D])
    w_out = din("w_out", [DEPTH, D, D])
    ffn_w1 = din("ffn_w1", [2, D, DFF])
    ffn_w3 = din("ffn_w3", [2, D, DFF])
    ffn_w2 = din("ffn_w2", [2, DFF, D])
    moe_router = din("moe_router", [2, D, NEXP])
    moe_w1 = din("moe_w1", [2, NEXP, D, DFF])
    moe_w3 = din("moe_w3", [2, NEXP, D, DFF])
    moe_w2 = din("moe_w2", [2, NEXP, DFF, D])
    final_g = din("final_g", [1, D])
    c_ident = din("c_ident", [128, 128])
    c_ropeH = din("c_ropeH", [2, S, 32])
    c_ropeR = din("c_ropeR", [2, S, 16])
    c_maskC = din("c_maskC", [128, 2, 128])
    out = nc.dram_tensor("out", [S, D], F32, kind="ExternalOutput").ap()
    xd = nc.dram_tensor("xd", [T, D], F32, kind="Internal").ap()
    md = nc.dram_tensor("md", [4, T, D], BF16, kind="Internal").ap()
    dbg_out = None
    if dbg is not None:
        dbg_out = nc.dram_tensor("dbg", [T, D], F32, kind="ExternalOutput").ap()

    P = Prog(nc, es)

    def sb(name, shape, dt):
        return es.enter_context(nc.sbuf_tensor(name, list(shape), dt))[:]

    ps = [es.enter_context(nc.psum_tensor("ps%d" % i, [128, 512], F32))[:] for i in range(8)]

    IDF = sb("idf", [128, 128], F32)
    IDB = sb("idb", [128, 128], BF16)
    ROPEH = sb("ropeh", [128, 2, 16, 32], F32)
    ROPER = sb("roper", [128, 2, 16, 16], F32)
    MASKC = sb("maskc", [128, 2, 128], BF16)
    CV = sb("cv", [128, 2, 8], F32)
    SL = sb("sl", [128, 2, 8, 128], F32)
    MOD = sb("mod", [128, 4, D], F32)
    SMP = sb("smp", [128, 516], F32)
    SKE = sb("ske", [128, 4], F32)
    HT = sb("ht", [128, 8, T], BF16)
    STG = sb("stg", [128, NSTG, 2048], F32)
    XT = sb("xt", [128, D], F32)
    HB = sb("hb", [128, D], F32)
    PB = HB
    STAT = sb("stat", [128, 32], F32)
    PBUF = sb("pbuf", [128, D], F32)
    TMPF = sb("tmpf", [128, 4, 256], F32)
    PB16 = sb("pb16", [128, 512], BF16)
    QT = sb("qt", [128, 4, 128], BF16)
    OB = sb("ob", [128, 256], BF16)
    OT = sb("ot", [128, 2, 128], BF16)
    DTT = sb("dtt", [128, 2, 128], BF16)
    MT8 = sb("mt8", [128, 8, 128], BF16)
    RT = sb("rt", [128, 8, NEXP], F32)
    RTB = sb("rtb", [128, 8, 16], BF16)
    COMB = sb("comb", [128, NT, NEXP], F32)
    RS = sb("rs", [128, 64], F32)
    UT = sb("ut", [128, 4, 512], BF16)
    SA = sb("sa", [128, 2, 512], BF16)
    R4 = sb("r4", [128, 12288], BF16)
    R2 = sb("r2", [128, 36864], BF16)

    KT = R2[:, 0:9216].rearrange("p (h t) -> p h t", h=4)
    VA = R2[:, 9216:13896].rearrange("p (t h d) -> p t h d", t=NT, h=4)
    BIAS = R2[:, 13896:13896 + NPAT * 4 * 128].rearrange("p (n q) -> p n q", q=128)
    XS = R2.bitcast(F32).rearrange("p (t d) -> p t d", t=NT)
    b0 = 13896 + NPAT * 4 * 128
    WG = R2[:, b0:b0 + 8192].rearrange("p (k c) -> p k c", k=8)
    WKV = WG
    WQ = R2[:, b0 + 8192:b0 + 10240].rearrange("p (k c) -> p k c", k=8)
    WBR = R2[:, b0 + 10240:b0 + 12288].rearrange("p (k c) -> p k c", k=2)
    WQB = R2[:, b0 + 12288:b0 + 13056].rearrange("p (k c) -> p k c", k=2)
    WKVB = R2[:, b0 + 13056:b0 + 13568]
    SIG = R2[:, b0 + 13568:b0 + 14592]
    MTL = R2[:, b0 + 14592:b0 + 15616]
    PT = R2[:, b0 + 15616:b0 + 16640].rearrange("p (a g q) -> p a g q", a=2, g=4)
    assert b0 + 16640 <= 36864
    WO = R4[:, 0:8192].rearrange("p (k c) -> p k c", k=8)
    M4 = MOD[:, 2:4, :].rearrange("p a d -> p (a d)").bitcast(BF16).rearrange("p (a d) -> p a d", a=4)
    H2F = PBUF.rearrange("p (k q) -> p k q", k=8)
    W1B = R4[:, 0:4096].rearrange("p (k c) -> p k c", k=8)
    W3B = R4[:, 4096:8192].rearrange("p (k c) -> p k c", k=8)
    W2B = R4[:, 8192:12288].rearrange("p (k c) -> p k c", k=4)

    def MM(o, lhsT, rhs, start, stop, r, w):
        P.op('pe', lambda e: e.matmul(o, lhsT, rhs, start=start, stop=stop), r, w)

    def TR(o, i, ident, r, w):
        P.op('pe', lambda e: e.transpose(o, i, ident), r, w)

    def ACT(o, i, func, r, w, **kw):
        P.op('act', lambda e: e.activation(o, i, func, **kw), r, w)

    def TT(eng, o, a, b, op, r, w):
        P.op(eng, lambda e: e.tensor_tensor(o, a, b, op), r, w)

    def TS(eng, o, a, s1, s2, op0, op1, r, w):
        if op1 is None:
            P.op(eng, lambda e: e.tensor_scalar(o, a, s1, None, op0), r, w)
        else:
            P.op(eng, lambda e: e.tensor_scalar(o, a, s1, s2, op0, op1), r, w)

    def STT(o, a, s, b, op0, op1, r, w):
        P.op('dve', lambda e: e.scalar_tensor_tensor(o, a, s, b, op0, op1), r, w)

    def CP(eng, o, i, r, w):
        if eng == 'act':
            P.op('act', lambda e: e.activation(o, i, AF.Copy), r, w)
        else:
            P.op(eng, lambda e: e.tensor_copy(o, i), r, w)

    def RED(o, i, op, r, w):
        P.op('dve', lambda e: e.tensor_reduce(o, i, AX.X, op), r, w)

    def RCP(o, i, r, w):
        P.op('dve', lambda e: e.reciprocal(o, i), r, w)

    def DMA(o, i, r, w):
        P.dma(lambda e: e.dma_start(out=o, in_=i), r, w)

    stg_rr = [0]

    def load_w(src, kc, ncols, dst, dkey, cast=True, use=None):
        pcw = 2048 // kc
        c0 = 0
        while c0 < ncols:
            cw = min(pcw, ncols - c0)
            s = stg_rr[0]
            stg_rr[0] = (s + 1) % NSTG
            sv = STG[:, s, 0:kc * cw].rearrange("p (k c) -> p k c", k=kc)
            DMA(sv, src[:, c0:c0 + cw].rearrange("(k p) c -> p k c", p=128), [], [('stg', s)])
            if cast:
                CP('pool', dst[:, :, c0:c0 + cw], sv, [('stg', s)], [dkey])
            else:
                use(sv, c0, cw, ('stg', s))
            c0 += cw

    import os
    KS = int(os.environ.get("KSETUP", "99"))
    if KS > 0:
        DMA(IDF, c_ident, [], ['idf'])
        CP('dve', IDB, IDF, ['idf'], ['idb'])
    if KS > 1:
        DMA(ROPEH[:, 0], c_ropeH[0].rearrange("(t p) d -> p t d", p=128), [], ['rope'])
        DMA(ROPEH[:, 1], c_ropeH[1].rearrange("(t p) d -> p t d", p=128), [], ['rope'])
        DMA(ROPER[:, 0], c_ropeR[0].rearrange("(t p) d -> p t d", p=128), [], ['rope'])
        DMA(ROPER[:, 1], c_ropeR[1].rearrange("(t p) d -> p t d", p=128), [], ['rope'])
    if KS > 2:
        DMA(PBUF[:, 0:256].rearrange("p (a q) -> p a q", a=2), c_maskC, [], ['pbuf'])
        CP('dve', MASKC, PBUF[:, 0:256].rearrange("p (a q) -> p a q", a=2), ['pbuf'], ['maskc'])
    if KS > 3:
        P.dma(lambda e: e.dma_start(out=CV, in_=cvec.rearrange("w (k p) -> p w k", p=128),
                                    allow_slow_non_contiguous=True), [], ['cv'])
    if KS > 4:
        ACT(CV, CV, AF.Silu, ['cv'], ['cv'])
    if KS > 5:
        for wch in range(2):
            CP('dve', SL[:, wch], CV[:, wch, :].rearrange("p (k o) -> p k o", o=1).to_broadcast([128, 8, 128]),
               ['cv'], ['sl'])

    def rope_view(tab, which, t, nh, half):
        return tab[:, which, t - 2, :].rearrange("p (o d) -> p o d", o=1).to_broadcast([128, nh, half])

    def rope(dst, src, tab, t, nh, half, skey, dkey):
        c = rope_view(tab, 0, t, nh, half)
        s = rope_view(tab, 1, t, nh, half)
        x1 = src[:, :, 0:half]
        x2 = src[:, :, half:2 * half]
        t1 = TMPF[:, 0, 0:nh * half].rearrange("p (h d) -> p h d", h=nh)
        t2 = TMPF[:, 1, 0:nh * half].rearrange("p (h d) -> p h d", h=nh)
        t3 = TMPF[:, 2, 0:nh * half].rearrange("p (h d) -> p h d", h=nh)
        t4 = TMPF[:, 3, 0:nh * half].rearrange("p (h d) -> p h d", h=nh)
        TT('dve', t1, x1, c, ALU.mult, [skey, 'rope'], ['tf0'])
        TT('pool', t2, x2, s, ALU.mult, [skey, 'rope'], ['tf1'])
        TT('dve', t3, x1, s, ALU.mult, [skey, 'rope'], ['tf2'])
        TT('pool', t4, x2, c, ALU.mult, [skey, 'rope'], ['tf3'])
        TT('dve', dst[:, :, 0:half], t1, t2, ALU.subtract, ['tf0', 'tf1'], [dkey])
        TT('dve', dst[:, :, half:2 * half], t3, t4, ALU.add, ['tf2', 'tf3'], [dkey])

    def head_rms(src, nh, hd, gain, skey):
        sq = TMPF[:, 0:1, :].rearrange("p a c -> p (a c)")[:, 0:nh * hd].rearrange("p (h d) -> p h d", h=nh)
        TT('dve', sq, src, src, ALU.mult, [skey], ['tf0'])
        RED(RS[:, 0:nh], sq, ALU.add, ['tf0'], ['rs'])
        ACT(RS[:, 8:8 + nh], RS[:, 0:nh], AF.Sqrt, ['rs'], ['rs'], bias=EPSB, scale=1.0 / hd)
        RCP(RS[:, 16:16 + nh], RS[:, 8:8 + nh], ['rs'], ['rs'])
        TT('dve', src, src, RS[:, 16:16 + nh].rearrange("p (h o) -> p h o", o=1).to_broadcast([128, nh, hd]),
           ALU.mult, [skey, 'rs'], [skey])
        TT('dve', src, src, gain.rearrange("p (o d) -> p o d", o=1).to_broadcast([128, nh, hd]),
           ALU.mult, [skey, 'smp'], [skey])

    EPSB = sb("epsb", [128, 1], F32)
    P.op('dve', lambda e: e.memset(EPSB, EPS), [], ['epsb'])

    def mod_tiles(l, col0, kind, slot, gain_src=None, which=(0, 1)):
        DMA(PB, b_ada[l:l + 1, col0:col0 + D].partition_broadcast(128), [], ['hb'])

        def use(sv, c0, cw, skey):
            for w in which:
                bank = ps[6 + w]
                for k in range(8):
                    MM(bank[:, 0:cw], SL[:, w, k, :], sv[:, k, :], k == 0, k == 7,
                       ['sl', skey], [('ps', 6 + w)])
                TT('dve', MOD[:, slot + w, c0:c0 + cw], bank[:, 0:cw], PB[:, c0:c0 + cw], ALU.add,
                   [('ps', 6 + w), 'hb'], [('mod', slot + w)])
        load_w(w_ada[l][:, col0:col0 + D], 8, D, None, None, cast=False, use=use)
        if kind == 'scale':
            DMA(PB, gain_src.partition_broadcast(128), [], ['hb'])
            for w in which:
                STT(MOD[:, slot + w], MOD[:, slot + w], 1.0, PB, ALU.add, ALU.mult,
                    [('mod', slot + w), 'hb'], [('mod', slot + w)])

    def norm_tile(t, src_ap, src_keys, gslot, sslot, router=False, xkey=None):
        w = 0 if t >= 2 else 1
        ACT(HB, src_ap, AF.Square, src_keys, ['hb'], accum_out=STAT[:, 0:1])
        ACT(STAT[:, 1:2], STAT[:, 0:1], AF.Sqrt, ['hb'], ['stat'], bias=EPSB, scale=1.0 / D)
        RCP(STAT[:, 2:3], STAT[:, 1:2], ['stat'], ['stat'])
        STT(HB, src_ap, STAT[:, 2:3], MOD[:, gslot + w], ALU.mult, ALU.mult,
            src_keys + ['stat', ('mod', gslot + w)], ['hb'])
        TT('dve', HB, HB, MOD[:, sslot + w], ALU.add, ['hb', ('mod', sslot + w)], ['hb'])
        for half in range(2):
            bank = ps[half]
            for kk in range(4):
                k = half * 4 + kk
                TR(bank[:, kk * 128:(kk + 1) * 128], HB[:, k * 128:(k + 1) * 128], IDF,
                   ['hb', 'idf'], [('ps', half)])
            pv = bank.rearrange("p (k q) -> p k q", k=4)
            CP('act' if half == 0 else 'dve', HT[:, half * 4:half * 4 + 4, t * 128:(t + 1) * 128], pv,
               [('ps', half)], [('ht', t)])
            if router:
                CP('dve' if half == 0 else 'act', H2F[:, half * 4:half * 4 + 4, :], pv,
                   [('ps', half)], ['h2f'])


    class _Stop(Exception):
        pass
    KSTOP = int(os.environ.get('KSTOP', '0'))

    def ck(n):
        if KSTOP == n:
            raise _Stop()

    def x_src(l):
        return xin if l == 0 else xd

    def kv_tiles(m, t, lastl):
        if t < 2:
            return [(0, None), (1, None)]
        i = t - 2
        if m == 0:
            return [(j + 2, ('A', pid)) for (j, pid) in NAT_PLAN[i]] + [(0, None), (1, None)]
        if m == 2:
            lst = []
            if i - 1 >= 0:
                lst.append((t - 1, ('C', 0)))
            lst.append((t, None))
            if i + 1 < 16:
                lst.append((t + 1, ('C', 1)))
            return lst + [(0, None), (1, None)]
        return [(j, None) for j in range(NT)]

    GQN = SMP[:, 0:64]
    GKN = SMP[:, 64:128]
    MQN = SMP[:, 128:384]
    MKVN = SMP[:, 384:512]

    def evac_scaled(dst, src_ps, pkey, dkey, scale):
        P.op('act', lambda e: e.activation(dst, src_ps, AF.Copy, scale=scale), [pkey], [dkey])

    def transposes_to(dst_fn, src16, nblk, width, skey, dkey, bank_i=1):
        pb = ps[bank_i].bitcast(BF16)
        for b in range(nblk):
            TR(pb[0:width, b * 128:(b + 1) * 128], src16[:, b * width:(b + 1) * width], IDB,
               [skey, 'idb'], [('ps', bank_i)])
        for b in range(nblk):
            CP('act' if b % 2 == 0 else 'dve', dst_fn(b), pb[0:width, b * 128:(b + 1) * 128],
               [('ps', bank_i)], [dkey])

    def proj(bank_i, t, wview, ncols, wkey):
        for k in range(8):
            MM(ps[bank_i][:, 0:ncols], HT[:, k, t * 128:(t + 1) * 128], wview[:, k, 0:ncols], k == 0, k == 7,
               [('ht', t), wkey], [('ps', bank_i)])

    att_i = [0]

    def layer(l):
        lastl = (l == DEPTH - 1)
        q_tiles = list(range(2, NT)) if lastl else list(range(NT))
        DMA(SMP, smallp[l:l + 1, :].partition_broadcast(128), [], ['smp'])
        ACT(SKE, SMP[:, 512:516], AF.Exp, ['smp'], ['ske'])
        mod_tiles(l, 1 * D, 'scale', 0, norm1_g[l:l + 1, :])
        mod_tiles(l, 0 * D, 'shift', 2)
        ck(1)
        for t in range(NT):
            DMA(XT, x_src(l)[t * 128:(t + 1) * 128, :], [], ['xt'])
            norm_tile(t, XT, ['xt'], 0, 2)
        P.barrier()
        ck(2)

        for m in [int(c_) for c_ in os.environ.get('KMIX', '0123')]:
            nh = 4
            nkv = 4 if m in (0, 3) else 2
            hd = 128 if m == 3 else 64
            if m == 0:
                load_w(w_in[l][:, OFF['ak']:OFF['ak'] + 512], 8, 512, WKV, 'wg')
                DMA_bias = True
                for c0 in range(0, NPAT * 4, 16):
                    c1 = min(c0 + 16, NPAT * 4)
                    s_ = stg_rr[0]
                    stg_rr[0] = (s_ + 1) % NSTG
                    sv = STG[:, s_, 0:(c1 - c0) * 128].rearrange("p (n q) -> p n q", q=128)
                    DMA(sv, biasA[l][:, c0:c1, :], [], [('stg', s_)])
                    CP('pool', BIAS[:, c0:c1, :], sv, [('stg', s_)], ['bias'])
                kvw = 512
            elif m == 1:
                load_w(w_in[l][:, OFF['bk']:OFF['bk'] + 256], 8, 256, WKV, 'wg')
                kvw = 256
            elif m == 2:
                load_w(w_in[l][:, OFF['ck']:OFF['ck'] + 256], 8, 256, WKV, 'wg')
                kvw = 256
            else:
                load_w(w_in[l][:, OFF['dkva']:OFF['dkva'] + 160], 8, 160, WKV, 'wg')
                load_w(mla_wkvb[l], 1, 512, WKVB.rearrange("p (k c) -> p k c", k=1), 'wkvb')
                kvw = 160
            if m == 0:
                ck(3)
            P.op('dve', lambda e: e.memset(VA[:, :, :, 64:65], 1.0), [], ['va'])
            for t in range(NT):
                lat = t >= 2
                proj(0, t, WKV, kvw, 'wg')
                CP('act', PBUF[:, 0:kvw], ps[0][:, 0:kvw], [('ps', 0)], ['pbuf'])
                if m == 0:
                    CP('dve', PB16[:, 0:256], PBUF[:, 0:256], ['pbuf'], ['pb16'])
                    CP('pool', VA[:, t, :, 0:64], PBUF[:, 256:512].rearrange("p (h d) -> p h d", h=4), ['pbuf'], ['va'])
                    transposes_to(lambda b: KT[0:64, b, t * 128:(t + 1) * 128], PB16, 4, 64, 'pb16', 'kt')
                elif m in (1, 2):
                    kview = PBUF[:, 0:128].rearrange("p (h d) -> p h d", h=2)
                    if m == 1:
                        head_rms(kview, 2, 64, GKN, 'pbuf')
                    k16 = PB16[:, 0:128].rearrange("p (h d) -> p h d", h=2)
                    if lat:
                        rope(k16, kview, ROPEH, t, 2, 32, 'pbuf', 'pb16')
                    else:
                        CP('dve', k16, kview, ['pbuf'], ['pb16'])
                    CP('pool', VA[:, t, 0:2, 0:64], PBUF[:, 128:256].rearrange("p (h d) -> p h d", h=2), ['pbuf'], ['va'])
                    transposes_to(lambda b: KT[0:64, b, t * 128:(t + 1) * 128], PB16, 2, 64, 'pb16', 'kt')
                else:
                    cview = PBUF[:, 0:128].rearrange("p (h d) -> p h d", h=1)
                    head_rms(cview, 1, 128, MKVN, 'pbuf')
                    CP('dve', PB16[:, 0:128], PBUF[:, 0:128], ['pbuf'], ['pb16'])
                    transposes_to(lambda b: DTT[:, 0, :], PB16, 1, 128, 'pb16', 'dtt')
                    ck(14)
                    MM(ps[2][:, 0:512], DTT[:, 0, :], WKVB, True, True, ['dtt', 'wkvb'], [('ps', 2)])
                    ck(15)
                    kf = PB16[:, 0:512].rearrange("p (h d) -> p h d", h=4)
                    P.op('dve', lambda e: e.memset(PB16[:, 0:512], 0.0), [], ['pb16'])
                    CP('act', PBUF[:, 512:1024], ps[2], [('ps', 2)], ['pbuf2'])
                    dkv = PBUF[:, 512:1024].rearrange("p (h d) -> p h d", h=4)
                    CP('pool', kf[:, :, 0:64], dkv[:, :, 0:64], ['pbuf2'], ['pb16'])
                    CP('pool', VA[:, t, :, 0:64], dkv[:, :, 64:128], ['pbuf2'], ['va'])
                    ck(16)
                    pe_src = PBUF[:, 128:160].rearrange("p (h d) -> p h d", h=1)
                    pe_dst = PBUF[:, 160:192].rearrange("p (h d) -> p h d", h=1)
                    if lat:
                        rope(pe_dst, pe_src, ROPER, t, 1, 16, 'pbuf', 'pbuf')
                    else:
                        CP('dve', pe_dst, pe_src, ['pbuf'], ['pbuf'])
                    ck(17)
                    CP('dve', kf[:, :, 64:96], pe_dst.to_broadcast([128, 4, 32]), ['pbuf'], ['pb16'])
                    ck(18)
                    transposes_to(lambda b: KT[:, b, t * 128:(t + 1) * 128], PB16, 4, 128, 'pb16', 'kt')
            if m == 0:
                ck(4)
            if m == 3:
                ck(10)
            qoff = [OFF['aq'], OFF['bq'], OFF['cq'], OFF['dqa']][m]
            load_w(w_in[l][:, qoff:qoff + 256], 8, 256, WQ, 'wq')
            if m == 3:
                load_w(mla_wqb[l], 2, 384, WQB, 'wqb')
            load_w(w_in[l][:, OFF['gate'] + m * D:OFF['gate'] + (m + 1) * D], 8, D, WG, 'wg')
            load_w(w_branch[l, m], 2, D, WBR, 'wbr')
            for t in q_tiles:
                lat = t >= 2
                proj(0, t, WQ, 256, 'wq')
                qscale = 0.125 if m != 3 else float(96 ** -0.5)
                if m == 3:
                    evac_scaled(PBUF[:, 0:256], ps[0][:, 0:256], ('ps', 0), 'pbuf', 1.0)
                    qv = PBUF[:, 0:256].rearrange("p (h d) -> p h d", h=1)
                    head_rms(qv, 1, 256, MQN, 'pbuf')
                    CP('dve', PB16[:, 0:256], PBUF[:, 0:256], ['pbuf'], ['pb16'])
                    transposes_to(lambda b: DTT[:, b, :], PB16, 2, 128, 'pb16', 'dtt')
                    for k in range(2):
                        MM(ps[2][:, 0:384], DTT[:, k, :], WQB[:, k, :], k == 0, k == 1, ['dtt', 'wqb'], [('ps', 2)])
                    evac_scaled(PBUF[:, 0:384], ps[2][:, 0:384], ('ps', 2), 'pbuf', qscale)
                    q4 = PBUF[:, 0:384].rearrange("p (h d) -> p h d", h=4)
                    q16 = PB16[:, 0:512].rearrange("p (h d) -> p h d", h=4)
                    P.op('dve', lambda e: e.memset(PB16[:, 0:512], 0.0), [], ['pb16'])
                    CP('pool', q16[:, :, 0:64], q4[:, :, 0:64], ['pbuf'], ['pb16'])
                    if lat:
                        rope(q16[:, :, 64:96], q4[:, :, 64:96], ROPER, t, 4, 16, 'pbuf', 'pb16')
                    else:
                        CP('dve', q16[:, :, 64:96], q4[:, :, 64:96], ['pbuf'], ['pb16'])
                    transposes_to(lambda b: QT[:, b, :], PB16, 4, 128, 'pb16', 'qt')
                else:
                    evac_scaled(PBUF[:, 0:256], ps[0][:, 0:256], ('ps', 0), 'pbuf', qscale)
                    q4 = PBUF[:, 0:256].rearrange("p (h d) -> p h d", h=4)
                    q16 = PB16[:, 0:256].rearrange("p (h d) -> p h d", h=4)
                    if m == 1:
                        head_rms(q4, 4, 64, GQN, 'pbuf')
                        P.op('dve', lambda e: e.tensor_scalar(PBUF[:, 0:256], PBUF[:, 0:256], 0.125, None, ALU.mult),
                             ['pbuf'], ['pbuf'])
                    if lat and m in (1, 2):
                        rope(q16, q4, ROPEH, t, 4, 32, 'pbuf', 'pb16')
                    else:
                        CP('dve', q16, q4, ['pbuf'], ['pb16'])
                    transposes_to(lambda b: QT[0:64, b, :], PB16, 4, 64, 'pb16', 'qt')
                kts = kv_tiles(m, t, lastl)
                o_i = 4 if (att_i[0] % 2 == 0) else 7
                att_i[0] += 1
                OPS = ps[o_i][:, 0:260].rearrange("p (h d) -> p h d", h=4)
                items = []
                for h in range(4):
                    for g0 in range(0, len(kts), 4):
                        items.append((h, g0, kts[g0:g0 + 4]))

                def emit_qk(ix):
                    h, g0, grp = items[ix]
                    kvh = h if nkv == 4 else h // 2
                    sb_i = 2 + (ix % 2)
                    Sv = ps[sb_i].rearrange("p (g q) -> p g q", g=4)
                    for gi_, (kt, bspec) in enumerate(grp):
                        MM(Sv[:, gi_, :], KT[0:hd, kvh, kt * 128:(kt + 1) * 128], QT[0:hd, h, :], True, bspec is None,
                           ['kt', 'qt'], [('ps', sb_i)])
                        if bspec is not None:
                            brhs = BIAS[:, bspec[1] * 4 + h, :] if bspec[0] == 'A' else MASKC[:, bspec[1], :]
                            MM(Sv[:, gi_, :], IDB, brhs, False, True, ['idb', 'bias', 'maskc'], [('ps', sb_i)])

                def emit_exp_pv(ix):
                    h, g0, grp = items[ix]
                    kvh = h if nkv == 4 else h // 2
                    sb_i = 2 + (ix % 2)
                    pt_i = ix % 2
                    Sv = ps[sb_i].rearrange("p (g q) -> p g q", g=4)
                    ng = len(grp)
                    ACT(PT[:, pt_i, 0:ng, :], Sv[:, 0:ng, :], AF.Exp, [('ps', sb_i)], [('pt', pt_i)])
                    for gi_, (kt, bspec) in enumerate(grp):
                        first = (g0 == 0 and gi_ == 0)
                        last = (g0 + gi_ == len(kts) - 1)
                        MM(OPS[:, h, :], PT[:, pt_i, gi_, :], VA[:, kt, kvh, :], first, last,
                           [('pt', pt_i), 'va'], [('ps', o_i)])

                emit_qk(0)
                for ix in range(len(items)):
                    if ix + 1 < len(items):
                        emit_qk(ix + 1)
                    emit_exp_pv(ix)
                den = RS[:, 32:36]
                if m == 2:
                    TT('dve', den, OPS[:, :, 64], SKE, ALU.add, [('ps', o_i), 'ske'], ['rs2'])
                else:
                    CP('dve', den, OPS[:, :, 64], [('ps', o_i)], ['rs2'])
                RCP(RS[:, 36:40], den, ['rs2'], ['rs2'])
                TT('dve', OB.rearrange("p (h d) -> p h d", h=4), OPS[:, :, 0:64],
                   RS[:, 36:40].rearrange("p (h o) -> p h o", o=1).to_broadcast([128, 4, 64]), ALU.mult,
                   [('ps', o_i), 'rs2'], ['ob'])
                transposes_to(lambda b: OT[:, b, :], OB, 2, 128, 'ob', 'ot', bank_i=1)
                for hf in range(2):
                    cs = slice(hf * 512, (hf + 1) * 512)
                    for k in range(8):
                        MM(ps[5], HT[:, k, t * 128:(t + 1) * 128], WG[:, k, cs], k == 0, k == 7,
                           [('ht', t), 'wg'], [('ps', 5)])
                    for k in range(2):
                        MM(ps[6], OT[:, k, :], WBR[:, k, cs], k == 0, k == 1, ['ot', 'wbr'], [('ps', 6)])
                    ACT(SIG[:, cs], ps[5], AF.Sigmoid, [('ps', 5)], ['sig'])
                    TT('dve', MTL[:, cs], ps[6], SIG[:, cs], ALU.mult, [('ps', 6), 'sig'], ['mtl'])
                DMA(md[m, t * 128:(t + 1) * 128, :], MTL, ['mtl'], [('md', m, t)])
                if m == 0 and t == 0:
                    ck(5)
                if m == 3 and t == 0:
                    ck(11)
                if m == 3 and t == 2:
                    ck(12)
            ck(6 + m)
        P.barrier()

        mod_tiles(l, 2 * D, 'gate', 0, which=(0,) if lastl else (0, 1))
        load_w(w_out[l], 8, D, WO, 'wo')
        for t in q_tiles:
            w = 0 if t >= 2 else 1
            DMA(M4, md[:, t * 128:(t + 1) * 128, :].rearrange("m p d -> p m d"),
                [('md', mm_, t) for mm_ in range(4)], ['m4'])
            DMA(XT, x_src(l)[t * 128:(t + 1) * 128, :], [], ['xt'])
            TT('dve', M4[:, 0, :], M4[:, 0, :], M4[:, 1, :], ALU.add, ['m4'], ['m4'])
            TT('pool', M4[:, 2, :], M4[:, 2, :], M4[:, 3, :], ALU.add, ['m4'], ['m4b'])
            TT('dve', M4[:, 0, :], M4[:, 0, :], M4[:, 2, :], ALU.add, ['m4', 'm4b'], ['m4'])
            pb = ps[1].bitcast(BF16)
            for k in range(8):
                TR(pb[:, k * 128:(k + 1) * 128], M4[:, 0, k * 128:(k + 1) * 128], IDB, ['m4', 'idb'], [('ps', 1)])
            CP('act', MT8, pb.rearrange("p (k q) -> p k q", k=8), [('ps', 1)], ['mt8'])
            for hf in range(2):
                cs = slice(hf * 512, (hf + 1) * 512)
                for k in range(8):
                    MM(ps[5 + hf], MT8[:, k, :], WO[:, k, cs], k == 0, k == 7, ['mt8', 'wo'], [('ps', 5 + hf)])
                TT('dve', HB[:, cs], ps[5 + hf], MOD[:, w, cs], ALU.mult, [('ps', 5 + hf), ('mod', w)], ['hb'])
                TT('pool', XS[:, t, cs], HB[:, cs], XT[:, cs], ALU.add, ['hb', 'xt'], [('x', t)])
        P.barrier()
        if dbg == ('xm', l):
            for t in q_tiles:
                DMA(dbg_out[t * 128:(t + 1) * 128, :], XS[:, t, :], [('x', t)], ['dbgo'])
            return True

        moe = (l % 2 == 1)
        mod_tiles(l, 4 * D, 'scale', 0, norm2_g[l:l + 1, :], which=(0,) if lastl else (0, 1))
        mod_tiles(l, 3 * D, 'shift', 2, which=(0,) if lastl else (0, 1))
        if moe:
            DMA(RT, moe_router[l // 2].rearrange("(k p) e -> p k e", p=128), [], ['rt'])
            P.op('dve', lambda e: e.memset(RTB, 0.0), [], ['rtb'])
            CP('dve', RTB[:, :, 0:8], RT, ['rt', 'rtb'], ['rtb'])
        for t in q_tiles:
            norm_tile(t, XS[:, t, :], [('x', t)], 0, 2, router=False)
            if moe:
                lg = ps[7][:, 0:NEXP]
                for k in range(8):
                    MM(ps[7][:, 0:16], HT[:, k, t * 128:(t + 1) * 128], RTB[:, k, :], k == 0, k == 7, [('ht', t), 'rtb'], [('ps', 7)])
                L = RS[:, 0:8]
                CP('dve', L, lg, [('ps', 7)], ['rs'])
                RED(RS[:, 8:9], L, ALU.max, ['rs'], ['rs'])
                TT('dve', RS[:, 16:24], L, RS[:, 8:9].to_broadcast([128, 8]), ALU.is_equal, ['rs'], ['rs'])
                STT(RS[:, 24:32], RS[:, 16:24], -1e30, L, ALU.mult, ALU.add, ['rs'], ['rs'])
                RED(RS[:, 9:10], RS[:, 24:32], ALU.max, ['rs'], ['rs'])
                TT('dve', RS[:, 40:48], RS[:, 24:32], RS[:, 9:10].to_broadcast([128, 8]), ALU.is_equal, ['rs'], ['rs'])
                TT('dve', RS[:, 10:11], RS[:, 9:10], RS[:, 8:9], ALU.subtract, ['rs'], ['rs'])
                ACT(RS[:, 11:12], RS[:, 10:11], AF.Exp, ['rs'], ['rs'])
                TS('dve', RS[:, 12:13], RS[:, 11:12], 1.0, None, ALU.add, None, ['rs'], ['rs'])
                RCP(RS[:, 13:14], RS[:, 12:13], ['rs'], ['rs'])
                TT('dve', RS[:, 14:15], RS[:, 11:12], RS[:, 13:14], ALU.mult, ['rs'], ['rs'])
                TT('dve', RS[:, 48:56], RS[:, 16:24], RS[:, 13:14].to_broadcast([128, 8]), ALU.mult, ['rs'], ['rs'])
                STT(COMB[:, t, :], RS[:, 40:48], RS[:, 14:15], RS[:, 48:56], ALU.mult, ALU.add, ['rs'], ['comb'])
                if t == 0:
                    ck(20)
        mod_tiles(l, 5 * D, 'gate', 0, which=(0,) if lastl else (0, 1))

        chunks = []
        if not lastl:
            chunks.append([0, 1])
        for c in range(4):
            chunks.append([2 + 4 * c + i for i in range(4)])
        nexp = NEXP if moe else 1

        def wset(s_):
            o = s_ * 6144
            return (R4[:, o:o + 2048].rearrange("p (k c) -> p k c", k=8),
                    R4[:, o + 2048:o + 4096].rearrange("p (k c) -> p k c", k=8),
                    R4[:, o + 4096:o + 6144].rearrange("p (k c) -> p k c", k=2),
                    MOD[:, 2 + s_, :].bitcast(BF16).rearrange("p (k c) -> p k c", k=2))

        def g2b(w):
            return MOD[:, w, :].rearrange("p (o d) -> p o d", o=1).to_broadcast([128, 2, D])

        gi = 0
        ci = 0
        for e_ in range(nexp):
            if moe:
                w1, w3, w2 = moe_w1[l // 2, e_], moe_w3[l // 2, e_], moe_w2[l // 2, e_]
            else:
                w1, w3, w2 = ffn_w1[l // 2], ffn_w3[l // 2], ffn_w2[l // 2]
            for g in range(DFF // 256):
                s_ = gi % 2
                gi += 1
                W1s, W3s, W2s, W2cs = wset(s_)
                load_w(w1[:, g * 256:(g + 1) * 256], 8, 256, None, None, cast=False,
                       use=lambda sv, c0, cw, skey: CP('act', W1s, sv, [skey], [('w1b', s_)]))
                load_w(w3[:, g * 256:(g + 1) * 256], 8, 256, None, None, cast=False,
                       use=lambda sv, c0, cw, skey: CP('act', W3s, sv, [skey], [('w3b', s_)]))

                def use_w2(sv, c0, cw, skey):
                    TT('pool', W2s, sv, g2b(0), ALU.mult, [skey, ('mod', 0)], [('w2b', s_)])
                    if not lastl:
                        TT('pool', W2cs, sv, g2b(1), ALU.mult, [skey, ('mod', 1)], [('mod', 2 + s_)])
                load_w(w2[g * 256:(g + 1) * 256, :], 2, D, None, None, cast=False, use=use_w2)
                for ch in chunks:
                    n = len(ch) * 128
                    t0 = ch[0] * 128
                    up = (ci % 2) * 2
                    ci += 1
                    for fc in range(2):
                        for k in range(8):
                            MM(ps[fc][:, 0:n], W1s[:, k, fc * 128:(fc + 1) * 128], HT[:, k, t0:t0 + n], k == 0, k == 7,
                               [('w1b', s_)] + [('ht', t) for t in ch], [('ps', fc)])
                        for k in range(8):
                            MM(ps[2 + fc][:, 0:n], W3s[:, k, fc * 128:(fc + 1) * 128], HT[:, k, t0:t0 + n], k == 0, k == 7,
                               [('w3b', s_)] + [('ht', t) for t in ch], [('ps', 2 + fc)])
                        ACT(SA[:, fc, 0:n], ps[fc][:, 0:n], AF.Silu, [('ps', fc)], [('sa', fc)])
                        TT('dve', UT[:, up + fc, 0:n], ps[2 + fc][:, 0:n], SA[:, fc, 0:n], ALU.mult,
                           [('ps', 2 + fc), ('sa', fc)], [('ut', up + fc)])
                    for ti, t in enumerate(ch):
                        ob_ = 4 + 2 * (ti % 2)
                        wsel, wkey = (W2s, ('w2b', s_)) if t >= 2 else (W2cs, ('mod', 2 + s_))
                        for hf in range(2):
                            cs = slice(hf * 512, (hf + 1) * 512)
                            for fc in range(2):
                                MM(ps[ob_ + hf], UT[:, up + fc, ti * 128:(ti + 1) * 128], wsel[:, fc, cs], fc == 0, fc == 1,
                                   [('ut', up + fc), wkey], [('ps', ob_ + hf)])
                            if moe:
                                STT(XS[:, t, cs], ps[ob_ + hf], COMB[:, t, e_:e_ + 1], XS[:, t, cs], ALU.mult, ALU.add,
                                    [('ps', ob_ + hf), 'comb', ('x', t)], [('x', t)])
                            else:
                                TT('dve', XS[:, t, cs], ps[ob_ + hf], XS[:, t, cs], ALU.add,
                                   [('ps', ob_ + hf), ('x', t)], [('x', t)])
        P.barrier()
        if dbg == ('xf', l):
            for t in q_tiles:
                DMA(dbg_out[t * 128:(t + 1) * 128, :], XS[:, t, :], [('x', t)], ['dbgo'])
            return True
        if not lastl:
            for t in range(NT):
                DMA(xd[t * 128:(t + 1) * 128, :], XS[:, t, :], [('x', t)], [('xd', t)])
            P.barrier()
        return False

    stopped = False
    try:
        for l in range(n_layers):
            stopped = layer(l)
            if stopped:
                break
    except _Stop:
        stopped = True
    if not stopped and n_layers == DEPTH:
        DMA(MOD[:, 0, :], final_g.partition_broadcast(128), [], [('mod', 0)])
        for t in range(2, NT):
            src = XS[:, t, :]
            ACT(HB, src, AF.Square, [('x', t)], ['hb'], accum_out=STAT[:, 0:1])
            ACT(STAT[:, 1:2], STAT[:, 0:1], AF.Sqrt, ['hb'], ['stat'], bias=EPSB, scale=1.0 / D)
            RCP(STAT[:, 2:3], STAT[:, 1:2], ['stat'], ['stat'])
            STT(HB, src, STAT[:, 2:3], MOD[:, 0, :], ALU.mult, ALU.mult, [('x', t), 'stat', ('mod', 0)], ['hb'])
            DMA(out[(t - 2) * 128:(t - 1) * 128, :], HB, ['hb'], ['out'])
    elif not stopped:
        DMA(out[0:128, :], HB, [], ['out'])
    P.barrier()
    P.emit()
    print("instructions:", P.n, {k: len(v) for k, v in P.streams.items()})


_CACHE = {}


def make_in_maps(inp, n_cores=8):
    f = lambda a: np.ascontiguousarray(np.asarray(a), dtype=np.float32)
    cosH, sinH, cosR, sinR = _rope_tables()
    shared = {
        "norm1_g": f(inp["norm1_g"]), "norm2_g": f(inp["norm2_g"]), "w_ada": f(inp["w_ada"]),
        "b_ada": f(inp["b_ada"]), "w_in": f(inp["w_in"]), "biasA": _natten_bias(f(inp["na_rpb"])),
        "smallp": np.ascontiguousarray(np.concatenate(
            [f(inp["gb_qnorm"]), f(inp["gb_knorm"]), f(inp["mla_qnorm"]), f(inp["mla_kvnorm"]), f(inp["wc_sink"])],
            axis=1)),
        "mla_wqb": f(inp["mla_wqb"]), "mla_wkvb": f(inp["mla_wkvb"]), "w_branch": f(inp["w_branch"]),
        "w_out": f(inp["w_out"]), "ffn_w1": f(inp["ffn_w1"]), "ffn_w3": f(inp["ffn_w3"]), "ffn_w2": f(inp["ffn_w2"]),
        "moe_router": f(inp["moe_router"]), "moe_w1": f(inp["moe_w1"]), "moe_w3": f(inp["moe_w3"]),
        "moe_w2": f(inp["moe_w2"]), "final_g": f(inp["final_g"]).reshape(1, D),
        "c_ident": np.eye(128, dtype=np.float32),
        "c_ropeH": np.ascontiguousarray(np.stack([cosH, sinH])),
        "c_ropeR": np.ascontiguousarray(np.stack([cosR, sinR])),
        "c_maskC": _maskC(),
    }
    x = f(inp["x"]); ctx = f(inp["ctx"]); c = f(inp["c"]); cc = f(inp["c_ctx"])
    maps = []
    for b in range(n_cores):
        m = dict(shared)
        m["xin"] = np.ascontiguousarray(np.concatenate([ctx[b], x[b]], axis=0))
        m["cvec"] = np.ascontiguousarray(np.stack([c[b], cc]))
        maps.append(m)
    return maps


def kernel(**inputs):
    if "nc" not in _CACHE:
        _CACHE["nc"] = build(DEPTH)
    nc = _CACHE["nc"]
    maps = make_in_maps(inputs, 8)
    res = run_bass_kernel_spmd(nc, maps, core_ids=list(range(8)))
    return np.stack([np.asarray(r["out"], dtype=np.float32) for r in res.results], axis=0)
```

```python
import numpy as np
from contextlib import ExitStack
import concourse.bass as bass
import concourse.mybir as mybir
from concourse.bass_utils import run_bass_kernel_spmd

F32 = mybir.dt.float32
BF16 = mybir.dt.bfloat16
AF = mybir.ActivationFunctionType
ALU = mybir.AluOpType
AX = mybir.AxisListType

D = 1024
S = 2048
C = 256
T = S + C
NT = T // 128
DEPTH = 4
IN_W = 6304
DFF = 3584
NEXP = 8
EPS = 1e-6
NEG = -30000.0
NDMA = 12
NSTG = 3

OFF = dict(aq=0, ak=256, av=512, bq=768, bk=1024, bv=1152, cq=1280, ck=1536, cv=1664,
           dqa=1792, dkva=2048, gate=2208)


class Prog:
    CE = ['pe', 'act', 'dve', 'pool']

    def __init__(self, nc, es):
        self.nc = nc
        self.streams = {e: [] for e in self.CE + ['sp']}
        self.sems = {}
        for e in self.CE:
            self.sems[e] = es.enter_context(nc.semaphore("s_" + e))
        for i in range(NDMA):
            self.sems['d%d' % i] = es.enter_context(nc.semaphore("s_d%d" % i))
        self.cnt = {k: 0 for k in self.sems}
        self.known = {e: {k: 0 for k in self.sems} for e in self.streams}
        self.lastw = {}
        self.readers = {}
        self.dma_rr = 0
        self.n = 0
        self.last_real = {e: None for e in self.CE}
        self.pend = {e: False for e in self.CE}

    def _materialize(self, s):
        if s in self.pend and self.pend[s]:
            self.streams[s][self.last_real[s]][2] = s
            self.cnt[s] += 1
            self.pend[s] = False

    def _deps(self, eng, reads, writes):
        deps = {}

        def add(d):
            if d is not None:
                if d[0] == 'pe' and eng == 'pe':
                    return
                if deps.get(d[0], 0) < d[1]:
                    deps[d[0]] = d[1]
        for k in reads:
            add(self.lastw.get(k))
        for k in writes:
            add(self.lastw.get(k))
            for r in self.readers.get(k, ()):
                add(r)
        waits = []
        kn = self.known[eng]
        for s, i in deps.items():
            if kn[s] < i:
                if i > self.cnt[s]:
                    self._materialize(s)
                assert i <= self.cnt[s], (s, i, self.cnt[s])
                kn[s] = i
                waits.append((s, i))
        return waits

    def _commit(self, tok, reads, writes):
        for k in writes:
            self.lastw[k] = tok
            self.readers[k] = []
        for k in reads:
            self.readers.setdefault(k, []).append(tok)

    def op(self, eng, fn, reads=(), writes=()):
        waits = self._deps(eng, reads, writes)
        tok = (eng, self.cnt[eng] + 1)
        self.streams[eng].append([waits, fn, None])
        self.last_real[eng] = len(self.streams[eng]) - 1
        self.pend[eng] = True
        self._commit(tok, reads, writes)
        self.n += 1

    def dma(self, fn, reads=(), writes=()):
        s = 'd%d' % self.dma_rr
        self.dma_rr = (self.dma_rr + 1) % NDMA
        waits = self._deps('sp', reads, writes)
        if self.known['sp'][s] < self.cnt[s]:
            self.known['sp'][s] = self.cnt[s]
            waits.append((s, self.cnt[s]))
        self.cnt[s] += 1
        tok = (s, self.cnt[s])
        self.streams['sp'].append([waits, fn, s])
        self._commit(tok, reads, writes)
        self.n += 1

    def barrier(self):
        for e in self.CE:
            self._materialize(e)
        snap = dict(self.cnt)
        for e in self.streams:
            waits = []
            for s, i in snap.items():
                if self.known[e][s] < i:
                    self.known[e][s] = i
                    waits.append((s, i))
            if waits:
                self.streams[e].append([waits, None, None])

    def emit(self):
        nc = self.nc
        sems = self.sems

        def mult(s):
            return 16 if s[0] == 'd' else 1

        def run(engine, stream):
            for waits, fn, inc in stream:
                for s, i in waits:
                    engine.wait_ge(sems[s], i * mult(s))
                if fn is None:
                    continue
                ins = fn(engine)
                if inc is not None:
                    ins.then_inc(sems[inc], mult(inc))

        with nc.Block() as block:
            @block.tensor
            def _(e):
                run(e, self.streams['pe'])

            @block.scalar
            def _(e):
                run(e, self.streams['act'])

            @block.vector
            def _(e):
                run(e, self.streams['dve'])

            @block.gpsimd
            def _(e):
                run(e, self.streams['pool'])

            @block.sync
            def _(e):
                run(e, self.streams['sp'])


def _rope_tables():
    def ang(pos, dim):
        inv = (10000.0 ** (-np.arange(0, dim, 2, dtype=np.float32) / dim)).astype(np.float32)
        return pos.astype(np.float32)[:, None] * inv[None, :]
    t = np.arange(S)
    out = []
    for rot in (64, 32):
        half = rot // 2
        a = np.concatenate([ang(t // 64, half), ang(t % 64, half)], axis=-1).astype(np.float32)
        out += [np.cos(a).astype(np.float32), np.sin(a).astype(np.float32)]
    return out


def _natten_plan():
    rows = S // 64
    kh = 8
    pats = {}
    plan = []
    for i in range(16):
        lst = []
        for j in range(16):
            sig = []
            anyv = False
            for a in range(2):
                for b in range(2):
                    r = 2 * i + b
                    rk = 2 * j + a
                    st = min(max(r - kh // 2, 0), rows - kh)
                    if st <= rk < st + kh:
                        sig.append(rk - r + 7)
                        anyv = True
                    else:
                        sig.append(-1)
            if anyv:
                sig = tuple(sig)
                if sig not in pats:
                    pats[sig] = len(pats)
                lst.append((j, pats[sig]))
        plan.append(lst)
    return plan, pats


NAT_PLAN, NAT_PATS = _natten_plan()
NPAT = len(NAT_PATS)


def _natten_bias(rpb):
    col = np.arange(64)
    cstart = np.clip(col - 8, 0, 48)
    col_ok = (col[None, :] >= cstart[:, None]) & (col[None, :] < cstart[:, None] + 16)
    dc = np.clip(col[None, :] - col[:, None] + 15, 0, 30)
    out = np.full((DEPTH, 128, NPAT * 4, 128), NEG, dtype=np.float32)
    for sig, pid in NAT_PATS.items():
        for a in range(2):
            for b in range(2):
                dr = sig[a * 2 + b]
                if dr < 0:
                    continue
                g = rpb[:, :, dr, :][:, :, dc]
                g = np.where(col_ok[None, None], g, np.float32(NEG))
                g = np.transpose(g, (0, 3, 1, 2))
                out[:, a * 64:(a + 1) * 64, pid * 4:(pid + 1) * 4, b * 64:(b + 1) * 64] = g
    return out


def _maskC():
    kk = np.arange(128)[:, None]
    qq = np.arange(128)[None, :]
    m0 = np.where(qq <= kk, 0.0, NEG).astype(np.float32)
    m1 = np.where(kk <= qq, 0.0, NEG).astype(np.float32)
    return np.stack([m0, m1], axis=1)


def build(n_layers=DEPTH, dbg=None):
    nc = bass.Bass("TRN2", target_bir_lowering=False, dynamic_dma_scratch_size=256)
    es = ExitStack()
    with es:
        _build_body(nc, es, n_layers, dbg)
    return nc


def _build_body(nc, es, n_layers, dbg):
    def din(name, shape):
        return nc.dram_tensor(name, list(shape), F32, kind="ExternalInput").ap()

    xin = din("xin", [T, D])
    cvec = din("cvec", [2, D])
    norm1_g = din("norm1_g", [DEPTH, D])
    norm2_g = din("norm2_g", [DEPTH, D])
    w_ada = din("w_ada", [DEPTH, D, 6 * D])
    b_ada = din("b_ada", [DEPTH, 6 * D])
    w_in = din("w_in", [DEPTH, D, IN_W])
    biasA = din("biasA", [DEPTH, 128, NPAT * 4, 128])
    smallp = din("smallp", [DEPTH, 516])
    mla_wqb = din("mla_wqb", [DEPTH, 256, 384])
    mla_wkvb = din("mla_wkvb", [DEPTH, 128, 512])
    w_branch = din("w_branch", [DEPTH, 4, 256, D])
    w_out = din("w_out", [DEPTH, D, D])
    ffn_w1 = din("ffn_w1", [2, D, DFF])
    ffn_w3 = din("ffn_w3", [2, D, DFF])
    ffn_w2 = din("ffn_w2", [2, DFF, D])
    moe_router = din("moe_router", [2, D, NEXP])
    moe_w1 = din("moe_w1", [2, NEXP, D, DFF])
    moe_w3 = din("moe_w3", [2, NEXP, D, DFF])
    moe_w2 = din("moe_w2", [2, NEXP, DFF, D])
    final_g = din("final_g", [1, D])
    c_ident = din("c_ident", [128, 128])
    c_ropeH = din("c_ropeH", [2, S, 32])
    c_ropeR = din("c_ropeR", [2, S, 16])
    c_maskC = din("c_maskC", [128, 2, 128])
    out = nc.dram_tensor("out", [S, D], F32, kind="ExternalOutput").ap()
    xd = nc.dram_tensor("xd", [T, D], F32, kind="Internal").ap()
    md = nc.dram_tensor("md", [4, T, D], BF16, kind="Internal").ap()
    dbg_out = None
    if dbg is not None:
        dbg_out = nc.dram_tensor("dbg", [T, D], F32, kind="ExternalOutput").ap()

    P = Prog(nc, es)

    def sb(name, shape, dt):
        return es.enter_context(nc.sbuf_tensor(name, list(shape), dt))[:]

    ps = [es.enter_context(nc.psum_tensor("ps%d" % i, [128, 512], F32))[:] for i in range(8)]

    IDF = sb("idf", [128, 128], F32)
    IDB = sb("idb", [128, 128], BF16)
    ROPEH = sb("ropeh", [128, 2, 16, 32], F32)
    ROPER = sb("roper", [128, 2, 16, 16], F32)
    MASKC = sb("maskc", [128, 2, 128], BF16)
    CV = sb("cv", [128, 2, 8], F32)
    SL = sb("sl", [128, 2, 8, 128], F32)
    MOD = sb("mod", [128, 4, D], F32)
    SMP = sb("smp", [128, 516], F32)
    SKE = sb("ske", [128, 4], F32)
    HT = sb("ht", [128, 8, T], BF16)
    STG = sb("stg", [128, NSTG, 2048], F32)
    XT = sb("xt", [128, D], F32)
    HB = sb("hb", [128, D], F32)
    PB = HB
    STAT = sb("stat", [128, 32], F32)
    PBUF = sb("pbuf", [128, D], F32)
    TMPF = sb("tmpf", [128, 4, 256], F32)
    PB16 = sb("pb16", [128, 512], BF16)
    QT = sb("qt", [128, 2, 4, 128], BF16)
    OB = sb("ob", [128, 256], BF16)
    OT = sb("ot", [128, 2, 128], BF16)
    DTT = sb("dtt", [128, 2, 128], BF16)
    MT8 = sb("mt8", [128, 8, 128], BF16)
    RT = sb("rt", [128, 8, NEXP], F32)
    RTB = sb("rtb", [128, 8, 16], BF16)
    COMB = sb("comb", [128, NT, NEXP], F32)
    RS = sb("rs", [128, 64], F32)
    UT = sb("ut", [128, 4, 512], BF16)
    SA = sb("sa", [128, 2, 512], BF16)
    R4 = sb("r4", [128, 12288], BF16)
    R2 = sb("r2", [128, 36864], BF16)

    KT = R2[:, 0:9216].rearrange("p (h t) -> p h t", h=4)
    VA = R2[:, 9216:13896].rearrange("p (t h d) -> p t h d", t=NT, h=4)
    BIAS = R2[:, 13896:13896 + NPAT * 4 * 128].rearrange("p (n q) -> p n q", q=128)
    XS = R2.bitcast(F32).rearrange("p (t d) -> p t d", t=NT)
    b0 = 13896 + NPAT * 4 * 128
    WG = R2[:, b0:b0 + 8192].rearrange("p (k c) -> p k c", k=8)
    WKV = WG
    WQ = R2[:, b0 + 8192:b0 + 10240].rearrange("p (k c) -> p k c", k=8)
    WBR = R2[:, b0 + 10240:b0 + 12288].rearrange("p (k c) -> p k c", k=2)
    WQB = R2[:, b0 + 12288:b0 + 13056].rearrange("p (k c) -> p k c", k=2)
    WKVB = R2[:, b0 + 13056:b0 + 13568]
    SIG = R2[:, b0 + 13568:b0 + 14592]
    MTL = R2[:, b0 + 14592:b0 + 15616]
    PT = R2[:, b0 + 15616:b0 + 16640].rearrange("p (a g q) -> p a g q", a=2, g=4)
    assert b0 + 16640 <= 36864
    WO = R4[:, 0:8192].rearrange("p (k c) -> p k c", k=8)
    M4 = MOD[:, 2:4, :].rearrange("p a d -> p (a d)").bitcast(BF16).rearrange("p (a d) -> p a d", a=4)
    H2F = PBUF.rearrange("p (k q) -> p k q", k=8)
    W1B = R4[:, 0:4096].rearrange("p (k c) -> p k c", k=8)
    W3B = R4[:, 4096:8192].rearrange("p (k c) -> p k c", k=8)
    W2B = R4[:, 8192:12288].rearrange("p (k c) -> p k c", k=4)

    def MM(o, lhsT, rhs, start, stop, r, w):
        P.op('pe', lambda e: e.matmul(o, lhsT, rhs, start=start, stop=stop), r, w)

    def TR(o, i, ident, r, w):
        P.op('pe', lambda e: e.transpose(o, i, ident), r, w)

    def ACT(o, i, func, r, w, **kw):
        P.op('act', lambda e: e.activation(o, i, func, **kw), r, w)

    def TT(eng, o, a, b, op, r, w):
        P.op(eng, lambda e: e.tensor_tensor(o, a, b, op), r, w)

    def TS(eng, o, a, s1, s2, op0, op1, r, w):
        if op1 is None:
            P.op(eng, lambda e: e.tensor_scalar(o, a, s1, None, op0), r, w)
        else:
            P.op(eng, lambda e: e.tensor_scalar(o, a, s1, s2, op0, op1), r, w)

    def STT(o, a, s, b, op0, op1, r, w):
        P.op('dve', lambda e: e.scalar_tensor_tensor(o, a, s, b, op0, op1), r, w)

    def CP(eng, o, i, r, w):
        if eng == 'act':
            P.op('act', lambda e: e.activation(o, i, AF.Copy), r, w)
        else:
            P.op(eng, lambda e: e.tensor_copy(o, i), r, w)

    def RED(o, i, op, r, w):
        P.op('dve', lambda e: e.tensor_reduce(o, i, AX.X, op), r, w)

    def RCP(o, i, r, w):
        P.op('dve', lambda e: e.reciprocal(o, i), r, w)

    def DMA(o, i, r, w):
        P.dma(lambda e: e.dma_start(out=o, in_=i), r, w)

    stg_rr = [0]

    def load_w(src, kc, ncols, dst, dkey, cast=True, use=None):
        pcw = 2048 // kc
        c0 = 0
        while c0 < ncols:
            cw = min(pcw, ncols - c0)
            s = stg_rr[0]
            stg_rr[0] = (s + 1) % NSTG
            sv = STG[:, s, 0:kc * cw].rearrange("p (k c) -> p k c", k=kc)
            DMA(sv, src[:, c0:c0 + cw].rearrange("(k p) c -> p k c", p=128), [], [('stg', s)])
            if cast:
                CP('pool', dst[:, :, c0:c0 + cw], sv, [('stg', s)], [dkey])
            else:
                use(sv, c0, cw, ('stg', s))
            c0 += cw

    import os
    KS = int(os.environ.get("KSETUP", "99"))
    if KS > 0:
        DMA(IDF, c_ident, [], ['idf'])
        CP('dve', IDB, IDF, ['idf'], ['idb'])
    if KS > 1:
        DMA(ROPEH[:, 0], c_ropeH[0].rearrange("(t p) d -> p t d", p=128), [], ['rope'])
        DMA(ROPEH[:, 1], c_ropeH[1].rearrange("(t p) d -> p t d", p=128), [], ['rope'])
        DMA(ROPER[:, 0], c_ropeR[0].rearrange("(t p) d -> p t d", p=128), [], ['rope'])
        DMA(ROPER[:, 1], c_ropeR[1].rearrange("(t p) d -> p t d", p=128), [], ['rope'])
    if KS > 2:
        DMA(PBUF[:, 0:256].rearrange("p (a q) -> p a q", a=2), c_maskC, [], ['pbuf'])
        CP('dve', MASKC, PBUF[:, 0:256].rearrange("p (a q) -> p a q", a=2), ['pbuf'], ['maskc'])
    if KS > 3:
        P.dma(lambda e: e.dma_start(out=CV, in_=cvec.rearrange("w (k p) -> p w k", p=128),
                                    allow_slow_non_contiguous=True), [], ['cv'])
    if KS > 4:
        ACT(CV, CV, AF.Silu, ['cv'], ['cv'])
    if KS > 5:
        for wch in range(2):
            CP('dve', SL[:, wch], CV[:, wch, :].rearrange("p (k o) -> p k o", o=1).to_broadcast([128, 8, 128]),
               ['cv'], ['sl'])

    def rope_view(tab, which, t, nh, half):
        return tab[:, which, t - 2, :].rearrange("p (o d) -> p o d", o=1).to_broadcast([128, nh, half])

    def rope(dst, src, tab, t, nh, half, skey, dkey):
        c = rope_view(tab, 0, t, nh, half)
        s = rope_view(tab, 1, t, nh, half)
        x1 = src[:, :, 0:half]
        x2 = src[:, :, half:2 * half]
        t1 = TMPF[:, 0, 0:nh * half].rearrange("p (h d) -> p h d", h=nh)
        t2 = TMPF[:, 1, 0:nh * half].rearrange("p (h d) -> p h d", h=nh)
        t3 = TMPF[:, 2, 0:nh * half].rearrange("p (h d) -> p h d", h=nh)
        t4 = TMPF[:, 3, 0:nh * half].rearrange("p (h d) -> p h d", h=nh)
        TT('dve', t1, x1, c, ALU.mult, [skey, 'rope'], ['tf0'])
        TT('pool', t2, x2, s, ALU.mult, [skey, 'rope'], ['tf1'])
        TT('dve', t3, x1, s, ALU.mult, [skey, 'rope'], ['tf2'])
        TT('pool', t4, x2, c, ALU.mult, [skey, 'rope'], ['tf3'])
        TT('dve', dst[:, :, 0:half], t1, t2, ALU.subtract, ['tf0', 'tf1'], [dkey])
        TT('dve', dst[:, :, half:2 * half], t3, t4, ALU.add, ['tf2', 'tf3'], [dkey])

    def head_rms(src, nh, hd, gain, skey):
        sq = TMPF[:, 0:1, :].rearrange("p a c -> p (a c)")[:, 0:nh * hd].rearrange("p (h d) -> p h d", h=nh)
        TT('dve', sq, src, src, ALU.mult, [skey], ['tf0'])
        RED(RS[:, 0:nh], sq, ALU.add, ['tf0'], ['rs'])
        ACT(RS[:, 8:8 + nh], RS[:, 0:nh], AF.Sqrt, ['rs'], ['rs'], bias=EPSB, scale=1.0 / hd)
        RCP(RS[:, 16:16 + nh], RS[:, 8:8 + nh], ['rs'], ['rs'])
        TT('dve', src, src, RS[:, 16:16 + nh].rearrange("p (h o) -> p h o", o=1).to_broadcast([128, nh, hd]),
           ALU.mult, [skey, 'rs'], [skey])
        TT('dve', src, src, gain.rearrange("p (o d) -> p o d", o=1).to_broadcast([128, nh, hd]),
           ALU.mult, [skey, 'smp'], [skey])

    EPSB = sb("epsb", [128, 1], F32)
    P.op('dve', lambda e: e.memset(EPSB, EPS), [], ['epsb'])

    def mod_tiles(l, col0, kind, slot, gain_src=None, which=(0, 1)):
        DMA(PB, b_ada[l:l + 1, col0:col0 + D].partition_broadcast(128), [], ['hb'])

        def use(sv, c0, cw, skey):
            for w in which:
                bank = ps[6 + w]
                for k in range(8):
                    MM(bank[:, 0:cw], SL[:, w, k, :], sv[:, k, :], k == 0, k == 7,
                       ['sl', skey], [('ps', 6 + w)])
                TT('dve', MOD[:, slot + w, c0:c0 + cw], bank[:, 0:cw], PB[:, c0:c0 + cw], ALU.add,
                   [('ps', 6 + w), 'hb'], [('mod', slot + w)])
        load_w(w_ada[l][:, col0:col0 + D], 8, D, None, None, cast=False, use=use)
        if kind == 'scale':
            DMA(PB, gain_src.partition_broadcast(128), [], ['hb'])
            for w in which:
                STT(MOD[:, slot + w], MOD[:, slot + w], 1.0, PB, ALU.add, ALU.mult,
                    [('mod', slot + w), 'hb'], [('mod', slot + w)])

    def norm_tile(t, src_ap, src_keys, gslot, sslot, router=False, xkey=None):
        w = 0 if t >= 2 else 1
        ACT(HB, src_ap, AF.Square, src_keys, ['hb'], accum_out=STAT[:, 0:1])
        ACT(STAT[:, 1:2], STAT[:, 0:1], AF.Sqrt, ['hb'], ['stat'], bias=EPSB, scale=1.0 / D)
        RCP(STAT[:, 2:3], STAT[:, 1:2], ['stat'], ['stat'])
        STT(HB, src_ap, STAT[:, 2:3], MOD[:, gslot + w], ALU.mult, ALU.mult,
            src_keys + ['stat', ('mod', gslot + w)], ['hb'])
        TT('dve', HB, HB, MOD[:, sslot + w], ALU.add, ['hb', ('mod', sslot + w)], ['hb'])
        for half in range(2):
            bank = ps[half]
            for kk in range(4):
                k = half * 4 + kk
                TR(bank[:, kk * 128:(kk + 1) * 128], HB[:, k * 128:(k + 1) * 128], IDF,
                   ['hb', 'idf'], [('ps', half)])
            pv = bank.rearrange("p (k q) -> p k q", k=4)
            CP('act' if half == 0 else 'dve', HT[:, half * 4:half * 4 + 4, t * 128:(t + 1) * 128], pv,
               [('ps', half)], [('ht', t)])
            if router:
                CP('dve' if half == 0 else 'act', H2F[:, half * 4:half * 4 + 4, :], pv,
                   [('ps', half)], ['h2f'])


    class _Stop(Exception):
        pass
    KSTOP = int(os.environ.get('KSTOP', '0'))

    def ck(n):
        if KSTOP == n:
            raise _Stop()

    def x_src(l):
        return xin if l == 0 else xd

    def kv_tiles(m, t, lastl):
        if t < 2:
            return [(0, None), (1, None)]
        i = t - 2
        if m == 0:
            return [(j + 2, ('A', pid)) for (j, pid) in NAT_PLAN[i]] + [(0, None), (1, None)]
        if m == 2:
            lst = []
            if i - 1 >= 0:
                lst.append((t - 1, ('C', 0)))
            lst.append((t, None))
            if i + 1 < 16:
                lst.append((t + 1, ('C', 1)))
            return lst + [(0, None), (1, None)]
        return [(j, None) for j in range(NT)]

    GQN = SMP[:, 0:64]
    GKN = SMP[:, 64:128]
    MQN = SMP[:, 128:384]
    MKVN = SMP[:, 384:512]

    def evac_scaled(dst, src_ps, pkey, dkey, scale):
        P.op('act', lambda e: e.activation(dst, src_ps, AF.Copy, scale=scale), [pkey], [dkey])

    def transposes_to(dst_fn, src16, nblk, width, skey, dkey, bank_i=1):
        pb = ps[bank_i].bitcast(BF16)
        for b in range(nblk):
            TR(pb[0:width, b * 128:(b + 1) * 128], src16[:, b * width:(b + 1) * width], IDB,
               [skey, 'idb'], [('ps', bank_i)])
        for b in range(nblk):
            CP('act' if b % 2 == 0 else 'dve', dst_fn(b), pb[0:width, b * 128:(b + 1) * 128],
               [('ps', bank_i)], [dkey])

    def proj(bank_i, t, wview, ncols, wkey):
        for k in range(8):
            MM(ps[bank_i][:, 0:ncols], HT[:, k, t * 128:(t + 1) * 128], wview[:, k, 0:ncols], k == 0, k == 7,
               [('ht', t), wkey], [('ps', bank_i)])

    att_i = [0]

    def layer(l):
        lastl = (l == DEPTH - 1)
        q_tiles = list(range(2, NT)) if lastl else list(range(NT))
        DMA(SMP, smallp[l:l + 1, :].partition_broadcast(128), [], ['smp'])
        ACT(SKE, SMP[:, 512:516], AF.Exp, ['smp'], ['ske'])
        mod_tiles(l, 1 * D, 'scale', 0, norm1_g[l:l + 1, :])
        mod_tiles(l, 0 * D, 'shift', 2)
        ck(1)
        for t in range(NT):
            DMA(XT, x_src(l)[t * 128:(t + 1) * 128, :], [], ['xt'])
            norm_tile(t, XT, ['xt'], 0, 2)
        P.barrier()
        ck(2)

        for m in [int(c_) for c_ in os.environ.get('KMIX', '0123')]:
            nh = 4
            nkv = 4 if m in (0, 3) else 2
            hd = 128 if m == 3 else 64
            if m == 0:
                load_w(w_in[l][:, OFF['ak']:OFF['ak'] + 512], 8, 512, WKV, 'wg')
                DMA_bias = True
                for c0 in range(0, NPAT * 4, 16):
                    c1 = min(c0 + 16, NPAT * 4)
                    s_ = stg_rr[0]
                    stg_rr[0] = (s_ + 1) % NSTG
                    sv = STG[:, s_, 0:(c1 - c0) * 128].rearrange("p (n q) -> p n q", q=128)
                    DMA(sv, biasA[l][:, c0:c1, :], [], [('stg', s_)])
                    CP('pool', BIAS[:, c0:c1, :], sv, [('stg', s_)], ['bias'])
                kvw = 512
            elif m == 1:
                load_w(w_in[l][:, OFF['bk']:OFF['bk'] + 256], 8, 256, WKV, 'wg')
                kvw = 256
            elif m == 2:
                load_w(w_in[l][:, OFF['ck']:OFF['ck'] + 256], 8, 256, WKV, 'wg')
                kvw = 256
            else:
                load_w(w_in[l][:, OFF['dkva']:OFF['dkva'] + 160], 8, 160, WKV, 'wg')
                load_w(mla_wkvb[l], 1, 512, WKVB.rearrange("p (k c) -> p k c", k=1), 'wkvb')
                kvw = 160
            if m == 0:
                ck(3)
            P.op('dve', lambda e: e.memset(VA[:, :, :, 64:65], 1.0), [], ['va'])
            for t in range(NT):
                lat = t >= 2
                proj(0, t, WKV, kvw, 'wg')
                CP('act', PBUF[:, 0:kvw], ps[0][:, 0:kvw], [('ps', 0)], ['pbuf'])
                if m == 0:
                    CP('dve', PB16[:, 0:256], PBUF[:, 0:256], ['pbuf'], ['pb16'])
                    CP('pool', VA[:, t, :, 0:64], PBUF[:, 256:512].rearrange("p (h d) -> p h d", h=4), ['pbuf'], ['va'])
                    transposes_to(lambda b: KT[0:64, b, t * 128:(t + 1) * 128], PB16, 4, 64, 'pb16', 'kt')
                elif m in (1, 2):
                    kview = PBUF[:, 0:128].rearrange("p (h d) -> p h d", h=2)
                    if m == 1:
                        head_rms(kview, 2, 64, GKN, 'pbuf')
                    k16 = PB16[:, 0:128].rearrange("p (h d) -> p h d", h=2)
                    if lat:
                        rope(k16, kview, ROPEH, t, 2, 32, 'pbuf', 'pb16')
                    else:
                        CP('dve', k16, kview, ['pbuf'], ['pb16'])
                    CP('pool', VA[:, t, 0:2, 0:64], PBUF[:, 128:256].rearrange("p (h d) -> p h d", h=2), ['pbuf'], ['va'])
                    transposes_to(lambda b: KT[0:64, b, t * 128:(t + 1) * 128], PB16, 2, 64, 'pb16', 'kt')
                else:
                    cview = PBUF[:, 0:128].rearrange("p (h d) -> p h d", h=1)
                    head_rms(cview, 1, 128, MKVN, 'pbuf')
                    CP('dve', PB16[:, 0:128], PBUF[:, 0:128], ['pbuf'], ['pb16'])
                    transposes_to(lambda b: DTT[:, 0, :], PB16, 1, 128, 'pb16', 'dtt')
                    ck(14)
                    MM(ps[2][:, 0:512], DTT[:, 0, :], WKVB, True, True, ['dtt', 'wkvb'], [('ps', 2)])
                    ck(15)
                    kf = PB16[:, 0:512].rearrange("p (h d) -> p h d", h=4)
                    P.op('dve', lambda e: e.memset(PB16[:, 0:512], 0.0), [], ['pb16'])
                    CP('act', PBUF[:, 512:1024], ps[2], [('ps', 2)], ['pbuf2'])
                    dkv = PBUF[:, 512:1024].rearrange("p (h d) -> p h d", h=4)
                    CP('pool', kf[:, :, 0:64], dkv[:, :, 0:64], ['pbuf2'], ['pb16'])
                    CP('pool', VA[:, t, :, 0:64], dkv[:, :, 64:128], ['pbuf2'], ['va'])
                    ck(16)
                    pe_src = PBUF[:, 128:160].rearrange("p (h d) -> p h d", h=1)
                    pe_dst = PBUF[:, 160:192].rearrange("p (h d) -> p h d", h=1)
                    if lat:
                        rope(pe_dst, pe_src, ROPER, t, 1, 16, 'pbuf', 'pbuf')
                    else:
                        CP('dve', pe_dst, pe_src, ['pbuf'], ['pbuf'])
                    ck(17)
                    CP('dve', kf[:, :, 64:96], pe_dst.to_broadcast([128, 4, 32]), ['pbuf'], ['pb16'])
                    ck(18)
                    transposes_to(lambda b: KT[:, b, t * 128:(t + 1) * 128], PB16, 4, 128, 'pb16', 'kt')
            if m == 0:
                ck(4)
            if m == 3:
                ck(10)
            qoff = [OFF['aq'], OFF['bq'], OFF['cq'], OFF['dqa']][m]
            load_w(w_in[l][:, qoff:qoff + 256], 8, 256, WQ, 'wq')
            if m == 3:
                load_w(mla_wqb[l], 2, 384, WQB, 'wqb')
            load_w(w_in[l][:, OFF['gate'] + m * D:OFF['gate'] + (m + 1) * D], 8, D, WG, 'wg')
            load_w(w_branch[l, m], 2, D, WBR, 'wbr')
            def q_prep(t, QTv, qkey):
                lat = t >= 2
                proj(0, t, WQ, 256, 'wq')
                qscale = 0.125 if m != 3 else float(96 ** -0.5)
                if m == 3:
                    evac_scaled(PBUF[:, 0:256], ps[0][:, 0:256], ('ps', 0), 'pbuf', 1.0)
                    qv = PBUF[:, 0:256].rearrange("p (h d) -> p h d", h=1)
                    head_rms(qv, 1, 256, MQN, 'pbuf')
                    CP('dve', PB16[:, 0:256], PBUF[:, 0:256], ['pbuf'], ['pb16'])
                    yield
                    transposes_to(lambda b: DTT[:, b, :], PB16, 2, 128, 'pb16', 'dtt')
                    for k in range(2):
                        MM(ps[0][:, 0:384], DTT[:, k, :], WQB[:, k, :], k == 0, k == 1, ['dtt', 'wqb'], [('ps', 0)])
                    evac_scaled(PBUF[:, 0:384], ps[0][:, 0:384], ('ps', 0), 'pbuf', qscale)
                    q4 = PBUF[:, 0:384].rearrange("p (h d) -> p h d", h=4)
                    q16 = PB16[:, 0:512].rearrange("p (h d) -> p h d", h=4)
                    P.op('dve', lambda e: e.memset(PB16[:, 0:512], 0.0), [], ['pb16'])
                    CP('pool', q16[:, :, 0:64], q4[:, :, 0:64], ['pbuf'], ['pb16'])
                    if lat:
                        rope(q16[:, :, 64:96], q4[:, :, 64:96], ROPER, t, 4, 16, 'pbuf', 'pb16')
                    else:
                        CP('dve', q16[:, :, 64:96], q4[:, :, 64:96], ['pbuf'], ['pb16'])
                    yield
                    transposes_to(lambda b: QTv[:, b, :], PB16, 4, 128, 'pb16', qkey)
                else:
                    evac_scaled(PBUF[:, 0:256], ps[0][:, 0:256], ('ps', 0), 'pbuf', qscale)
                    q4 = PBUF[:, 0:256].rearrange("p (h d) -> p h d", h=4)
                    q16 = PB16[:, 0:256].rearrange("p (h d) -> p h d", h=4)
                    if m == 1:
                        head_rms(q4, 4, 64, GQN, 'pbuf')
                        P.op('dve', lambda e: e.tensor_scalar(PBUF[:, 0:256], PBUF[:, 0:256], 0.125, None, ALU.mult),
                             ['pbuf'], ['pbuf'])
                    if lat and m in (1, 2):
                        rope(q16, q4, ROPEH, t, 4, 32, 'pbuf', 'pb16')
                    else:
                        CP('dve', q16, q4, ['pbuf'], ['pb16'])
                    yield
                    transposes_to(lambda b: QTv[0:64, b, :], PB16, 4, 64, 'pb16', qkey)

            def q_body(t, QTv, qkey, nxt):
                lat = t >= 2
                kts = kv_tiles(m, t, lastl)
                o_i = 4 if (att_i[0] % 2 == 0) else 7
                att_i[0] += 1
                OPS = ps[o_i][:, 0:260].rearrange("p (h d) -> p h d", h=4)
                items = []
                for h in range(4):
                    for g0 in range(0, len(kts), 4):
                        items.append((h, g0, kts[g0:g0 + 4]))

                def emit_qk(ix):
                    h, g0, grp = items[ix]
                    kvh = h if nkv == 4 else h // 2
                    sb_i = 2 + (ix % 2)
                    Sv = ps[sb_i].rearrange("p (g q) -> p g q", g=4)
                    for gi_, (kt, bspec) in enumerate(grp):
                        MM(Sv[:, gi_, :], KT[0:hd, kvh, kt * 128:(kt + 1) * 128], QTv[0:hd, h, :], True, bspec is None,
                           ['kt', qkey], [('ps', sb_i)])
                        if bspec is not None:
                            brhs = BIAS[:, bspec[1] * 4 + h, :] if bspec[0] == 'A' else MASKC[:, bspec[1], :]
                            MM(Sv[:, gi_, :], IDB, brhs, False, True, ['idb', 'bias', 'maskc'], [('ps', sb_i)])

                def emit_exp_pv(ix):
                    h, g0, grp = items[ix]
                    kvh = h if nkv == 4 else h // 2
                    sb_i = 2 + (ix % 2)
                    pt_i = ix % 2
                    Sv = ps[sb_i].rearrange("p (g q) -> p g q", g=4)
                    ng = len(grp)
                    ACT(PT[:, pt_i, 0:ng, :], Sv[:, 0:ng, :], AF.Exp, [('ps', sb_i)], [('pt', pt_i)])
                    for gi_, (kt, bspec) in enumerate(grp):
                        first = (g0 == 0 and gi_ == 0)
                        last = (g0 + gi_ == len(kts) - 1)
                        MM(OPS[:, h, :], PT[:, pt_i, gi_, :], VA[:, kt, kvh, :], first, last,
                           [('pt', pt_i), 'va'], [('ps', o_i)])

                step_pts = set([0, len(items) // 3, (2 * len(items)) // 3])
                emit_qk(0)
                for ix in range(len(items)):
                    if nxt is not None and ix in step_pts:
                        next(nxt, None)
                    if ix + 1 < len(items):
                        emit_qk(ix + 1)
                    emit_exp_pv(ix)
                if nxt is not None:
                    for _ in nxt:
                        pass
                den = RS[:, 32:36]
                if m == 2:
                    TT('dve', den, OPS[:, :, 64], SKE, ALU.add, [('ps', o_i), 'ske'], ['rs2'])
                else:
                    CP('dve', den, OPS[:, :, 64], [('ps', o_i)], ['rs2'])
                RCP(RS[:, 36:40], den, ['rs2'], ['rs2'])
                TT('dve', OB.rearrange("p (h d) -> p h d", h=4), OPS[:, :, 0:64],
                   RS[:, 36:40].rearrange("p (h o) -> p h o", o=1).to_broadcast([128, 4, 64]), ALU.mult,
                   [('ps', o_i), 'rs2'], ['ob'])
                transposes_to(lambda b: OT[:, b, :], OB, 2, 128, 'ob', 'ot', bank_i=1)
                for hf in range(2):
                    cs = slice(hf * 512, (hf + 1) * 512)
                    for k in range(8):
                        MM(ps[5], HT[:, k, t * 128:(t + 1) * 128], WG[:, k, cs], k == 0, k == 7,
                           [('ht', t), 'wg'], [('ps', 5)])
                    for k in range(2):
                        MM(ps[6], OT[:, k, :], WBR[:, k, cs], k == 0, k == 1, ['ot', 'wbr'], [('ps', 6)])
                    ACT(SIG[:, cs], ps[5], AF.Sigmoid, [('ps', 5)], ['sig'])
                    TT('dve', MTL[:, cs], ps[6], SIG[:, cs], ALU.mult, [('ps', 6), 'sig'], ['mtl'])
                DMA(md[m, t * 128:(t + 1) * 128, :], MTL, ['mtl'], [('md', m, t)])
                if m == 0 and t == 0:
                    ck(5)
                if m == 3 and t == 0:
                    ck(11)
                if m == 3 and t == 2:
                    ck(12)
            g0_ = q_prep(q_tiles[0], QT[:, 0], ('qt', 0))
            for _ in g0_:
                pass
            for idx_, t_ in enumerate(q_tiles):
                nx_ = None
                if idx_ + 1 < len(q_tiles):
                    nx_ = q_prep(q_tiles[idx_ + 1], QT[:, (idx_ + 1) % 2], ('qt', (idx_ + 1) % 2))
                q_body(t_, QT[:, idx_ % 2], ('qt', idx_ % 2), nx_)
            ck(6 + m)
        P.barrier()

        mod_tiles(l, 2 * D, 'gate', 0, which=(0,) if lastl else (0, 1))
        load_w(w_out[l], 8, D, WO, 'wo')
        for t in q_tiles:
            w = 0 if t >= 2 else 1
            DMA(M4, md[:, t * 128:(t + 1) * 128, :].rearrange("m p d -> p m d"),
                [('md', mm_, t) for mm_ in range(4)], ['m4'])
            DMA(XT, x_src(l)[t * 128:(t + 1) * 128, :], [], ['xt'])
            TT('dve', M4[:, 0, :], M4[:, 0, :], M4[:, 1, :], ALU.add, ['m4'], ['m4'])
            TT('pool', M4[:, 2, :], M4[:, 2, :], M4[:, 3, :], ALU.add, ['m4'], ['m4b'])
            TT('dve', M4[:, 0, :], M4[:, 0, :], M4[:, 2, :], ALU.add, ['m4', 'm4b'], ['m4'])
            pb = ps[1].bitcast(BF16)
            for k in range(8):
                TR(pb[:, k * 128:(k + 1) * 128], M4[:, 0, k * 128:(k + 1) * 128], IDB, ['m4', 'idb'], [('ps', 1)])
            CP('act', MT8, pb.rearrange("p (k q) -> p k q", k=8), [('ps', 1)], ['mt8'])
            for hf in range(2):
                cs = slice(hf * 512, (hf + 1) * 512)
                for k in range(8):
                    MM(ps[5 + hf], MT8[:, k, :], WO[:, k, cs], k == 0, k == 7, ['mt8', 'wo'], [('ps', 5 + hf)])
                TT('dve', HB[:, cs], ps[5 + hf], MOD[:, w, cs], ALU.mult, [('ps', 5 + hf), ('mod', w)], ['hb'])
                TT('pool', XS[:, t, cs], HB[:, cs], XT[:, cs], ALU.add, ['hb', 'xt'], [('x', t)])
        P.barrier()
        if dbg == ('xm', l):
            for t in q_tiles:
                DMA(dbg_out[t * 128:(t + 1) * 128, :], XS[:, t, :], [('x', t)], ['dbgo'])
            return True

        moe = (l % 2 == 1)
        mod_tiles(l, 4 * D, 'scale', 0, norm2_g[l:l + 1, :], which=(0,) if lastl else (0, 1))
        mod_tiles(l, 3 * D, 'shift', 2, which=(0,) if lastl else (0, 1))
        if moe:
            DMA(RT, moe_router[l // 2].rearrange("(k p) e -> p k e", p=128), [], ['rt'])
            P.op('dve', lambda e: e.memset(RTB, 0.0), [], ['rtb'])
            CP('dve', RTB[:, :, 0:8], RT, ['rt', 'rtb'], ['rtb'])
        for t in q_tiles:
            norm_tile(t, XS[:, t, :], [('x', t)], 0, 2, router=False)
            if moe:
                lg = ps[7][:, 0:NEXP]
                for k in range(8):
                    MM(ps[7][:, 0:16], HT[:, k, t * 128:(t + 1) * 128], RTB[:, k, :], k == 0, k == 7, [('ht', t), 'rtb'], [('ps', 7)])
                L = RS[:, 0:8]
                CP('dve', L, lg, [('ps', 7)], ['rs'])
                RED(RS[:, 8:9], L, ALU.max, ['rs'], ['rs'])
                TT('dve', RS[:, 16:24], L, RS[:, 8:9].to_broadcast([128, 8]), ALU.is_equal, ['rs'], ['rs'])
                STT(RS[:, 24:32], RS[:, 16:24], -1e30, L, ALU.mult, ALU.add, ['rs'], ['rs'])
                RED(RS[:, 9:10], RS[:, 24:32], ALU.max, ['rs'], ['rs'])
                TT('dve', RS[:, 40:48], RS[:, 24:32], RS[:, 9:10].to_broadcast([128, 8]), ALU.is_equal, ['rs'], ['rs'])
                TT('dve', RS[:, 10:11], RS[:, 9:10], RS[:, 8:9], ALU.subtract, ['rs'], ['rs'])
                ACT(RS[:, 11:12], RS[:, 10:11], AF.Exp, ['rs'], ['rs'])
                TS('dve', RS[:, 12:13], RS[:, 11:12], 1.0, None, ALU.add, None, ['rs'], ['rs'])
                RCP(RS[:, 13:14], RS[:, 12:13], ['rs'], ['rs'])
                TT('dve', RS[:, 14:15], RS[:, 11:12], RS[:, 13:14], ALU.mult, ['rs'], ['rs'])
                TT('dve', RS[:, 48:56], RS[:, 16:24], RS[:, 13:14].to_broadcast([128, 8]), ALU.mult, ['rs'], ['rs'])
                STT(COMB[:, t, :], RS[:, 40:48], RS[:, 14:15], RS[:, 48:56], ALU.mult, ALU.add, ['rs'], ['comb'])
                if t == 0:
                    ck(20)
        mod_tiles(l, 5 * D, 'gate', 0, which=(0,) if lastl else (0, 1))

        chunks = []
        if not lastl:
            chunks.append([0, 1])
        for c in range(4):
            chunks.append([2 + 4 * c + i for i in range(4)])
        nexp = NEXP if moe else 1

        def wset(s_):
            o = s_ * 6144
            return (R4[:, o:o + 2048].rearrange("p (k c) -> p k c", k=8),
                    R4[:, o + 2048:o + 4096].rearrange("p (k c) -> p k c", k=8),
                    R4[:, o + 4096:o + 6144].rearrange("p (k c) -> p k c", k=2),
                    MOD[:, 2 + s_, :].bitcast(BF16).rearrange("p (k c) -> p k c", k=2))

        def g2b(w):
            return MOD[:, w, :].rearrange("p (o d) -> p o d", o=1).to_broadcast([128, 2, D])

        gi = 0
        ci = 0
        for e_ in range(nexp):
            if moe:
                w1, w3, w2 = moe_w1[l // 2, e_], moe_w3[l // 2, e_], moe_w2[l // 2, e_]
            else:
                w1, w3, w2 = ffn_w1[l // 2], ffn_w3[l // 2], ffn_w2[l // 2]
            for g in range(DFF // 256):
                s_ = gi % 2
                gi += 1
                W1s, W3s, W2s, W2cs = wset(s_)
                load_w(w1[:, g * 256:(g + 1) * 256], 8, 256, None, None, cast=False,
                       use=lambda sv, c0, cw, skey: CP('act', W1s, sv, [skey], [('w1b', s_)]))
                load_w(w3[:, g * 256:(g + 1) * 256], 8, 256, None, None, cast=False,
                       use=lambda sv, c0, cw, skey: CP('act', W3s, sv, [skey], [('w3b', s_)]))

                def use_w2(sv, c0, cw, skey):
                    TT('pool', W2s, sv, g2b(0), ALU.mult, [skey, ('mod', 0)], [('w2b', s_)])
                    if not lastl:
                        TT('pool', W2cs, sv, g2b(1), ALU.mult, [skey, ('mod', 1)], [('mod', 2 + s_)])
                load_w(w2[g * 256:(g + 1) * 256, :], 2, D, None, None, cast=False, use=use_w2)
                for ch in chunks:
                    n = len(ch) * 128
                    t0 = ch[0] * 128
                    up = (ci % 2) * 2
                    ci += 1
                    for fc in range(2):
                        for k in range(8):
                            MM(ps[fc][:, 0:n], W1s[:, k, fc * 128:(fc + 1) * 128], HT[:, k, t0:t0 + n], k == 0, k == 7,
                               [('w1b', s_)] + [('ht', t) for t in ch], [('ps', fc)])
                        for k in range(8):
                            MM(ps[2 + fc][:, 0:n], W3s[:, k, fc * 128:(fc + 1) * 128], HT[:, k, t0:t0 + n], k == 0, k == 7,
                               [('w3b', s_)] + [('ht', t) for t in ch], [('ps', 2 + fc)])
                        ACT(SA[:, fc, 0:n], ps[fc][:, 0:n], AF.Silu, [('ps', fc)], [('sa', fc)])
                        TT('dve', UT[:, up + fc, 0:n], ps[2 + fc][:, 0:n], SA[:, fc, 0:n], ALU.mult,
                           [('ps', 2 + fc), ('sa', fc)], [('ut', up + fc)])
                    for ti, t in enumerate(ch):
                        ob_ = 4 + 2 * (ti % 2)
                        wsel, wkey = (W2s, ('w2b', s_)) if t >= 2 else (W2cs, ('mod', 2 + s_))
                        for hf in range(2):
                            cs = slice(hf * 512, (hf + 1) * 512)
                            for fc in range(2):
                                MM(ps[ob_ + hf], UT[:, up + fc, ti * 128:(ti + 1) * 128], wsel[:, fc, cs], fc == 0, fc == 1,
                                   [('ut', up + fc), wkey], [('ps', ob_ + hf)])
                            if moe:
                                STT(XS[:, t, cs], ps[ob_ + hf], COMB[:, t, e_:e_ + 1], XS[:, t, cs], ALU.mult, ALU.add,
                                    [('ps', ob_ + hf), 'comb', ('x', t)], [('x', t)])
                            else:
                                TT('dve', XS[:, t, cs], ps[ob_ + hf], XS[:, t, cs], ALU.add,
                                   [('ps', ob_ + hf), ('x', t)], [('x', t)])
        P.barrier()
        if dbg == ('xf', l):
            for t in q_tiles:
                DMA(dbg_out[t * 128:(t + 1) * 128, :], XS[:, t, :], [('x', t)], ['dbgo'])
            return True
        if not lastl:
            for t in range(NT):
                DMA(xd[t * 128:(t + 1) * 128, :], XS[:, t, :], [('x', t)], [('xd', t)])
            P.barrier()
        return False

    stopped = False
    try:
        for l in range(n_layers):
            stopped = layer(l)
            if stopped:
                break
    except _Stop:
        stopped = True
    if not stopped and n_layers == DEPTH:
        DMA(MOD[:, 0, :], final_g.partition_broadcast(128), [], [('mod', 0)])
        for t in range(2, NT):
            src = XS[:, t, :]
            ACT(HB, src, AF.Square, [('x', t)], ['hb'], accum_out=STAT[:, 0:1])
            ACT(STAT[:, 1:2], STAT[:, 0:1], AF.Sqrt, ['hb'], ['stat'], bias=EPSB, scale=1.0 / D)
            RCP(STAT[:, 2:3], STAT[:, 1:2], ['stat'], ['stat'])
            STT(HB, src, STAT[:, 2:3], MOD[:, 0, :], ALU.mult, ALU.mult, [('x', t), 'stat', ('mod', 0)], ['hb'])
            DMA(out[(t - 2) * 128:(t - 1) * 128, :], HB, ['hb'], ['out'])
    elif not stopped:
        DMA(out[0:128, :], HB, [], ['out'])
    P.barrier()
    P.emit()
    print("instructions:", P.n, {k: len(v) for k, v in P.streams.items()})


_CACHE = {}


def make_in_maps(inp, n_cores=8):
    f = lambda a: np.ascontiguousarray(np.asarray(a), dtype=np.float32)
    cosH, sinH, cosR, sinR = _rope_tables()
    shared = {
        "norm1_g": f(inp["norm1_g"]), "norm2_g": f(inp["norm2_g"]), "w_ada": f(inp["w_ada"]),
        "b_ada": f(inp["b_ada"]), "w_in": f(inp["w_in"]), "biasA": _natten_bias(f(inp["na_rpb"])),
        "smallp": np.ascontiguousarray(np.concatenate(
            [f(inp["gb_qnorm"]), f(inp["gb_knorm"]), f(inp["mla_qnorm"]), f(inp["mla_kvnorm"]), f(inp["wc_sink"])],
            axis=1)),
        "mla_wqb": f(inp["mla_wqb"]), "mla_wkvb": f(inp["mla_wkvb"]), "w_branch": f(inp["w_branch"]),
        "w_out": f(inp["w_out"]), "ffn_w1": f(inp["ffn_w1"]), "ffn_w3": f(inp["ffn_w3"]), "ffn_w2": f(inp["ffn_w2"]),
        "moe_router": f(inp["moe_router"]), "moe_w1": f(inp["moe_w1"]), "moe_w3": f(inp["moe_w3"]),
        "moe_w2": f(inp["moe_w2"]), "final_g": f(inp["final_g"]).reshape(1, D),
        "c_ident": np.eye(128, dtype=np.float32),
        "c_ropeH": np.ascontiguousarray(np.stack([cosH, sinH])),
        "c_ropeR": np.ascontiguousarray(np.stack([cosR, sinR])),
        "c_maskC": _maskC(),
    }
    x = f(inp["x"]); ctx = f(inp["ctx"]); c = f(inp["c"]); cc = f(inp["c_ctx"])
    maps = []
    for b in range(n_cores):
        m = dict(shared)
        m["xin"] = np.ascontiguousarray(np.concatenate([ctx[b], x[b]], axis=0))
        m["cvec"] = np.ascontiguousarray(np.stack([c[b], cc]))
        maps.append(m)
    return maps


def kernel(**inputs):
    if "nc" not in _CACHE:
        _CACHE["nc"] = build(DEPTH)
    nc = _CACHE["nc"]
    maps = make_in_maps(inputs, 8)
    res = run_bass_kernel_spmd(nc, maps, core_ids=list(range(8)))
    return np.stack([np.asarray(r["out"], dtype=np.float32) for r in res.results], axis=0)
```

```python
import numpy as np
from contextlib import ExitStack
import concourse.bass as bass
import concourse.mybir as mybir
from concourse.bass_utils import run_bass_kernel_spmd

F32 = mybir.dt.float32
BF16 = mybir.dt.bfloat16
AF = mybir.ActivationFunctionType
ALU = mybir.AluOpType
AX = mybir.AxisListType

D = 1024
S = 2048
C = 256
T = S + C
NT = T // 128
DEPTH = 4
IN_W = 6304
DFF = 3584
NEXP = 8
EPS = 1e-6
NEG = -30000.0
NDMA = 12
NSTG = 3

OFF = dict(aq=0, ak=256, av=512, bq=768, bk=1024, bv=1152, cq=1280, ck=1536, cv=1664,
           dqa=1792, dkva=2048, gate=2208)


class Prog:
    CE = ['pe', 'act', 'dve', 'pool']

    def __init__(self, nc, es):
        self.nc = nc
        self.streams = {e: [] for e in self.CE + ['sp']}
        self.sems = {}
        for e in self.CE:
            self.sems[e] = es.enter_context(nc.semaphore("s_" + e))
        for i in range(NDMA):
            self.sems['d%d' % i] = es.enter_context(nc.semaphore("s_d%d" % i))
        self.cnt = {k: 0 for k in self.sems}
        self.known = {e: {k: 0 for k in self.sems} for e in self.streams}
        self.lastw = {}
        self.readers = {}
        self.dma_rr = 0
        self.n = 0
        self.last_real = {e: None for e in self.CE}
        self.pend = {e: False for e in self.CE}

    def _materialize(self, s):
        if s in self.pend and self.pend[s]:
            self.streams[s][self.last_real[s]][2] = s
            self.cnt[s] += 1
            self.pend[s] = False

    def _deps(self, eng, reads, writes):
        deps = {}

        def add(d):
            if d is not None:
                if d[0] == 'pe' and eng == 'pe':
                    return
                if deps.get(d[0], 0) < d[1]:
                    deps[d[0]] = d[1]
        for k in reads:
            add(self.lastw.get(k))
        for k in writes:
            add(self.lastw.get(k))
            for r in self.readers.get(k, ()):
                add(r)
        waits = []
        kn = self.known[eng]
        for s, i in deps.items():
            if kn[s] < i:
                if i > self.cnt[s]:
                    self._materialize(s)
                assert i <= self.cnt[s], (s, i, self.cnt[s])
                kn[s] = i
                waits.append((s, i))
        return waits

    def _commit(self, tok, reads, writes):
        for k in writes:
            self.lastw[k] = tok
            self.readers[k] = []
        for k in reads:
            self.readers.setdefault(k, []).append(tok)

    def op(self, eng, fn, reads=(), writes=()):
        waits = self._deps(eng, reads, writes)
        tok = (eng, self.cnt[eng] + 1)
        self.streams[eng].append([waits, fn, None])
        self.last_real[eng] = len(self.streams[eng]) - 1
        self.pend[eng] = True
        self._commit(tok, reads, writes)
        self.n += 1

    def dma(self, fn, reads=(), writes=()):
        s = 'd%d' % self.dma_rr
        self.dma_rr = (self.dma_rr + 1) % NDMA
        waits = self._deps('sp', reads, writes)
        if self.known['sp'][s] < self.cnt[s]:
            self.known['sp'][s] = self.cnt[s]
            waits.append((s, self.cnt[s]))
        self.cnt[s] += 1
        tok = (s, self.cnt[s])
        self.streams['sp'].append([waits, fn, s])
        self._commit(tok, reads, writes)
        self.n += 1

    def barrier(self):
        for e in self.CE:
            self._materialize(e)
        snap = dict(self.cnt)
        for e in self.streams:
            waits = []
            for s, i in snap.items():
                if self.known[e][s] < i:
                    self.known[e][s] = i
                    waits.append((s, i))
            if waits:
                self.streams[e].append([waits, None, None])

    def emit(self):
        nc = self.nc
        sems = self.sems

        def mult(s):
            return 16 if s[0] == 'd' else 1

        def run(engine, stream):
            for waits, fn, inc in stream:
                for s, i in waits:
                    engine.wait_ge(sems[s], i * mult(s))
                if fn is None:
                    continue
                ins = fn(engine)
                if inc is not None:
                    ins.then_inc(sems[inc], mult(inc))

        with nc.Block() as block:
            @block.tensor
            def _(e):
                run(e, self.streams['pe'])

            @block.scalar
            def _(e):
                run(e, self.streams['act'])

            @block.vector
            def _(e):
                run(e, self.streams['dve'])

            @block.gpsimd
            def _(e):
                run(e, self.streams['pool'])

            @block.sync
            def _(e):
                run(e, self.streams['sp'])


def _rope_tables():
    def ang(pos, dim):
        inv = (10000.0 ** (-np.arange(0, dim, 2, dtype=np.float32) / dim)).astype(np.float32)
        return pos.astype(np.float32)[:, None] * inv[None, :]
    t = np.arange(S)
    out = []
    for rot in (64, 32):
        half = rot // 2
        a = np.concatenate([ang(t // 64, half), ang(t % 64, half)], axis=-1).astype(np.float32)
        out += [np.cos(a).astype(np.float32), np.sin(a).astype(np.float32)]
    return out


def _natten_plan():
    rows = S // 64
    kh = 8
    pats = {}
    plan = []
    for i in range(16):
        lst = []
        for j in range(16):
            sig = []
            anyv = False
            for a in range(2):
                for b in range(2):
                    r = 2 * i + b
                    rk = 2 * j + a
                    st = min(max(r - kh // 2, 0), rows - kh)
                    if st <= rk < st + kh:
                        sig.append(rk - r + 7)
                        anyv = True
                    else:
                        sig.append(-1)
            if anyv:
                sig = tuple(sig)
                if sig not in pats:
                    pats[sig] = len(pats)
                lst.append((j, pats[sig]))
        plan.append(lst)
    return plan, pats


NAT_PLAN, NAT_PATS = _natten_plan()
NPAT = len(NAT_PATS)


def _natten_bias(rpb):
    col = np.arange(64)
    cstart = np.clip(col - 8, 0, 48)
    col_ok = (col[None, :] >= cstart[:, None]) & (col[None, :] < cstart[:, None] + 16)
    dc = np.clip(col[None, :] - col[:, None] + 15, 0, 30)
    out = np.full((DEPTH, 128, NPAT * 4, 128), NEG, dtype=np.float32)
    for sig, pid in NAT_PATS.items():
        for a in range(2):
            for b in range(2):
                dr = sig[a * 2 + b]
                if dr < 0:
                    continue
                g = rpb[:, :, dr, :][:, :, dc]
                g = np.where(col_ok[None, None], g, np.float32(NEG))
                g = np.transpose(g, (0, 3, 1, 2))
                out[:, a * 64:(a + 1) * 64, pid * 4:(pid + 1) * 4, b * 64:(b + 1) * 64] = g
    return out


def _maskC():
    kk = np.arange(128)[:, None]
    qq = np.arange(128)[None, :]
    m0 = np.where(qq <= kk, 0.0, NEG).astype(np.float32)
    m1 = np.where(kk <= qq, 0.0, NEG).astype(np.float32)
    return np.stack([m0, m1], axis=1)


def build(n_layers=DEPTH, dbg=None):
    nc = bass.Bass("TRN2", target_bir_lowering=False, dynamic_dma_scratch_size=256)
    es = ExitStack()
    with es:
        _build_body(nc, es, n_layers, dbg)
    return nc


def _build_body(nc, es, n_layers, dbg):
    def din(name, shape):
        return nc.dram_tensor(name, list(shape), F32, kind="ExternalInput").ap()

    xin = din("xin", [T, D])
    cvec = din("cvec", [2, D])
    norm1_g = din("norm1_g", [DEPTH, D])
    norm2_g = din("norm2_g", [DEPTH, D])
    w_ada = din("w_ada", [DEPTH, D, 6 * D])
    b_ada = din("b_ada", [DEPTH, 6 * D])
    w_in = din("w_in", [DEPTH, D, IN_W])
    biasA = din("biasA", [DEPTH, 128, NPAT * 4, 128])
    smallp = din("smallp", [DEPTH, 516])
    mla_wqb = din("mla_wqb", [DEPTH, 256, 384])
    mla_wkvb = din("mla_wkvb", [DEPTH, 128, 512])
    w_branch = din("w_branch", [DEPTH, 4, 256, D])
    w_out = din("w_out", [DEPTH, D, D])
    ffn_w1 = din("ffn_w1", [2, D, DFF])
    ffn_w3 = din("ffn_w3", [2, D, DFF])
    ffn_w2 = din("ffn_w2", [2, DFF, D])
    moe_router = din("moe_router", [2, D, NEXP])
    moe_w1 = din("moe_w1", [2, NEXP, D, DFF])
    moe_w3 = din("moe_w3", [2, NEXP, D, DFF])
    moe_w2 = din("moe_w2", [2, NEXP, DFF, D])
    final_g = din("final_g", [1, D])
    c_ident = din("c_ident", [128, 128])
    c_ropeH = din("c_ropeH", [2, S, 32])
    c_ropeR = din("c_ropeR", [2, S, 16])
    c_maskC = din("c_maskC", [128, 2, 128])
    out = nc.dram_tensor("out", [S, D], F32, kind="ExternalOutput").ap()
    xd = nc.dram_tensor("xd", [T, D], F32, kind="Internal").ap()
    md = nc.dram_tensor("md", [4, T, D], BF16, kind="Internal").ap()
    dbg_out = None
    if dbg is not None:
        dbg_out = nc.dram_tensor("dbg", [T, D], F32, kind="ExternalOutput").ap()

    P = Prog(nc, es)

    def sb(name, shape, dt):
        return es.enter_context(nc.sbuf_tensor(name, list(shape), dt))[:]

    ps = [es.enter_context(nc.psum_tensor("ps%d" % i, [128, 512], F32))[:] for i in range(8)]

    IDF = sb("idf", [128, 128], F32)
    IDB = sb("idb", [128, 128], BF16)
    ROPEH = sb("ropeh", [128, 2, 16, 32], F32)
    ROPER = sb("roper", [128, 2, 16, 16], F32)
    MASKC = sb("maskc", [128, 2, 128], BF16)
    CV = sb("cv", [128, 2, 8], F32)
    SL = sb("sl", [128, 2, 8, 128], F32)
    MOD = sb("mod", [128, 4, D], F32)
    SMP = sb("smp", [128, 516], F32)
    SKE = sb("ske", [128, 4], F32)
    HT = sb("ht", [128, 8, T], BF16)
    STG = sb("stg", [128, NSTG, 2048], F32)
    XT = sb("xt", [128, D], F32)
    HB = sb("hb", [128, D], F32)
    PB = HB
    STAT = sb("stat", [128, 32], F32)
    PBUF = sb("pbuf", [128, D], F32)
    TMPF = sb("tmpf", [128, 4, 256], F32)
    PB16 = sb("pb16", [128, 512], BF16)
    QT = sb("qt", [128, 2, 4, 128], BF16)
    OB = sb("ob", [128, 256], BF16)
    OT = sb("ot", [128, 2, 128], BF16)
    DTT = sb("dtt", [128, 2, 128], BF16)
    MT8 = sb("mt8", [128, 8, 128], BF16)
    RT = sb("rt", [128, 8, NEXP], F32)
    RTB = sb("rtb", [128, 8, 16], BF16)
    COMB = sb("comb", [128, NT, NEXP], F32)
    RS = sb("rs", [128, 64], F32)
    UT = sb("ut", [128, 4, 512], BF16)
    SA = sb("sa", [128, 2, 512], BF16)
    R4 = sb("r4", [128, 12288], BF16)
    R2 = sb("r2", [128, 36864], BF16)

    KT = R2[:, 0:9216].rearrange("p (h t) -> p h t", h=4)
    VA = R2[:, 9216:13896].rearrange("p (t h d) -> p t h d", t=NT, h=4)
    BIAS = R2[:, 13896:13896 + NPAT * 4 * 128].rearrange("p (n q) -> p n q", q=128)
    XS = R2.bitcast(F32).rearrange("p (t d) -> p t d", t=NT)
    b0 = 13896 + NPAT * 4 * 128
    WG = R2[:, b0:b0 + 8192].rearrange("p (k c) -> p k c", k=8)
    WKV = WG
    WQ = R2[:, b0 + 8192:b0 + 10240].rearrange("p (k c) -> p k c", k=8)
    WBR = R2[:, b0 + 10240:b0 + 12288].rearrange("p (k c) -> p k c", k=2)
    WQB = R2[:, b0 + 12288:b0 + 13056].rearrange("p (k c) -> p k c", k=2)
    WKVB = R2[:, b0 + 13056:b0 + 13568]
    SIG = R2[:, b0 + 13568:b0 + 14592]
    MTL = R2[:, b0 + 14592:b0 + 15616]
    PT = R2[:, b0 + 15616:b0 + 17152].rearrange("p (a g q) -> p a g q", a=3, g=4)
    assert b0 + 17152 <= 36864
    WO = R4[:, 0:8192].rearrange("p (k c) -> p k c", k=8)
    M4 = MOD[:, 2:4, :].rearrange("p a d -> p (a d)").bitcast(BF16).rearrange("p (a d) -> p a d", a=4)
    H2F = PBUF.rearrange("p (k q) -> p k q", k=8)
    W1B = R4[:, 0:4096].rearrange("p (k c) -> p k c", k=8)
    W3B = R4[:, 4096:8192].rearrange("p (k c) -> p k c", k=8)
    W2B = R4[:, 8192:12288].rearrange("p (k c) -> p k c", k=4)

    def MM(o, lhsT, rhs, start, stop, r, w):
        P.op('pe', lambda e: e.matmul(o, lhsT, rhs, start=start, stop=stop), r, w)

    def TR(o, i, ident, r, w):
        P.op('pe', lambda e: e.transpose(o, i, ident), r, w)

    def ACT(o, i, func, r, w, **kw):
        P.op('act', lambda e: e.activation(o, i, func, **kw), r, w)

    def TT(eng, o, a, b, op, r, w):
        P.op(eng, lambda e: e.tensor_tensor(o, a, b, op), r, w)

    def TS(eng, o, a, s1, s2, op0, op1, r, w):
        if op1 is None:
            P.op(eng, lambda e: e.tensor_scalar(o, a, s1, None, op0), r, w)
        else:
            P.op(eng, lambda e: e.tensor_scalar(o, a, s1, s2, op0, op1), r, w)

    def STT(o, a, s, b, op0, op1, r, w):
        P.op('dve', lambda e: e.scalar_tensor_tensor(o, a, s, b, op0, op1), r, w)

    def CP(eng, o, i, r, w):
        if eng == 'act':
            P.op('act', lambda e: e.activation(o, i, AF.Copy), r, w)
        else:
            P.op(eng, lambda e: e.tensor_copy(o, i), r, w)

    def RED(o, i, op, r, w):
        P.op('dve', lambda e: e.tensor_reduce(o, i, AX.X, op), r, w)

    def RCP(o, i, r, w):
        P.op('dve', lambda e: e.reciprocal(o, i), r, w)

    def DMA(o, i, r, w):
        P.dma(lambda e: e.dma_start(out=o, in_=i), r, w)

    stg_rr = [0]

    def load_w(src, kc, ncols, dst, dkey, cast=True, use=None):
        pcw = 2048 // kc
        c0 = 0
        while c0 < ncols:
            cw = min(pcw, ncols - c0)
            s = stg_rr[0]
            stg_rr[0] = (s + 1) % NSTG
            sv = STG[:, s, 0:kc * cw].rearrange("p (k c) -> p k c", k=kc)
            DMA(sv, src[:, c0:c0 + cw].rearrange("(k p) c -> p k c", p=128), [], [('stg', s)])
            if cast:
                CP('pool', dst[:, :, c0:c0 + cw], sv, [('stg', s)], [dkey])
            else:
                use(sv, c0, cw, ('stg', s))
            c0 += cw

    import os
    KS = int(os.environ.get("KSETUP", "99"))
    if KS > 0:
        DMA(IDF, c_ident, [], ['idf'])
        CP('dve', IDB, IDF, ['idf'], ['idb'])
    if KS > 1:
        DMA(ROPEH[:, 0], c_ropeH[0].rearrange("(t p) d -> p t d", p=128), [], ['rope'])
        DMA(ROPEH[:, 1], c_ropeH[1].rearrange("(t p) d -> p t d", p=128), [], ['rope'])
        DMA(ROPER[:, 0], c_ropeR[0].rearrange("(t p) d -> p t d", p=128), [], ['rope'])
        DMA(ROPER[:, 1], c_ropeR[1].rearrange("(t p) d -> p t d", p=128), [], ['rope'])
    if KS > 2:
        DMA(PBUF[:, 0:256].rearrange("p (a q) -> p a q", a=2), c_maskC, [], ['pbuf'])
        CP('dve', MASKC, PBUF[:, 0:256].rearrange("p (a q) -> p a q", a=2), ['pbuf'], ['maskc'])
    if KS > 3:
        P.dma(lambda e: e.dma_start(out=CV, in_=cvec.rearrange("w (k p) -> p w k", p=128),
                                    allow_slow_non_contiguous=True), [], ['cv'])
    if KS > 4:
        ACT(CV, CV, AF.Silu, ['cv'], ['cv'])
    if KS > 5:
        for wch in range(2):
            CP('dve', SL[:, wch], CV[:, wch, :].rearrange("p (k o) -> p k o", o=1).to_broadcast([128, 8, 128]),
               ['cv'], ['sl'])

    def rope_view(tab, which, t, nh, half):
        return tab[:, which, t - 2, :].rearrange("p (o d) -> p o d", o=1).to_broadcast([128, nh, half])

    def rope(dst, src, tab, t, nh, half, skey, dkey):
        c = rope_view(tab, 0, t, nh, half)
        s = rope_view(tab, 1, t, nh, half)
        x1 = src[:, :, 0:half]
        x2 = src[:, :, half:2 * half]
        t1 = TMPF[:, 0, 0:nh * half].rearrange("p (h d) -> p h d", h=nh)
        t2 = TMPF[:, 1, 0:nh * half].rearrange("p (h d) -> p h d", h=nh)
        t3 = TMPF[:, 2, 0:nh * half].rearrange("p (h d) -> p h d", h=nh)
        t4 = TMPF[:, 3, 0:nh * half].rearrange("p (h d) -> p h d", h=nh)
        TT('dve', t1, x1, c, ALU.mult, [skey, 'rope'], ['tf0'])
        TT('pool', t2, x2, s, ALU.mult, [skey, 'rope'], ['tf1'])
        TT('dve', t3, x1, s, ALU.mult, [skey, 'rope'], ['tf2'])
        TT('pool', t4, x2, c, ALU.mult, [skey, 'rope'], ['tf3'])
        TT('dve', dst[:, :, 0:half], t1, t2, ALU.subtract, ['tf0', 'tf1'], [dkey])
        TT('dve', dst[:, :, half:2 * half], t3, t4, ALU.add, ['tf2', 'tf3'], [dkey])

    def head_rms(src, nh, hd, gain, skey):
        sq = TMPF[:, 0:1, :].rearrange("p a c -> p (a c)")[:, 0:nh * hd].rearrange("p (h d) -> p h d", h=nh)
        TT('dve', sq, src, src, ALU.mult, [skey], ['tf0'])
        RED(RS[:, 0:nh], sq, ALU.add, ['tf0'], ['rs'])
        ACT(RS[:, 8:8 + nh], RS[:, 0:nh], AF.Sqrt, ['rs'], ['rs'], bias=EPSB, scale=1.0 / hd)
        RCP(RS[:, 16:16 + nh], RS[:, 8:8 + nh], ['rs'], ['rs'])
        TT('dve', src, src, RS[:, 16:16 + nh].rearrange("p (h o) -> p h o", o=1).to_broadcast([128, nh, hd]),
           ALU.mult, [skey, 'rs'], [skey])
        TT('dve', src, src, gain.rearrange("p (o d) -> p o d", o=1).to_broadcast([128, nh, hd]),
           ALU.mult, [skey, 'smp'], [skey])

    EPSB = sb("epsb", [128, 1], F32)
    P.op('dve', lambda e: e.memset(EPSB, EPS), [], ['epsb'])

    def mod_tiles(l, col0, kind, slot, gain_src=None, which=(0, 1)):
        DMA(PB, b_ada[l:l + 1, col0:col0 + D].partition_broadcast(128), [], ['hb'])

        def use(sv, c0, cw, skey):
            for w in which:
                bank = ps[6 + w]
                for k in range(8):
                    MM(bank[:, 0:cw], SL[:, w, k, :], sv[:, k, :], k == 0, k == 7,
                       ['sl', skey], [('ps', 6 + w)])
                TT('dve', MOD[:, slot + w, c0:c0 + cw], bank[:, 0:cw], PB[:, c0:c0 + cw], ALU.add,
                   [('ps', 6 + w), 'hb'], [('mod', slot + w)])
        load_w(w_ada[l][:, col0:col0 + D], 8, D, None, None, cast=False, use=use)
        if kind == 'scale':
            DMA(PB, gain_src.partition_broadcast(128), [], ['hb'])
            for w in which:
                STT(MOD[:, slot + w], MOD[:, slot + w], 1.0, PB, ALU.add, ALU.mult,
                    [('mod', slot + w), 'hb'], [('mod', slot + w)])

    def norm_tile(t, src_ap, src_keys, gslot, sslot, router=False, xkey=None):
        w = 0 if t >= 2 else 1
        ACT(HB, src_ap, AF.Square, src_keys, ['hb'], accum_out=STAT[:, 0:1])
        ACT(STAT[:, 1:2], STAT[:, 0:1], AF.Sqrt, ['hb'], ['stat'], bias=EPSB, scale=1.0 / D)
        RCP(STAT[:, 2:3], STAT[:, 1:2], ['stat'], ['stat'])
        STT(HB, src_ap, STAT[:, 2:3], MOD[:, gslot + w], ALU.mult, ALU.mult,
            src_keys + ['stat', ('mod', gslot + w)], ['hb'])
        TT('dve', HB, HB, MOD[:, sslot + w], ALU.add, ['hb', ('mod', sslot + w)], ['hb'])
        for half in range(2):
            bank = ps[half]
            for kk in range(4):
                k = half * 4 + kk
                TR(bank[:, kk * 128:(kk + 1) * 128], HB[:, k * 128:(k + 1) * 128], IDF,
                   ['hb', 'idf'], [('ps', half)])
            pv = bank.rearrange("p (k q) -> p k q", k=4)
            CP('act' if half == 0 else 'dve', HT[:, half * 4:half * 4 + 4, t * 128:(t + 1) * 128], pv,
               [('ps', half)], [('ht', t)])
            if router:
                CP('dve' if half == 0 else 'act', H2F[:, half * 4:half * 4 + 4, :], pv,
                   [('ps', half)], ['h2f'])


    class _Stop(Exception):
        pass
    KSTOP = int(os.environ.get('KSTOP', '0'))

    def ck(n):
        if KSTOP == n:
            raise _Stop()

    def x_src(l):
        return xin if l == 0 else xd

    def kv_tiles(m, t, lastl):
        if t < 2:
            return [(0, None), (1, None)]
        i = t - 2
        if m == 0:
            return [(j + 2, ('A', pid)) for (j, pid) in NAT_PLAN[i]] + [(0, None), (1, None)]
        if m == 2:
            lst = []
            if i - 1 >= 0:
                lst.append((t - 1, ('C', 0)))
            lst.append((t, None))
            if i + 1 < 16:
                lst.append((t + 1, ('C', 1)))
            return lst + [(0, None), (1, None)]
        return [(j, None) for j in range(NT)]

    GQN = SMP[:, 0:64]
    GKN = SMP[:, 64:128]
    MQN = SMP[:, 128:384]
    MKVN = SMP[:, 384:512]

    def evac_scaled(dst, src_ps, pkey, dkey, scale):
        P.op('act', lambda e: e.activation(dst, src_ps, AF.Copy, scale=scale), [pkey], [dkey])

    def transposes_to(dst_fn, src16, nblk, width, skey, dkey, bank_i=1, half=False):
        pb = ps[bank_i].bitcast(BF16)
        pkey = ('ps', bank_i)
        if half:
            pb = ps[0].bitcast(BF16)[:, 512:1024]
            pkey = ('ps', '0b')
        for b in range(nblk):
            TR(pb[0:width, b * 128:(b + 1) * 128], src16[:, b * width:(b + 1) * width], IDB,
               [skey, 'idb'], [pkey])
        for b in range(nblk):
            CP('act' if b % 2 == 0 else 'dve', dst_fn(b), pb[0:width, b * 128:(b + 1) * 128],
               [pkey], [dkey])

    def proj(bank_i, t, wview, ncols, wkey):
        for k in range(8):
            MM(ps[bank_i][:, 0:ncols], HT[:, k, t * 128:(t + 1) * 128], wview[:, k, 0:ncols], k == 0, k == 7,
               [('ht', t), wkey], [('ps', bank_i)])

    att_i = [0]

    def layer(l):
        lastl = (l == DEPTH - 1)
        q_tiles = list(range(2, NT)) if lastl else list(range(NT))
        DMA(SMP, smallp[l:l + 1, :].partition_broadcast(128), [], ['smp'])
        ACT(SKE, SMP[:, 512:516], AF.Exp, ['smp'], ['ske'])
        mod_tiles(l, 1 * D, 'scale', 0, norm1_g[l:l + 1, :])
        mod_tiles(l, 0 * D, 'shift', 2)
        ck(1)
        for t in range(NT):
            DMA(XT, x_src(l)[t * 128:(t + 1) * 128, :], [], ['xt'])
            norm_tile(t, XT, ['xt'], 0, 2)
        P.barrier()
        ck(2)

        for m in [int(c_) for c_ in os.environ.get('KMIX', '0123')]:
            nh = 4
            nkv = 4 if m in (0, 3) else 2
            hd = 128 if m == 3 else 64
            if m == 0:
                load_w(w_in[l][:, OFF['ak']:OFF['ak'] + 512], 8, 512, WKV, 'wg')
                DMA_bias = True
                for c0 in range(0, NPAT * 4, 16):
                    c1 = min(c0 + 16, NPAT * 4)
                    s_ = stg_rr[0]
                    stg_rr[0] = (s_ + 1) % NSTG
                    sv = STG[:, s_, 0:(c1 - c0) * 128].rearrange("p (n q) -> p n q", q=128)
                    DMA(sv, biasA[l][:, c0:c1, :], [], [('stg', s_)])
                    CP('pool', BIAS[:, c0:c1, :], sv, [('stg', s_)], ['bias'])
                kvw = 512
            elif m == 1:
                load_w(w_in[l][:, OFF['bk']:OFF['bk'] + 256], 8, 256, WKV, 'wg')
                kvw = 256
            elif m == 2:
                load_w(w_in[l][:, OFF['ck']:OFF['ck'] + 256], 8, 256, WKV, 'wg')
                kvw = 256
            else:
                load_w(w_in[l][:, OFF['dkva']:OFF['dkva'] + 160], 8, 160, WKV, 'wg')
                load_w(mla_wkvb[l], 1, 512, WKVB.rearrange("p (k c) -> p k c", k=1), 'wkvb')
                kvw = 160
            if m == 0:
                ck(3)
            P.op('dve', lambda e: e.memset(VA[:, :, :, 64:65], 1.0), [], ['va'])
            for t in range(NT):
                lat = t >= 2
                proj(0, t, WKV, kvw, 'wg')
                CP('act', PBUF[:, 0:kvw], ps[0][:, 0:kvw], [('ps', 0)], ['pbuf'])
                if m == 0:
                    CP('dve', PB16[:, 0:256], PBUF[:, 0:256], ['pbuf'], ['pb16'])
                    CP('pool', VA[:, t, :, 0:64], PBUF[:, 256:512].rearrange("p (h d) -> p h d", h=4), ['pbuf'], ['va'])
                    transposes_to(lambda b: KT[0:64, b, t * 128:(t + 1) * 128], PB16, 4, 64, 'pb16', 'kt')
                elif m in (1, 2):
                    kview = PBUF[:, 0:128].rearrange("p (h d) -> p h d", h=2)
                    if m == 1:
                        head_rms(kview, 2, 64, GKN, 'pbuf')
                    k16 = PB16[:, 0:128].rearrange("p (h d) -> p h d", h=2)
                    if lat:
                        rope(k16, kview, ROPEH, t, 2, 32, 'pbuf', 'pb16')
                    else:
                        CP('dve', k16, kview, ['pbuf'], ['pb16'])
                    CP('pool', VA[:, t, 0:2, 0:64], PBUF[:, 128:256].rearrange("p (h d) -> p h d", h=2), ['pbuf'], ['va'])
                    transposes_to(lambda b: KT[0:64, b, t * 128:(t + 1) * 128], PB16, 2, 64, 'pb16', 'kt')
                else:
                    cview = PBUF[:, 0:128].rearrange("p (h d) -> p h d", h=1)
                    head_rms(cview, 1, 128, MKVN, 'pbuf')
                    CP('dve', PB16[:, 0:128], PBUF[:, 0:128], ['pbuf'], ['pb16'])
                    transposes_to(lambda b: DTT[:, 0, :], PB16, 1, 128, 'pb16', 'dtt')
                    ck(14)
                    MM(ps[2][:, 0:512], DTT[:, 0, :], WKVB, True, True, ['dtt', 'wkvb'], [('ps', 2)])
                    ck(15)
                    kf = PB16[:, 0:512].rearrange("p (h d) -> p h d", h=4)
                    P.op('dve', lambda e: e.memset(PB16[:, 0:512], 0.0), [], ['pb16'])
                    CP('act', PBUF[:, 512:1024], ps[2], [('ps', 2)], ['pbuf2'])
                    dkv = PBUF[:, 512:1024].rearrange("p (h d) -> p h d", h=4)
                    CP('pool', kf[:, :, 0:64], dkv[:, :, 0:64], ['pbuf2'], ['pb16'])
                    CP('pool', VA[:, t, :, 0:64], dkv[:, :, 64:128], ['pbuf2'], ['va'])
                    ck(16)
                    pe_src = PBUF[:, 128:160].rearrange("p (h d) -> p h d", h=1)
                    pe_dst = PBUF[:, 160:192].rearrange("p (h d) -> p h d", h=1)
                    if lat:
                        rope(pe_dst, pe_src, ROPER, t, 1, 16, 'pbuf', 'pbuf')
                    else:
                        CP('dve', pe_dst, pe_src, ['pbuf'], ['pbuf'])
                    ck(17)
                    CP('dve', kf[:, :, 64:96], pe_dst.to_broadcast([128, 4, 32]), ['pbuf'], ['pb16'])
                    ck(18)
                    transposes_to(lambda b: KT[:, b, t * 128:(t + 1) * 128], PB16, 4, 128, 'pb16', 'kt')
            if m == 0:
                ck(4)
            if m == 3:
                ck(10)
            qoff = [OFF['aq'], OFF['bq'], OFF['cq'], OFF['dqa']][m]
            load_w(w_in[l][:, qoff:qoff + 256], 8, 256, WQ, 'wq')
            if m == 3:
                load_w(mla_wqb[l], 2, 384, WQB, 'wqb')
            load_w(w_in[l][:, OFF['gate'] + m * D:OFF['gate'] + (m + 1) * D], 8, D, WG, 'wg')
            load_w(w_branch[l, m], 2, D, WBR, 'wbr')
            def q_prep(t, QTv, qkey):
                lat = t >= 2
                proj(0, t, WQ, 256, 'wq')
                qscale = 0.125 if m != 3 else float(96 ** -0.5)
                if m == 3:
                    evac_scaled(PBUF[:, 0:256], ps[0][:, 0:256], ('ps', 0), 'pbuf', 1.0)
                    qv = PBUF[:, 0:256].rearrange("p (h d) -> p h d", h=1)
                    head_rms(qv, 1, 256, MQN, 'pbuf')
                    CP('dve', PB16[:, 0:256], PBUF[:, 0:256], ['pbuf'], ['pb16'])
                    yield
                    transposes_to(lambda b: DTT[:, b, :], PB16, 2, 128, 'pb16', 'dtt', half=True)
                    for k in range(2):
                        MM(ps[5][:, 0:384], DTT[:, k, :], WQB[:, k, :], k == 0, k == 1, ['dtt', 'wqb'], [('ps', 5)])
                    evac_scaled(PBUF[:, 0:384], ps[5][:, 0:384], ('ps', 5), 'pbuf', qscale)
                    q4 = PBUF[:, 0:384].rearrange("p (h d) -> p h d", h=4)
                    q16 = PB16[:, 0:512].rearrange("p (h d) -> p h d", h=4)
                    P.op('dve', lambda e: e.memset(PB16[:, 0:512], 0.0), [], ['pb16'])
                    CP('pool', q16[:, :, 0:64], q4[:, :, 0:64], ['pbuf'], ['pb16'])
                    if lat:
                        rope(q16[:, :, 64:96], q4[:, :, 64:96], ROPER, t, 4, 16, 'pbuf', 'pb16')
                    else:
                        CP('dve', q16[:, :, 64:96], q4[:, :, 64:96], ['pbuf'], ['pb16'])
                    yield
                    transposes_to(lambda b: QTv[:, b, :], PB16, 4, 128, 'pb16', qkey, half=True)
                else:
                    evac_scaled(PBUF[:, 0:256], ps[0][:, 0:256], ('ps', 0), 'pbuf', qscale)
                    q4 = PBUF[:, 0:256].rearrange("p (h d) -> p h d", h=4)
                    q16 = PB16[:, 0:256].rearrange("p (h d) -> p h d", h=4)
                    if m == 1:
                        head_rms(q4, 4, 64, GQN, 'pbuf')
                        P.op('dve', lambda e: e.tensor_scalar(PBUF[:, 0:256], PBUF[:, 0:256], 0.125, None, ALU.mult),
                             ['pbuf'], ['pbuf'])
                    if lat and m in (1, 2):
                        rope(q16, q4, ROPEH, t, 4, 32, 'pbuf', 'pb16')
                    else:
                        CP('dve', q16, q4, ['pbuf'], ['pb16'])
                    yield
                    transposes_to(lambda b: QTv[0:64, b, :], PB16, 4, 64, 'pb16', qkey, half=True)

            def q_body(t, QTv, qkey, nxt):
                lat = t >= 2
                kts = kv_tiles(m, t, lastl)
                o_i = 4 if (att_i[0] % 2 == 0) else 7
                att_i[0] += 1
                OPS = ps[o_i][:, 0:260].rearrange("p (h d) -> p h d", h=4)
                items = []
                for h in range(4):
                    for g0 in range(0, len(kts), 4):
                        items.append((h, g0, kts[g0:g0 + 4]))

                def emit_qk(ix):
                    h, g0, grp = items[ix]
                    kvh = h if nkv == 4 else h // 2
                    sb_i = 1 + (ix % 3)
                    Sv = ps[sb_i].rearrange("p (g q) -> p g q", g=4)
                    for gi_, (kt, bspec) in enumerate(grp):
                        MM(Sv[:, gi_, :], KT[0:hd, kvh, kt * 128:(kt + 1) * 128], QTv[0:hd, h, :], True, bspec is None,
                           ['kt', qkey], [('ps', sb_i)])
                        if bspec is not None:
                            brhs = BIAS[:, bspec[1] * 4 + h, :] if bspec[0] == 'A' else MASKC[:, bspec[1], :]
                            MM(Sv[:, gi_, :], IDB, brhs, False, True, ['idb', 'bias', 'maskc'], [('ps', sb_i)])

                def emit_exp_pv(ix):
                    h, g0, grp = items[ix]
                    kvh = h if nkv == 4 else h // 2
                    sb_i = 1 + (ix % 3)
                    pt_i = ix % 3
                    Sv = ps[sb_i].rearrange("p (g q) -> p g q", g=4)
                    ng = len(grp)
                    ACT(PT[:, pt_i, 0:ng, :], Sv[:, 0:ng, :], AF.Exp, [('ps', sb_i)], [('pt', pt_i)])
                    for gi_, (kt, bspec) in enumerate(grp):
                        first = (g0 == 0 and gi_ == 0)
                        last = (g0 + gi_ == len(kts) - 1)
                        MM(OPS[:, h, :], PT[:, pt_i, gi_, :], VA[:, kt, kvh, :], first, last,
                           [('pt', pt_i), 'va'], [('ps', o_i)])

                step_pts = set([0, len(items) // 3, (2 * len(items)) // 3])
                emit_qk(0)
                if len(items) > 1:
                    emit_qk(1)
                for ix in range(len(items)):
                    if nxt is not None and ix in step_pts:
                        next(nxt, None)
                    if ix + 2 < len(items):
                        emit_qk(ix + 2)
                    emit_exp_pv(ix)
                if nxt is not None:
                    for _ in nxt:
                        pass
                den = RS[:, 32:36]
                if m == 2:
                    TT('dve', den, OPS[:, :, 64], SKE, ALU.add, [('ps', o_i), 'ske'], ['rs2'])
                else:
                    CP('dve', den, OPS[:, :, 64], [('ps', o_i)], ['rs2'])
                RCP(RS[:, 36:40], den, ['rs2'], ['rs2'])
                TT('dve', OB.rearrange("p (h d) -> p h d", h=4), OPS[:, :, 0:64],
                   RS[:, 36:40].rearrange("p (h o) -> p h o", o=1).to_broadcast([128, 4, 64]), ALU.mult,
                   [('ps', o_i), 'rs2'], ['ob'])
                transposes_to(lambda b: OT[:, b, :], OB, 2, 128, 'ob', 'ot', half=True)
                for hf in range(2):
                    cs = slice(hf * 512, (hf + 1) * 512)
                    for k in range(8):
                        MM(ps[5], HT[:, k, t * 128:(t + 1) * 128], WG[:, k, cs], k == 0, k == 7,
                           [('ht', t), 'wg'], [('ps', 5)])
                    for k in range(2):
                        MM(ps[6], OT[:, k, :], WBR[:, k, cs], k == 0, k == 1, ['ot', 'wbr'], [('ps', 6)])
                    ACT(SIG[:, cs], ps[5], AF.Sigmoid, [('ps', 5)], ['sig'])
                    TT('dve', MTL[:, cs], ps[6], SIG[:, cs], ALU.mult, [('ps', 6), 'sig'], ['mtl'])
                DMA(md[m, t * 128:(t + 1) * 128, :], MTL, ['mtl'], [('md', m, t)])
                if m == 0 and t == 0:
                    ck(5)
                if m == 3 and t == 0:
                    ck(11)
                if m == 3 and t == 2:
                    ck(12)
            g0_ = q_prep(q_tiles[0], QT[:, 0], ('qt', 0))
            for _ in g0_:
                pass
            for idx_, t_ in enumerate(q_tiles):
                nx_ = None
                if idx_ + 1 < len(q_tiles):
                    nx_ = q_prep(q_tiles[idx_ + 1], QT[:, (idx_ + 1) % 2], ('qt', (idx_ + 1) % 2))
                q_body(t_, QT[:, idx_ % 2], ('qt', idx_ % 2), nx_)
            ck(6 + m)
        P.barrier()

        mod_tiles(l, 2 * D, 'gate', 0, which=(0,) if lastl else (0, 1))
        load_w(w_out[l], 8, D, WO, 'wo')
        for t in q_tiles:
            w = 0 if t >= 2 else 1
            DMA(M4, md[:, t * 128:(t + 1) * 128, :].rearrange("m p d -> p m d"),
                [('md', mm_, t) for mm_ in range(4)], ['m4'])
            DMA(XT, x_src(l)[t * 128:(t + 1) * 128, :], [], ['xt'])
            TT('dve', M4[:, 0, :], M4[:, 0, :], M4[:, 1, :], ALU.add, ['m4'], ['m4'])
            TT('pool', M4[:, 2, :], M4[:, 2, :], M4[:, 3, :], ALU.add, ['m4'], ['m4b'])
            TT('dve', M4[:, 0, :], M4[:, 0, :], M4[:, 2, :], ALU.add, ['m4', 'm4b'], ['m4'])
            pb = ps[1].bitcast(BF16)
            for k in range(8):
                TR(pb[:, k * 128:(k + 1) * 128], M4[:, 0, k * 128:(k + 1) * 128], IDB, ['m4', 'idb'], [('ps', 1)])
            CP('act', MT8, pb.rearrange("p (k q) -> p k q", k=8), [('ps', 1)], ['mt8'])
            for hf in range(2):
                cs = slice(hf * 512, (hf + 1) * 512)
                for k in range(8):
                    MM(ps[5 + hf], MT8[:, k, :], WO[:, k, cs], k == 0, k == 7, ['mt8', 'wo'], [('ps', 5 + hf)])
                TT('dve', HB[:, cs], ps[5 + hf], MOD[:, w, cs], ALU.mult, [('ps', 5 + hf), ('mod', w)], ['hb'])
                TT('pool', XS[:, t, cs], HB[:, cs], XT[:, cs], ALU.add, ['hb', 'xt'], [('x', t)])
        P.barrier()
        if dbg == ('xm', l):
            for t in q_tiles:
                DMA(dbg_out[t * 128:(t + 1) * 128, :], XS[:, t, :], [('x', t)], ['dbgo'])
            return True

        moe = (l % 2 == 1)
        mod_tiles(l, 4 * D, 'scale', 0, norm2_g[l:l + 1, :], which=(0,) if lastl else (0, 1))
        mod_tiles(l, 3 * D, 'shift', 2, which=(0,) if lastl else (0, 1))
        if moe:
            DMA(RT, moe_router[l // 2].rearrange("(k p) e -> p k e", p=128), [], ['rt'])
            P.op('dve', lambda e: e.memset(RTB, 0.0), [], ['rtb'])
            CP('dve', RTB[:, :, 0:8], RT, ['rt', 'rtb'], ['rtb'])
        for t in q_tiles:
            norm_tile(t, XS[:, t, :], [('x', t)], 0, 2, router=False)
            if moe:
                lg = ps[7][:, 0:NEXP]
                for k in range(8):
                    MM(ps[7][:, 0:16], HT[:, k, t * 128:(t + 1) * 128], RTB[:, k, :], k == 0, k == 7, [('ht', t), 'rtb'], [('ps', 7)])
                L = RS[:, 0:8]
                CP('dve', L, lg, [('ps', 7)], ['rs'])
                RED(RS[:, 8:9], L, ALU.max, ['rs'], ['rs'])
                TT('dve', RS[:, 16:24], L, RS[:, 8:9].to_broadcast([128, 8]), ALU.is_equal, ['rs'], ['rs'])
                STT(RS[:, 24:32], RS[:, 16:24], -1e30, L, ALU.mult, ALU.add, ['rs'], ['rs'])
                RED(RS[:, 9:10], RS[:, 24:32], ALU.max, ['rs'], ['rs'])
                TT('dve', RS[:, 40:48], RS[:, 24:32], RS[:, 9:10].to_broadcast([128, 8]), ALU.is_equal, ['rs'], ['rs'])
                TT('dve', RS[:, 10:11], RS[:, 9:10], RS[:, 8:9], ALU.subtract, ['rs'], ['rs'])
                ACT(RS[:, 11:12], RS[:, 10:11], AF.Exp, ['rs'], ['rs'])
                TS('dve', RS[:, 12:13], RS[:, 11:12], 1.0, None, ALU.add, None, ['rs'], ['rs'])
                RCP(RS[:, 13:14], RS[:, 12:13], ['rs'], ['rs'])
                TT('dve', RS[:, 14:15], RS[:, 11:12], RS[:, 13:14], ALU.mult, ['rs'], ['rs'])
                TT('dve', RS[:, 48:56], RS[:, 16:24], RS[:, 13:14].to_broadcast([128, 8]), ALU.mult, ['rs'], ['rs'])
                STT(COMB[:, t, :], RS[:, 40:48], RS[:, 14:15], RS[:, 48:56], ALU.mult, ALU.add, ['rs'], ['comb'])
                if t == 0:
                    ck(20)
        mod_tiles(l, 5 * D, 'gate', 0, which=(0,) if lastl else (0, 1))

        chunks = []
        if not lastl:
            chunks.append([0, 1])
        for c in range(4):
            chunks.append([2 + 4 * c + i for i in range(4)])
        nexp = NEXP if moe else 1

        def wset(s_):
            o = s_ * 6144
            return (R4[:, o:o + 2048].rearrange("p (k c) -> p k c", k=8),
                    R4[:, o + 2048:o + 4096].rearrange("p (k c) -> p k c", k=8),
                    R4[:, o + 4096:o + 6144].rearrange("p (k c) -> p k c", k=2),
                    MOD[:, 2 + s_, :].bitcast(BF16).rearrange("p (k c) -> p k c", k=2))

        def g2b(w):
            return MOD[:, w, :].rearrange("p (o d) -> p o d", o=1).to_broadcast([128, 2, D])

        groups = [(e_, g) for e_ in range(nexp) for g in range(DFF // 256)]

        def wsrc(e_):
            if moe:
                return moe_w1[l // 2, e_], moe_w3[l // 2, e_], moe_w2[l // 2, e_]
            return ffn_w1[l // 2], ffn_w3[l // 2], ffn_w2[l // 2]

        def loads(gix):
            e_, g = groups[gix]
            s_ = gix % 2
            w1, w3, w2 = wsrc(e_)
            W1s, W3s, W2s, W2cs = wset(s_)
            load_w(w1[:, g * 256:(g + 1) * 256], 8, 256, None, None, cast=False,
                   use=lambda sv, c0, cw, skey: CP('act', W1s, sv, [skey], [('w1b', s_)]))
            load_w(w3[:, g * 256:(g + 1) * 256], 8, 256, None, None, cast=False,
                   use=lambda sv, c0, cw, skey: CP('act', W3s, sv, [skey], [('w3b', s_)]))

            def use_w2(sv, c0, cw, skey):
                TT('pool', W2s, sv, g2b(0), ALU.mult, [skey, ('mod', 0)], [('w2b', s_)])
                if not lastl:
                    TT('pool', W2cs, sv, g2b(1), ALU.mult, [skey, ('mod', 1)], [('mod', 2 + s_)])
            load_w(w2[g * 256:(g + 1) * 256, :], 2, D, None, None, cast=False, use=use_w2)

        steps = [(gix, cix) for gix in range(len(groups)) for cix in range(len(chunks))]

        def ab(six):
            gix, cix = steps[six]
            s_ = gix % 2
            ch = chunks[cix]
            n = len(ch) * 128
            t0 = ch[0] * 128
            up = (six % 2) * 2
            W1s, W3s, _w2, _w2c = wset(s_)
            for fc in range(2):
                for k in range(8):
                    MM(ps[fc][:, 0:n], W1s[:, k, fc * 128:(fc + 1) * 128], HT[:, k, t0:t0 + n], k == 0, k == 7,
                       [('w1b', s_)] + [('ht', t) for t in ch], [('ps', fc)])
                for k in range(8):
                    MM(ps[2 + fc][:, 0:n], W3s[:, k, fc * 128:(fc + 1) * 128], HT[:, k, t0:t0 + n], k == 0, k == 7,
                       [('w3b', s_)] + [('ht', t) for t in ch], [('ps', 2 + fc)])
                ACT(SA[:, fc, 0:n], ps[fc][:, 0:n], AF.Silu, [('ps', fc)], [('sa', fc)])
                TT('dve', UT[:, up + fc, 0:n], ps[2 + fc][:, 0:n], SA[:, fc, 0:n], ALU.mult,
                   [('ps', 2 + fc), ('sa', fc)], [('ut', up + fc)])

        def w2s(six):
            gix, cix = steps[six]
            e_ = groups[gix][0]
            s_ = gix % 2
            ch = chunks[cix]
            up = (six % 2) * 2
            _w1, _w3, W2s, W2cs = wset(s_)
            for ti, t in enumerate(ch):
                ob_ = 4 + 2 * (ti % 2)
                wsel, wkey = (W2s, ('w2b', s_)) if t >= 2 else (W2cs, ('mod', 2 + s_))
                for hf in range(2):
                    cs = slice(hf * 512, (hf + 1) * 512)
                    for fc in range(2):
                        MM(ps[ob_ + hf], UT[:, up + fc, ti * 128:(ti + 1) * 128], wsel[:, fc, cs], fc == 0, fc == 1,
                           [('ut', up + fc), wkey], [('ps', ob_ + hf)])
                    if moe:
                        STT(XS[:, t, cs], ps[ob_ + hf], COMB[:, t, e_:e_ + 1], XS[:, t, cs], ALU.mult, ALU.add,
                            [('ps', ob_ + hf), 'comb', ('x', t)], [('x', t)])
                    else:
                        TT('dve', XS[:, t, cs], ps[ob_ + hf], XS[:, t, cs], ALU.add,
                           [('ps', ob_ + hf), ('x', t)], [('x', t)])

        loads(0)
        if len(groups) > 1:
            loads(1)
        ab(0)
        for six in range(len(steps)):
            if six + 1 < len(steps):
                ab(six + 1)
            w2s(six)
            gix, cix = steps[six]
            if cix == len(chunks) - 1 and gix + 2 < len(groups):
                loads(gix + 2)
        P.barrier()
        if dbg == ('xf', l):
            for t in q_tiles:
                DMA(dbg_out[t * 128:(t + 1) * 128, :], XS[:, t, :], [('x', t)], ['dbgo'])
            return True
        if not lastl:
            for t in range(NT):
                DMA(xd[t * 128:(t + 1) * 128, :], XS[:, t, :], [('x', t)], [('xd', t)])
            P.barrier()
        return False

    stopped = False
    try:
        for l in range(n_layers):
            stopped = layer(l)
            if stopped:
                break
    except _Stop:
        stopped = True
    if not stopped and n_layers == DEPTH:
        DMA(MOD[:, 0, :], final_g.partition_broadcast(128), [], [('mod', 0)])
        for t in range(2, NT):
            src = XS[:, t, :]
            ACT(HB, src, AF.Square, [('x', t)], ['hb'], accum_out=STAT[:, 0:1])
            ACT(STAT[:, 1:2], STAT[:, 0:1], AF.Sqrt, ['hb'], ['stat'], bias=EPSB, scale=1.0 / D)
            RCP(STAT[:, 2:3], STAT[:, 1:2], ['stat'], ['stat'])
            STT(HB, src, STAT[:, 2:3], MOD[:, 0, :], ALU.mult, ALU.mult, [('x', t), 'stat', ('mod', 0)], ['hb'])
            DMA(out[(t - 2) * 128:(t - 1) * 128, :], HB, ['hb'], ['out'])
    elif not stopped:
        DMA(out[0:128, :], HB, [], ['out'])
    P.barrier()
    P.emit()
    print("instructions:", P.n, {k: len(v) for k, v in P.streams.items()})


_CACHE = {}


def make_in_maps(inp, n_cores=8):
    f = lambda a: np.ascontiguousarray(np.asarray(a), dtype=np.float32)
    cosH, sinH, cosR, sinR = _rope_tables()
    shared = {
        "norm1_g": f(inp["norm1_g"]), "norm2_g": f(inp["norm2_g"]), "w_ada": f(inp["w_ada"]),
        "b_ada": f(inp["b_ada"]), "w_in": f(inp["w_in"]), "biasA": _natten_bias(f(inp["na_rpb"])),
        "smallp": np.ascontiguousarray(np.concatenate(
            [f(inp["gb_qnorm"]), f(inp["gb_knorm"]), f(inp["mla_qnorm"]), f(inp["mla_kvnorm"]), f(inp["wc_sink"])],
            axis=1)),
        "mla_wqb": f(inp["mla_wqb"]), "mla_wkvb": f(inp["mla_wkvb"]), "w_branch": f(inp["w_branch"]),
        "w_out": f(inp["w_out"]), "ffn_w1": f(inp["ffn_w1"]), "ffn_w3": f(inp["ffn_w3"]), "ffn_w2": f(inp["ffn_w2"]),
        "moe_router": f(inp["moe_router"]), "moe_w1": f(inp["moe_w1"]), "moe_w3": f(inp["moe_w3"]),
        "moe_w2": f(inp["moe_w2"]), "final_g": f(inp["final_g"]).reshape(1, D),
        "c_ident": np.eye(128, dtype=np.float32),
        "c_ropeH": np.ascontiguousarray(np.stack([cosH, sinH])),
        "c_ropeR": np.ascontiguousarray(np.stack([cosR, sinR])),
        "c_maskC": _maskC(),
    }
    x = f(inp["x"]); ctx = f(inp["ctx"]); c = f(inp["c"]); cc = f(inp["c_ctx"])
    maps = []
    for b in range(n_cores):
        m = dict(shared)
        m["xin"] = np.ascontiguousarray(np.concatenate([ctx[b], x[b]], axis=0))
        m["cvec"] = np.ascontiguousarray(np.stack([c[b], cc]))
        maps.append(m)
    return maps


def kernel(**inputs):
    if "nc" not in _CACHE:
        _CACHE["nc"] = build(DEPTH)
    nc = _CACHE["nc"]
    maps = make_in_maps(inputs, 8)
    res = run_bass_kernel_spmd(nc, maps, core_ids=list(range(8)))
    return np.stack([np.asarray(r["out"], dtype=np.float32) for r in res.results], axis=0)
```

```python
import numpy as np
from contextlib import ExitStack
import concourse.bass as bass
import concourse.mybir as mybir
from concourse.bass_utils import run_bass_kernel_spmd

F32 = mybir.dt.float32
BF16 = mybir.dt.bfloat16
AF = mybir.ActivationFunctionType
ALU = mybir.AluOpType
AX = mybir.AxisListType

D = 1024
S = 2048
C = 256
T = S + C
NT = T // 128
DEPTH = 4
IN_W = 6304
DFF = 3584
NEXP = 8
EPS = 1e-6
NEG = -30000.0
NDMA = 12
NSTG = 3

OFF = dict(aq=0, ak=256, av=512, bq=768, bk=1024, bv=1152, cq=1280, ck=1536, cv=1664,
           dqa=1792, dkva=2048, gate=2208)


class Prog:
    CE = ['pe', 'act', 'dve', 'pool']

    def __init__(self, nc, es):
        self.nc = nc
        self.streams = {e: [] for e in self.CE + ['sp']}
        self.sems = {}
        for e in self.CE:
            self.sems[e] = es.enter_context(nc.semaphore("s_" + e))
        for i in range(NDMA):
            self.sems['d%d' % i] = es.enter_context(nc.semaphore("s_d%d" % i))
        self.cnt = {k: 0 for k in self.sems}
        self.known = {e: {k: 0 for k in self.sems} for e in self.streams}
        self.lastw = {}
        self.readers = {}
        self.dma_rr = 0
        self.n = 0
        self.last_real = {e: None for e in self.CE}
        self.pend = {e: False for e in self.CE}

    def _materialize(self, s):
        if s in self.pend and self.pend[s]:
            self.streams[s][self.last_real[s]][2] = s
            self.cnt[s] += 1
            self.pend[s] = False

    def _deps(self, eng, reads, writes):
        deps = {}

        def add(d):
            if d is not None:
                if d[0] == 'pe' and eng == 'pe':
                    return
                if deps.get(d[0], 0) < d[1]:
                    deps[d[0]] = d[1]
        for k in reads:
            add(self.lastw.get(k))
        for k in writes:
            add(self.lastw.get(k))
            for r in self.readers.get(k, ()):
                add(r)
        waits = []
        kn = self.known[eng]
        for s, i in deps.items():
            if kn[s] < i:
                if i > self.cnt[s]:
                    self._materialize(s)
                assert i <= self.cnt[s], (s, i, self.cnt[s])
                kn[s] = i
                waits.append((s, i))
        return waits

    def _commit(self, tok, reads, writes):
        for k in writes:
            self.lastw[k] = tok
            self.readers[k] = []
        for k in reads:
            self.readers.setdefault(k, []).append(tok)

    def op(self, eng, fn, reads=(), writes=()):
        waits = self._deps(eng, reads, writes)
        tok = (eng, self.cnt[eng] + 1)
        self.streams[eng].append([waits, fn, None])
        self.last_real[eng] = len(self.streams[eng]) - 1
        self.pend[eng] = True
        self._commit(tok, reads, writes)
        self.n += 1

    def dma(self, fn, reads=(), writes=()):
        s = 'd%d' % self.dma_rr
        self.dma_rr = (self.dma_rr + 1) % NDMA
        waits = self._deps('sp', reads, writes)
        if self.known['sp'][s] < self.cnt[s]:
            self.known['sp'][s] = self.cnt[s]
            waits.append((s, self.cnt[s]))
        self.cnt[s] += 1
        tok = (s, self.cnt[s])
        self.streams['sp'].append([waits, fn, s])
        self._commit(tok, reads, writes)
        self.n += 1

    def barrier(self):
        for e in self.CE:
            self._materialize(e)
        snap = dict(self.cnt)
        for e in self.streams:
            waits = []
            for s, i in snap.items():
                if self.known[e][s] < i:
                    self.known[e][s] = i
                    waits.append((s, i))
            if waits:
                self.streams[e].append([waits, None, None])

    def emit(self):
        nc = self.nc
        sems = self.sems

        def mult(s):
            return 16 if s[0] == 'd' else 1

        def run(engine, stream):
            for waits, fn, inc in stream:
                for s, i in waits:
                    engine.wait_ge(sems[s], i * mult(s))
                if fn is None:
                    continue
                ins = fn(engine)
                if inc is not None:
                    ins.then_inc(sems[inc], mult(inc))

        with nc.Block() as block:
            @block.tensor
            def _(e):
                run(e, self.streams['pe'])

            @block.scalar
            def _(e):
                run(e, self.streams['act'])

            @block.vector
            def _(e):
                run(e, self.streams['dve'])

            @block.gpsimd
            def _(e):
                run(e, self.streams['pool'])

            @block.sync
            def _(e):
                run(e, self.streams['sp'])


def _rope_tables():
    def ang(pos, dim):
        inv = (10000.0 ** (-np.arange(0, dim, 2, dtype=np.float32) / dim)).astype(np.float32)
        return pos.astype(np.float32)[:, None] * inv[None, :]
    t = np.arange(S)
    out = []
    for rot in (64, 32):
        half = rot // 2
        a = np.concatenate([ang(t // 64, half), ang(t % 64, half)], axis=-1).astype(np.float32)
        out += [np.cos(a).astype(np.float32), np.sin(a).astype(np.float32)]
    return out


def _natten_plan():
    rows = S // 64
    kh = 8
    pats = {}
    plan = []
    for i in range(16):
        lst = []
        for j in range(16):
            sig = []
            anyv = False
            for a in range(2):
                for b in range(2):
                    r = 2 * i + b
                    rk = 2 * j + a
                    st = min(max(r - kh // 2, 0), rows - kh)
                    if st <= rk < st + kh:
                        sig.append(rk - r + 7)
                        anyv = True
                    else:
                        sig.append(-1)
            if anyv:
                sig = tuple(sig)
                if sig not in pats:
                    pats[sig] = len(pats)
                lst.append((j, pats[sig]))
        plan.append(lst)
    return plan, pats


NAT_PLAN, NAT_PATS = _natten_plan()
NPAT = len(NAT_PATS)


def _natten_bias(rpb):
    col = np.arange(64)
    cstart = np.clip(col - 8, 0, 48)
    col_ok = (col[None, :] >= cstart[:, None]) & (col[None, :] < cstart[:, None] + 16)
    dc = np.clip(col[None, :] - col[:, None] + 15, 0, 30)
    out = np.full((DEPTH, 128, NPAT * 4, 128), NEG, dtype=np.float32)
    for sig, pid in NAT_PATS.items():
        for a in range(2):
            for b in range(2):
                dr = sig[a * 2 + b]
                if dr < 0:
                    continue
                g = rpb[:, :, dr, :][:, :, dc]
                g = np.where(col_ok[None, None], g, np.float32(NEG))
                g = np.transpose(g, (0, 3, 1, 2))
                out[:, a * 64:(a + 1) * 64, pid * 4:(pid + 1) * 4, b * 64:(b + 1) * 64] = g
    return out


def _maskC():
    kk = np.arange(128)[:, None]
    qq = np.arange(128)[None, :]
    m0 = np.where(qq <= kk, 0.0, NEG).astype(np.float32)
    m1 = np.where(kk <= qq, 0.0, NEG).astype(np.float32)
    return np.stack([m0, m1], axis=1)


def build(n_layers=DEPTH, dbg=None):
    nc = bass.Bass("TRN2", target_bir_lowering=False, dynamic_dma_scratch_size=256)
    es = ExitStack()
    with es:
        _build_body(nc, es, n_layers, dbg)
    return nc


def _build_body(nc, es, n_layers, dbg):
    def din(name, shape):
        return nc.dram_tensor(name, list(shape), F32, kind="ExternalInput").ap()

    xin = din("xin", [T, D])
    cvec = din("cvec", [2, D])
    norm1_g = din("norm1_g", [DEPTH, D])
    norm2_g = din("norm2_g", [DEPTH, D])
    w_ada = din("w_ada", [DEPTH, D, 6 * D])
    b_ada = din("b_ada", [DEPTH, 6 * D])
    w_in = din("w_in", [DEPTH, D, IN_W])
    biasA = din("biasA", [DEPTH, 128, NPAT * 4, 128])
    smallp = din("smallp", [DEPTH, 516])
    mla_wqb = din("mla_wqb", [DEPTH, 256, 384])
    mla_wkvb = din("mla_wkvb", [DEPTH, 128, 512])
    w_branch = din("w_branch", [DEPTH, 4, 256, D])
    w_out = din("w_out", [DEPTH, D, D])
    ffn_w1 = din("ffn_w1", [2, D, DFF])
    ffn_w3 = din("ffn_w3", [2, D, DFF])
    ffn_w2 = din("ffn_w2", [2, DFF, D])
    moe_router = din("moe_router", [2, D, NEXP])
    moe_w1 = din("moe_w1", [2, NEXP, D, DFF])
    moe_w3 = din("moe_w3", [2, NEXP, D, DFF])
    moe_w2 = din("moe_w2", [2, NEXP, DFF, D])
    final_g = din("final_g", [1, D])
    c_ident = din("c_ident", [128, 128])
    c_ropeH = din("c_ropeH", [2, S, 32])
    c_ropeR = din("c_ropeR", [2, S, 16])
    c_maskC = din("c_maskC", [128, 2, 128])
    out = nc.dram_tensor("out", [S, D], F32, kind="ExternalOutput").ap()
    xd = nc.dram_tensor("xd", [T, D], F32, kind="Internal").ap()
    md = nc.dram_tensor("md", [4, T, D], BF16, kind="Internal").ap()
    dbg_out = None
    if dbg is not None:
        dbg_out = nc.dram_tensor("dbg", [T, D], F32, kind="ExternalOutput").ap()

    P = Prog(nc, es)

    def sb(name, shape, dt):
        return es.enter_context(nc.sbuf_tensor(name, list(shape), dt))[:]

    ps = [es.enter_context(nc.psum_tensor("ps%d" % i, [128, 512], F32))[:] for i in range(8)]

    IDF = sb("idf", [128, 128], F32)
    IDB = sb("idb", [128, 128], BF16)
    ROPEH = sb("ropeh", [128, 2, 16, 32], F32)
    ROPER = sb("roper", [128, 2, 16, 16], F32)
    MASKC = sb("maskc", [128, 2, 128], BF16)
    CV = sb("cv", [128, 2, 8], F32)
    SL = sb("sl", [128, 2, 8, 128], F32)
    MOD = sb("mod", [128, 4, D], F32)
    SMP = sb("smp", [128, 516], F32)
    SKE = sb("ske", [128, 4], F32)
    HT = sb("ht", [128, 8, T], BF16)
    STG = sb("stg", [128, NSTG, 2048], F32)
    XT = sb("xt", [128, D], F32)
    HB = sb("hb", [128, D], F32)
    PB = HB
    STAT = sb("stat", [128, 32], F32)
    PBUF = sb("pbuf", [128, D], F32)
    TMPF = sb("tmpf", [128, 4, 256], F32)
    PB16 = sb("pb16", [128, 512], BF16)
    QT = sb("qt", [128, 2, 4, 128], BF16)
    OB = sb("ob", [128, 256], BF16)
    OT = sb("ot", [128, 2, 128], BF16)
    DTT = sb("dtt", [128, 2, 128], BF16)
    MT8 = sb("mt8", [128, 8, 128], BF16)
    RT = sb("rt", [128, 8, NEXP], F32)
    RTB = sb("rtb", [128, 8, 16], BF16)
    COMB = sb("comb", [128, NT, NEXP], F32)
    RS = sb("rs", [128, 64], F32)
    UT = sb("ut", [128, 4, 512], BF16)
    SA = sb("sa", [128, 2, 512], BF16)
    R4 = sb("r4", [128, 12288], BF16)
    R2 = sb("r2", [128, 36864], BF16)

    KT = R2[:, 0:9216].rearrange("p (h t) -> p h t", h=4)
    VA = R2[:, 9216:13896].rearrange("p (t h d) -> p t h d", t=NT, h=4)
    BIAS = R2[:, 13896:13896 + NPAT * 4 * 128].rearrange("p (n q) -> p n q", q=128)
    XS = R2.bitcast(F32).rearrange("p (t d) -> p t d", t=NT)
    b0 = 13896 + NPAT * 4 * 128
    WG = R2[:, b0:b0 + 8192].rearrange("p (k c) -> p k c", k=8)
    WKV = WG
    WQ = R2[:, b0 + 8192:b0 + 10240].rearrange("p (k c) -> p k c", k=8)
    WBR = R2[:, b0 + 10240:b0 + 12288].rearrange("p (k c) -> p k c", k=2)
    WQB = R2[:, b0 + 12288:b0 + 13056].rearrange("p (k c) -> p k c", k=2)
    WKVB = R2[:, b0 + 13056:b0 + 13568]
    SIG = R2[:, b0 + 13568:b0 + 14592]
    MTL = R2[:, b0 + 14592:b0 + 15616]
    PT = R2[:, b0 + 15616:b0 + 17152].rearrange("p (a g q) -> p a g q", a=3, g=4)
    assert b0 + 17152 <= 36864
    WO = R4[:, 0:8192].rearrange("p (k c) -> p k c", k=8)
    M4 = MOD[:, 2:4, :].rearrange("p a d -> p (a d)").bitcast(BF16).rearrange("p (a d) -> p a d", a=4)
    H2F = PBUF.rearrange("p (k q) -> p k q", k=8)
    W1B = R4[:, 0:4096].rearrange("p (k c) -> p k c", k=8)
    W3B = R4[:, 4096:8192].rearrange("p (k c) -> p k c", k=8)
    W2B = R4[:, 8192:12288].rearrange("p (k c) -> p k c", k=4)

    def MM(o, lhsT, rhs, start, stop, r, w):
        P.op('pe', lambda e: e.matmul(o, lhsT, rhs, start=start, stop=stop), r, w)

    def TR(o, i, ident, r, w):
        P.op('pe', lambda e: e.transpose(o, i, ident), r, w)

    def ACT(o, i, func, r, w, **kw):
        P.op('act', lambda e: e.activation(o, i, func, **kw), r, w)

    def TT(eng, o, a, b, op, r, w):
        P.op(eng, lambda e: e.tensor_tensor(o, a, b, op), r, w)

    def TS(eng, o, a, s1, s2, op0, op1, r, w):
        if op1 is None:
            P.op(eng, lambda e: e.tensor_scalar(o, a, s1, None, op0), r, w)
        else:
            P.op(eng, lambda e: e.tensor_scalar(o, a, s1, s2, op0, op1), r, w)

    def STT(o, a, s, b, op0, op1, r, w):
        P.op('dve', lambda e: e.scalar_tensor_tensor(o, a, s, b, op0, op1), r, w)

    def CP(eng, o, i, r, w):
        if eng == 'act':
            P.op('act', lambda e: e.activation(o, i, AF.Copy), r, w)
        else:
            P.op(eng, lambda e: e.tensor_copy(o, i), r, w)

    def RED(o, i, op, r, w):
        P.op('dve', lambda e: e.tensor_reduce(o, i, AX.X, op), r, w)

    def RCP(o, i, r, w):
        P.op('dve', lambda e: e.reciprocal(o, i), r, w)

    def DMA(o, i, r, w):
        P.dma(lambda e: e.dma_start(out=o, in_=i), r, w)

    stg_rr = [0]

    def load_w(src, kc, ncols, dst, dkey, cast=True, use=None):
        pcw = 2048 // kc
        c0 = 0
        while c0 < ncols:
            cw = min(pcw, ncols - c0)
            s = stg_rr[0]
            stg_rr[0] = (s + 1) % NSTG
            sv = STG[:, s, 0:kc * cw].rearrange("p (k c) -> p k c", k=kc)
            DMA(sv, src[:, c0:c0 + cw].rearrange("(k p) c -> p k c", p=128), [], [('stg', s)])
            if cast:
                CP('pool', dst[:, :, c0:c0 + cw], sv, [('stg', s)], [dkey])
            else:
                use(sv, c0, cw, ('stg', s))
            c0 += cw

    import os
    KS = int(os.environ.get("KSETUP", "99"))
    if KS > 0:
        DMA(IDF, c_ident, [], ['idf'])
        CP('dve', IDB, IDF, ['idf'], ['idb'])
    if KS > 1:
        DMA(ROPEH[:, 0], c_ropeH[0].rearrange("(t p) d -> p t d", p=128), [], ['rope'])
        DMA(ROPEH[:, 1], c_ropeH[1].rearrange("(t p) d -> p t d", p=128), [], ['rope'])
        DMA(ROPER[:, 0], c_ropeR[0].rearrange("(t p) d -> p t d", p=128), [], ['rope'])
        DMA(ROPER[:, 1], c_ropeR[1].rearrange("(t p) d -> p t d", p=128), [], ['rope'])
    if KS > 2:
        DMA(PBUF[:, 0:256].rearrange("p (a q) -> p a q", a=2), c_maskC, [], ['pbuf'])
        CP('dve', MASKC, PBUF[:, 0:256].rearrange("p (a q) -> p a q", a=2), ['pbuf'], ['maskc'])
    if KS > 3:
        P.dma(lambda e: e.dma_start(out=CV, in_=cvec.rearrange("w (k p) -> p w k", p=128),
                                    allow_slow_non_contiguous=True), [], ['cv'])
    if KS > 4:
        ACT(CV, CV, AF.Silu, ['cv'], ['cv'])
    if KS > 5:
        for wch in range(2):
            CP('dve', SL[:, wch], CV[:, wch, :].rearrange("p (k o) -> p k o", o=1).to_broadcast([128, 8, 128]),
               ['cv'], ['sl'])

    def rope_view(tab, which, t, nh, half):
        return tab[:, which, t - 2, :].rearrange("p (o d) -> p o d", o=1).to_broadcast([128, nh, half])

    def rope(dst, src, tab, t, nh, half, skey, dkey):
        c = rope_view(tab, 0, t, nh, half)
        s = rope_view(tab, 1, t, nh, half)
        x1 = src[:, :, 0:half]
        x2 = src[:, :, half:2 * half]
        t1 = TMPF[:, 0, 0:nh * half].rearrange("p (h d) -> p h d", h=nh)
        t2 = TMPF[:, 1, 0:nh * half].rearrange("p (h d) -> p h d", h=nh)
        t3 = TMPF[:, 2, 0:nh * half].rearrange("p (h d) -> p h d", h=nh)
        t4 = TMPF[:, 3, 0:nh * half].rearrange("p (h d) -> p h d", h=nh)
        TT('dve', t1, x1, c, ALU.mult, [skey, 'rope'], ['tf0'])
        TT('pool', t2, x2, s, ALU.mult, [skey, 'rope'], ['tf1'])
        TT('dve', t3, x1, s, ALU.mult, [skey, 'rope'], ['tf2'])
        TT('pool', t4, x2, c, ALU.mult, [skey, 'rope'], ['tf3'])
        TT('dve', dst[:, :, 0:half], t1, t2, ALU.subtract, ['tf0', 'tf1'], [dkey])
        TT('dve', dst[:, :, half:2 * half], t3, t4, ALU.add, ['tf2', 'tf3'], [dkey])

    def head_rms(src, nh, hd, gain, skey):
        sq = TMPF[:, 0:1, :].rearrange("p a c -> p (a c)")[:, 0:nh * hd].rearrange("p (h d) -> p h d", h=nh)
        TT('dve', sq, src, src, ALU.mult, [skey], ['tf0'])
        RED(RS[:, 0:nh], sq, ALU.add, ['tf0'], ['rs'])
        TS('dve', RS[:, 8:8 + nh], RS[:, 0:nh], 1.0 / hd, EPS, ALU.mult, ALU.add, ['rs'], ['rs'])
        TT('pool', RS[:, 16:16 + nh], RS[:, 8:8 + nh], NHALF[:, 0:nh], ALU.pow, ['rs', 'nhalf'], ['rs'])
        TT('dve', src, src, RS[:, 16:16 + nh].rearrange("p (h o) -> p h o", o=1).to_broadcast([128, nh, hd]),
           ALU.mult, [skey, 'rs'], [skey])
        TT('dve', src, src, gain.rearrange("p (o d) -> p o d", o=1).to_broadcast([128, nh, hd]),
           ALU.mult, [skey, 'smp'], [skey])

    EPSB = sb("epsb", [128, 1], F32)
    P.op('dve', lambda e: e.memset(EPSB, EPS), [], ['epsb'])
    NHALF = sb("nhalf", [128, 8], F32)
    P.op('dve', lambda e: e.memset(NHALF, -0.5), [], ['nhalf'])

    def mod_tiles(l, col0, kind, slot, gain_src=None, which=(0, 1)):
        DMA(PB, b_ada[l:l + 1, col0:col0 + D].partition_broadcast(128), [], ['hb'])

        def use(sv, c0, cw, skey):
            for w in which:
                bank = ps[6 + w]
                for k in range(8):
                    MM(bank[:, 0:cw], SL[:, w, k, :], sv[:, k, :], k == 0, k == 7,
                       ['sl', skey], [('ps', 6 + w)])
                TT('dve', MOD[:, slot + w, c0:c0 + cw], bank[:, 0:cw], PB[:, c0:c0 + cw], ALU.add,
                   [('ps', 6 + w), 'hb'], [('mod', slot + w)])
        load_w(w_ada[l][:, col0:col0 + D], 8, D, None, None, cast=False, use=use)
        if kind == 'scale':
            DMA(PB, gain_src.partition_broadcast(128), [], ['hb'])
            for w in which:
                STT(MOD[:, slot + w], MOD[:, slot + w], 1.0, PB, ALU.add, ALU.mult,
                    [('mod', slot + w), 'hb'], [('mod', slot + w)])

    def norm_tile(t, src_ap, src_keys, gslot, sslot, router=False, xkey=None):
        w = 0 if t >= 2 else 1
        ACT(HB, src_ap, AF.Square, src_keys, ['hb'], accum_out=STAT[:, 0:1])
        ACT(STAT[:, 1:2], STAT[:, 0:1], AF.Sqrt, ['hb'], ['stat'], bias=EPSB, scale=1.0 / D)
        RCP(STAT[:, 2:3], STAT[:, 1:2], ['stat'], ['stat'])
        STT(HB, src_ap, STAT[:, 2:3], MOD[:, gslot + w], ALU.mult, ALU.mult,
            src_keys + ['stat', ('mod', gslot + w)], ['hb'])
        TT('dve', HB, HB, MOD[:, sslot + w], ALU.add, ['hb', ('mod', sslot + w)], ['hb'])
        for half in range(2):
            bank = ps[half]
            for kk in range(4):
                k = half * 4 + kk
                TR(bank[:, kk * 128:(kk + 1) * 128], HB[:, k * 128:(k + 1) * 128], IDF,
                   ['hb', 'idf'], [('ps', half)])
            pv = bank.rearrange("p (k q) -> p k q", k=4)
            CP('act' if half == 0 else 'dve', HT[:, half * 4:half * 4 + 4, t * 128:(t + 1) * 128], pv,
               [('ps', half)], [('ht', t)])
            if router:
                CP('dve' if half == 0 else 'act', H2F[:, half * 4:half * 4 + 4, :], pv,
                   [('ps', half)], ['h2f'])


    class _Stop(Exception):
        pass
    KSTOP = int(os.environ.get('KSTOP', '0'))

    def ck(n):
        if KSTOP == n:
            raise _Stop()

    def x_src(l):
        return xin if l == 0 else xd

    def kv_tiles(m, t, lastl):
        if t < 2:
            return [(0, None), (1, None)]
        i = t - 2
        if m == 0:
            return [(j + 2, ('A', pid)) for (j, pid) in NAT_PLAN[i]] + [(0, None), (1, None)]
        if m == 2:
            lst = []
            if i - 1 >= 0:
                lst.append((t - 1, ('C', 0)))
            lst.append((t, None))
            if i + 1 < 16:
                lst.append((t + 1, ('C', 1)))
            return lst + [(0, None), (1, None)]
        return [(j, None) for j in range(NT)]

    GQN = SMP[:, 0:64]
    GKN = SMP[:, 64:128]
    MQN = SMP[:, 128:384]
    MKVN = SMP[:, 384:512]

    def evac_scaled(dst, src_ps, pkey, dkey, scale):
        P.op('dve', lambda e: e.tensor_scalar(dst, src_ps, scale, None, ALU.mult), [pkey], [dkey])

    def transposes_to(dst_fn, src16, nblk, width, skey, dkey, bank_i=1, half=False):
        pb = ps[bank_i].bitcast(BF16)
        pkey = ('ps', bank_i)
        if half:
            pb = ps[0].bitcast(BF16)[:, 512:1024]
            pkey = ('ps', '0b')
        for b in range(nblk):
            TR(pb[0:width, b * 128:(b + 1) * 128], src16[:, b * width:(b + 1) * width], IDB,
               [skey, 'idb'], [pkey])
        for b in range(nblk):
            CP('dve' if (half or b % 2 == 1) else 'act', dst_fn(b), pb[0:width, b * 128:(b + 1) * 128],
               [pkey], [dkey])

    def proj(bank_i, t, wview, ncols, wkey):
        for k in range(8):
            MM(ps[bank_i][:, 0:ncols], HT[:, k, t * 128:(t + 1) * 128], wview[:, k, 0:ncols], k == 0, k == 7,
               [('ht', t), wkey], [('ps', bank_i)])

    att_i = [0]

    def layer(l):
        lastl = (l == DEPTH - 1)
        q_tiles = list(range(2, NT)) if lastl else list(range(NT))
        DMA(SMP, smallp[l:l + 1, :].partition_broadcast(128), [], ['smp'])
        ACT(SKE, SMP[:, 512:516], AF.Exp, ['smp'], ['ske'])
        mod_tiles(l, 1 * D, 'scale', 0, norm1_g[l:l + 1, :])
        mod_tiles(l, 0 * D, 'shift', 2)
        ck(1)
        for t in range(NT):
            DMA(XT, x_src(l)[t * 128:(t + 1) * 128, :], [], ['xt'])
            norm_tile(t, XT, ['xt'], 0, 2)
        P.barrier()
        ck(2)

        for m in [int(c_) for c_ in os.environ.get('KMIX', '0123')]:
            nh = 4
            nkv = 4 if m in (0, 3) else 2
            hd = 128 if m == 3 else 64
            if m == 0:
                load_w(w_in[l][:, OFF['ak']:OFF['ak'] + 512], 8, 512, WKV, 'wg')
                DMA_bias = True
                for c0 in range(0, NPAT * 4, 16):
                    c1 = min(c0 + 16, NPAT * 4)
                    s_ = stg_rr[0]
                    stg_rr[0] = (s_ + 1) % NSTG
                    sv = STG[:, s_, 0:(c1 - c0) * 128].rearrange("p (n q) -> p n q", q=128)
                    DMA(sv, biasA[l][:, c0:c1, :], [], [('stg', s_)])
                    CP('pool', BIAS[:, c0:c1, :], sv, [('stg', s_)], ['bias'])
                kvw = 512
            elif m == 1:
                load_w(w_in[l][:, OFF['bk']:OFF['bk'] + 256], 8, 256, WKV, 'wg')
                kvw = 256
            elif m == 2:
                load_w(w_in[l][:, OFF['ck']:OFF['ck'] + 256], 8, 256, WKV, 'wg')
                kvw = 256
            else:
                load_w(w_in[l][:, OFF['dkva']:OFF['dkva'] + 160], 8, 160, WKV, 'wg')
                load_w(mla_wkvb[l], 1, 512, WKVB.rearrange("p (k c) -> p k c", k=1), 'wkvb')
                kvw = 160
            if m == 0:
                ck(3)
            P.op('dve', lambda e: e.memset(VA[:, :, :, 64:65], 1.0), [], ['va'])
            for t in range(NT):
                lat = t >= 2
                proj(0, t, WKV, kvw, 'wg')
                CP('act', PBUF[:, 0:kvw], ps[0][:, 0:kvw], [('ps', 0)], ['pbuf'])
                if m == 0:
                    CP('dve', PB16[:, 0:256], PBUF[:, 0:256], ['pbuf'], ['pb16'])
                    CP('pool', VA[:, t, :, 0:64], PBUF[:, 256:512].rearrange("p (h d) -> p h d", h=4), ['pbuf'], ['va'])
                    transposes_to(lambda b: KT[0:64, b, t * 128:(t + 1) * 128], PB16, 4, 64, 'pb16', 'kt')
                elif m in (1, 2):
                    kview = PBUF[:, 0:128].rearrange("p (h d) -> p h d", h=2)
                    if m == 1:
                        head_rms(kview, 2, 64, GKN, 'pbuf')
                    k16 = PB16[:, 0:128].rearrange("p (h d) -> p h d", h=2)
                    if lat:
                        rope(k16, kview, ROPEH, t, 2, 32, 'pbuf', 'pb16')
                    else:
                        CP('dve', k16, kview, ['pbuf'], ['pb16'])
                    CP('pool', VA[:, t, 0:2, 0:64], PBUF[:, 128:256].rearrange("p (h d) -> p h d", h=2), ['pbuf'], ['va'])
                    transposes_to(lambda b: KT[0:64, b, t * 128:(t + 1) * 128], PB16, 2, 64, 'pb16', 'kt')
                else:
                    cview = PBUF[:, 0:128].rearrange("p (h d) -> p h d", h=1)
                    head_rms(cview, 1, 128, MKVN, 'pbuf')
                    CP('dve', PB16[:, 0:128], PBUF[:, 0:128], ['pbuf'], ['pb16'])
                    transposes_to(lambda b: DTT[:, 0, :], PB16, 1, 128, 'pb16', 'dtt')
                    ck(14)
                    MM(ps[2][:, 0:512], DTT[:, 0, :], WKVB, True, True, ['dtt', 'wkvb'], [('ps', 2)])
                    ck(15)
                    kf = PB16[:, 0:512].rearrange("p (h d) -> p h d", h=4)
                    P.op('dve', lambda e: e.memset(PB16[:, 0:512], 0.0), [], ['pb16'])
                    CP('act', PBUF[:, 512:1024], ps[2], [('ps', 2)], ['pbuf2'])
                    dkv = PBUF[:, 512:1024].rearrange("p (h d) -> p h d", h=4)
                    CP('pool', kf[:, :, 0:64], dkv[:, :, 0:64], ['pbuf2'], ['pb16'])
                    CP('pool', VA[:, t, :, 0:64], dkv[:, :, 64:128], ['pbuf2'], ['va'])
                    ck(16)
                    pe_src = PBUF[:, 128:160].rearrange("p (h d) -> p h d", h=1)
                    pe_dst = PBUF[:, 160:192].rearrange("p (h d) -> p h d", h=1)
                    if lat:
                        rope(pe_dst, pe_src, ROPER, t, 1, 16, 'pbuf', 'pbuf')
                    else:
                        CP('dve', pe_dst, pe_src, ['pbuf'], ['pbuf'])
                    ck(17)
                    CP('dve', kf[:, :, 64:96], pe_dst.to_broadcast([128, 4, 32]), ['pbuf'], ['pb16'])
                    ck(18)
                    transposes_to(lambda b: KT[:, b, t * 128:(t + 1) * 128], PB16, 4, 128, 'pb16', 'kt')
            if m == 0:
                ck(4)
            if m == 3:
                ck(10)
            qoff = [OFF['aq'], OFF['bq'], OFF['cq'], OFF['dqa']][m]
            load_w(w_in[l][:, qoff:qoff + 256], 8, 256, WQ, 'wq')
            if m == 3:
                load_w(mla_wqb[l], 2, 384, WQB, 'wqb')
            load_w(w_in[l][:, OFF['gate'] + m * D:OFF['gate'] + (m + 1) * D], 8, D, WG, 'wg')
            load_w(w_branch[l, m], 2, D, None, None, cast=False,
                   use=lambda sv, c0, cw, skey: TS('dve', WBR[:, :, c0:c0 + cw], sv, 0.5, None, ALU.mult, None, [skey], ['wbr']))
            def q_prep(t, QTv, qkey):
                lat = t >= 2
                proj(0, t, WQ, 256, 'wq')
                qscale = 0.125 if m != 3 else float(96 ** -0.5)
                if m == 3:
                    evac_scaled(PBUF[:, 0:256], ps[0][:, 0:256], ('ps', 0), 'pbuf', 1.0)
                    qv = PBUF[:, 0:256].rearrange("p (h d) -> p h d", h=1)
                    head_rms(qv, 1, 256, MQN, 'pbuf')
                    CP('dve', PB16[:, 0:256], PBUF[:, 0:256], ['pbuf'], ['pb16'])
                    yield
                    transposes_to(lambda b: DTT[:, b, :], PB16, 2, 128, 'pb16', 'dtt', half=True)
                    for k in range(2):
                        MM(ps[5][:, 0:384], DTT[:, k, :], WQB[:, k, :], k == 0, k == 1, ['dtt', 'wqb'], [('ps', 5)])
                    evac_scaled(PBUF[:, 0:384], ps[5][:, 0:384], ('ps', 5), 'pbuf', qscale)
                    q4 = PBUF[:, 0:384].rearrange("p (h d) -> p h d", h=4)
                    q16 = PB16[:, 0:512].rearrange("p (h d) -> p h d", h=4)
                    P.op('dve', lambda e: e.memset(PB16[:, 0:512], 0.0), [], ['pb16'])
                    CP('pool', q16[:, :, 0:64], q4[:, :, 0:64], ['pbuf'], ['pb16'])
                    if lat:
                        rope(q16[:, :, 64:96], q4[:, :, 64:96], ROPER, t, 4, 16, 'pbuf', 'pb16')
                    else:
                        CP('dve', q16[:, :, 64:96], q4[:, :, 64:96], ['pbuf'], ['pb16'])
                    yield
                    transposes_to(lambda b: QTv[:, b, :], PB16, 4, 128, 'pb16', qkey, half=True)
                else:
                    evac_scaled(PBUF[:, 0:256], ps[0][:, 0:256], ('ps', 0), 'pbuf', qscale)
                    q4 = PBUF[:, 0:256].rearrange("p (h d) -> p h d", h=4)
                    q16 = PB16[:, 0:256].rearrange("p (h d) -> p h d", h=4)
                    if m == 1:
                        head_rms(q4, 4, 64, GQN, 'pbuf')
                        P.op('dve', lambda e: e.tensor_scalar(PBUF[:, 0:256], PBUF[:, 0:256], 0.125, None, ALU.mult),
                             ['pbuf'], ['pbuf'])
                    if lat and m in (1, 2):
                        rope(q16, q4, ROPEH, t, 4, 32, 'pbuf', 'pb16')
                    else:
                        CP('dve', q16, q4, ['pbuf'], ['pb16'])
                    yield
                    transposes_to(lambda b: QTv[0:64, b, :], PB16, 4, 64, 'pb16', qkey, half=True)

            def q_body(t, QTv, qkey, nxt):
                lat = t >= 2
                kts = kv_tiles(m, t, lastl)
                o_i = 4 if (att_i[0] % 2 == 0) else 7
                att_i[0] += 1
                OPS = ps[o_i][:, 0:260].rearrange("p (h d) -> p h d", h=4)
                items = []
                for h in range(4):
                    for g0 in range(0, len(kts), 4):
                        items.append((h, g0, kts[g0:g0 + 4]))

                def emit_qk(ix):
                    h, g0, grp = items[ix]
                    kvh = h if nkv == 4 else h // 2
                    sb_i = 1 + (ix % 3)
                    Sv = ps[sb_i].rearrange("p (g q) -> p g q", g=4)
                    for gi_, (kt, bspec) in enumerate(grp):
                        MM(Sv[:, gi_, :], KT[0:hd, kvh, kt * 128:(kt + 1) * 128], QTv[0:hd, h, :], True, bspec is None,
                           ['kt', qkey], [('ps', sb_i)])
                        if bspec is not None:
                            brhs = BIAS[:, bspec[1] * 4 + h, :] if bspec[0] == 'A' else MASKC[:, bspec[1], :]
                            MM(Sv[:, gi_, :], IDB, brhs, False, True, ['idb', 'bias', 'maskc'], [('ps', sb_i)])

                def emit_exp_pv(ix):
                    h, g0, grp = items[ix]
                    kvh = h if nkv == 4 else h // 2
                    sb_i = 1 + (ix % 3)
                    pt_i = ix % 3
                    Sv = ps[sb_i].rearrange("p (g q) -> p g q", g=4)
                    ng = len(grp)
                    ACT(PT[:, pt_i, 0:ng, :], Sv[:, 0:ng, :], AF.Exp, [('ps', sb_i)], [('pt', pt_i)])
                    for gi_, (kt, bspec) in enumerate(grp):
                        first = (g0 == 0 and gi_ == 0)
                        last = (g0 + gi_ == len(kts) - 1)
                        MM(OPS[:, h, :], PT[:, pt_i, gi_, :], VA[:, kt, kvh, :], first, last,
                           [('pt', pt_i), 'va'], [('ps', o_i)])

                step_pts = set([0, len(items) // 3, (2 * len(items)) // 3])
                emit_qk(0)
                if len(items) > 1:
                    emit_qk(1)
                for ix in range(len(items)):
                    if nxt is not None and ix in step_pts:
                        next(nxt, None)
                    if ix + 2 < len(items):
                        emit_qk(ix + 2)
                    emit_exp_pv(ix)
                if nxt is not None:
                    for _ in nxt:
                        pass
                den = RS[:, 32:36]
                if m == 2:
                    TT('dve', den, OPS[:, :, 64], SKE, ALU.add, [('ps', o_i), 'ske'], ['rs2'])
                else:
                    CP('dve', den, OPS[:, :, 64], [('ps', o_i)], ['rs2'])
                RCP(RS[:, 36:40], den, ['rs2'], ['rs2'])
                TT('dve', OB.rearrange("p (h d) -> p h d", h=4), OPS[:, :, 0:64],
                   RS[:, 36:40].rearrange("p (h o) -> p h o", o=1).to_broadcast([128, 4, 64]), ALU.mult,
                   [('ps', o_i), 'rs2'], ['ob'])
                transposes_to(lambda b: OT[:, b, :], OB, 2, 128, 'ob', 'ot', half=True)
                for hf in range(2):
                    cs = slice(hf * 512, (hf + 1) * 512)
                    for k in range(8):
                        MM(ps[5], HT[:, k, t * 128:(t + 1) * 128], WG[:, k, cs], k == 0, k == 7,
                           [('ht', t), 'wg'], [('ps', 5)])
                    for k in range(2):
                        MM(ps[6], OT[:, k, :], WBR[:, k, cs], k == 0, k == 1, ['ot', 'wbr'], [('ps', 6)])
                    ACT(SIG[:, cs], ps[5], AF.Tanh, [('ps', 5)], ['sig'], scale=0.5)
                    STT(MTL[:, cs], SIG[:, cs], 1.0, ps[6], ALU.add, ALU.mult, [('ps', 6), 'sig'], ['mtl'])
                DMA(md[m, t * 128:(t + 1) * 128, :], MTL, ['mtl'], [('md', m, t)])
                if m == 0 and t == 0:
                    ck(5)
                if m == 3 and t == 0:
                    ck(11)
                if m == 3 and t == 2:
                    ck(12)
            g0_ = q_prep(q_tiles[0], QT[:, 0], ('qt', 0))
            for _ in g0_:
                pass
            for idx_, t_ in enumerate(q_tiles):
                nx_ = None
                if idx_ + 1 < len(q_tiles):
                    nx_ = q_prep(q_tiles[idx_ + 1], QT[:, (idx_ + 1) % 2], ('qt', (idx_ + 1) % 2))
                q_body(t_, QT[:, idx_ % 2], ('qt', idx_ % 2), nx_)
            ck(6 + m)
        P.barrier()

        mod_tiles(l, 2 * D, 'gate', 0, which=(0,) if lastl else (0, 1))
        load_w(w_out[l], 8, D, WO, 'wo')
        for t in q_tiles:
            w = 0 if t >= 2 else 1
            DMA(M4, md[:, t * 128:(t + 1) * 128, :].rearrange("m p d -> p m d"),
                [('md', mm_, t) for mm_ in range(4)], ['m4'])
            DMA(XT, x_src(l)[t * 128:(t + 1) * 128, :], [], ['xt'])
            TT('dve', M4[:, 0, :], M4[:, 0, :], M4[:, 1, :], ALU.add, ['m4'], ['m4'])
            TT('pool', M4[:, 2, :], M4[:, 2, :], M4[:, 3, :], ALU.add, ['m4'], ['m4b'])
            TT('dve', M4[:, 0, :], M4[:, 0, :], M4[:, 2, :], ALU.add, ['m4', 'm4b'], ['m4'])
            pb = ps[1].bitcast(BF16)
            for k in range(8):
                TR(pb[:, k * 128:(k + 1) * 128], M4[:, 0, k * 128:(k + 1) * 128], IDB, ['m4', 'idb'], [('ps', 1)])
            CP('act', MT8, pb.rearrange("p (k q) -> p k q", k=8), [('ps', 1)], ['mt8'])
            for hf in range(2):
                cs = slice(hf * 512, (hf + 1) * 512)
                for k in range(8):
                    MM(ps[5 + hf], MT8[:, k, :], WO[:, k, cs], k == 0, k == 7, ['mt8', 'wo'], [('ps', 5 + hf)])
                TT('dve', HB[:, cs], ps[5 + hf], MOD[:, w, cs], ALU.mult, [('ps', 5 + hf), ('mod', w)], ['hb'])
                TT('pool', XS[:, t, cs], HB[:, cs], XT[:, cs], ALU.add, ['hb', 'xt'], [('x', t)])
        P.barrier()
        if dbg == ('xm', l):
            for t in q_tiles:
                DMA(dbg_out[t * 128:(t + 1) * 128, :], XS[:, t, :], [('x', t)], ['dbgo'])
            return True

        moe = (l % 2 == 1)
        mod_tiles(l, 4 * D, 'scale', 0, norm2_g[l:l + 1, :], which=(0,) if lastl else (0, 1))
        mod_tiles(l, 3 * D, 'shift', 2, which=(0,) if lastl else (0, 1))
        if moe:
            DMA(RT, moe_router[l // 2].rearrange("(k p) e -> p k e", p=128), [], ['rt'])
            P.op('dve', lambda e: e.memset(RTB, 0.0), [], ['rtb'])
            CP('dve', RTB[:, :, 0:8], RT, ['rt', 'rtb'], ['rtb'])
        for t in q_tiles:
            norm_tile(t, XS[:, t, :], [('x', t)], 0, 2, router=False)
            if moe:
                lg = ps[7][:, 0:NEXP]
                for k in range(8):
                    MM(ps[7][:, 0:16], HT[:, k, t * 128:(t + 1) * 128], RTB[:, k, :], k == 0, k == 7, [('ht', t), 'rtb'], [('ps', 7)])
                L = RS[:, 0:8]
                CP('dve', L, lg, [('ps', 7)], ['rs'])
                RED(RS[:, 8:9], L, ALU.max, ['rs'], ['rs'])
                TT('dve', RS[:, 16:24], L, RS[:, 8:9].to_broadcast([128, 8]), ALU.is_equal, ['rs'], ['rs'])
                STT(RS[:, 24:32], RS[:, 16:24], -1e30, L, ALU.mult, ALU.add, ['rs'], ['rs'])
                RED(RS[:, 9:10], RS[:, 24:32], ALU.max, ['rs'], ['rs'])
                TT('dve', RS[:, 40:48], RS[:, 24:32], RS[:, 9:10].to_broadcast([128, 8]), ALU.is_equal, ['rs'], ['rs'])
                TT('dve', RS[:, 10:11], RS[:, 9:10], RS[:, 8:9], ALU.subtract, ['rs'], ['rs'])
                ACT(RS[:, 11:12], RS[:, 10:11], AF.Exp, ['rs'], ['rs'])
                TS('dve', RS[:, 12:13], RS[:, 11:12], 1.0, None, ALU.add, None, ['rs'], ['rs'])
                RCP(RS[:, 13:14], RS[:, 12:13], ['rs'], ['rs'])
                TT('dve', RS[:, 14:15], RS[:, 11:12], RS[:, 13:14], ALU.mult, ['rs'], ['rs'])
                TT('dve', RS[:, 48:56], RS[:, 16:24], RS[:, 13:14].to_broadcast([128, 8]), ALU.mult, ['rs'], ['rs'])
                STT(COMB[:, t, :], RS[:, 40:48], RS[:, 14:15], RS[:, 48:56], ALU.mult, ALU.add, ['rs'], ['comb'])
                if t == 0:
                    ck(20)
        mod_tiles(l, 5 * D, 'gate', 0, which=(0,) if lastl else (0, 1))

        chunks = []
        if not lastl:
            chunks.append([0, 1])
        for c in range(4):
            chunks.append([2 + 4 * c + i for i in range(4)])
        nexp = NEXP if moe else 1

        def wset(s_):
            o = s_ * 6144
            return (R4[:, o:o + 2048].rearrange("p (k c) -> p k c", k=8),
                    R4[:, o + 2048:o + 4096].rearrange("p (k c) -> p k c", k=8),
                    R4[:, o + 4096:o + 6144].rearrange("p (k c) -> p k c", k=2),
                    MOD[:, 2 + s_, :].bitcast(BF16).rearrange("p (k c) -> p k c", k=2))

        def g2b(w):
            return MOD[:, w, :].rearrange("p (o d) -> p o d", o=1).to_broadcast([128, 2, D])

        groups = [(e_, g) for e_ in range(nexp) for g in range(DFF // 256)]

        def wsrc(e_):
            if moe:
                return moe_w1[l // 2, e_], moe_w3[l // 2, e_], moe_w2[l // 2, e_]
            return ffn_w1[l // 2], ffn_w3[l // 2], ffn_w2[l // 2]

        def loads(gix):
            e_, g = groups[gix]
            s_ = gix % 2
            w1, w3, w2 = wsrc(e_)
            W1s, W3s, W2s, W2cs = wset(s_)
            load_w(w1[:, g * 256:(g + 1) * 256], 8, 256, None, None, cast=False,
                   use=lambda sv, c0, cw, skey: CP('act', W1s, sv, [skey], [('w1b', s_)]))
            load_w(w3[:, g * 256:(g + 1) * 256], 8, 256, None, None, cast=False,
                   use=lambda sv, c0, cw, skey: CP('act', W3s, sv, [skey], [('w3b', s_)]))

            def use_w2(sv, c0, cw, skey):
                TT('pool', W2s, sv, g2b(0), ALU.mult, [skey, ('mod', 0)], [('w2b', s_)])
                if not lastl:
                    TT('pool', W2cs, sv, g2b(1), ALU.mult, [skey, ('mod', 1)], [('mod', 2 + s_)])
            load_w(w2[g * 256:(g + 1) * 256, :], 2, D, None, None, cast=False, use=use_w2)

        steps = [(gix, cix) for gix in range(len(groups)) for cix in range(len(chunks))]

        def ab(six):
            gix, cix = steps[six]
            s_ = gix % 2
            ch = chunks[cix]
            n = len(ch) * 128
            t0 = ch[0] * 128
            up = (six % 2) * 2
            W1s, W3s, _w2, _w2c = wset(s_)
            for fc in range(2):
                for k in range(8):
                    MM(ps[fc][:, 0:n], W1s[:, k, fc * 128:(fc + 1) * 128], HT[:, k, t0:t0 + n], k == 0, k == 7,
                       [('w1b', s_)] + [('ht', t) for t in ch], [('ps', fc)])
                for k in range(8):
                    MM(ps[2 + fc][:, 0:n], W3s[:, k, fc * 128:(fc + 1) * 128], HT[:, k, t0:t0 + n], k == 0, k == 7,
                       [('w3b', s_)] + [('ht', t) for t in ch], [('ps', 2 + fc)])
                ACT(SA[:, fc, 0:n], ps[fc][:, 0:n], AF.Silu, [('ps', fc)], [('sa', fc)])
                TT('dve', UT[:, up + fc, 0:n], ps[2 + fc][:, 0:n], SA[:, fc, 0:n], ALU.mult,
                   [('ps', 2 + fc), ('sa', fc)], [('ut', up + fc)])

        def w2s(six):
            gix, cix = steps[six]
            e_ = groups[gix][0]
            s_ = gix % 2
            ch = chunks[cix]
            up = (six % 2) * 2
            _w1, _w3, W2s, W2cs = wset(s_)
            for ti, t in enumerate(ch):
                ob_ = 4 + 2 * (ti % 2)
                wsel, wkey = (W2s, ('w2b', s_)) if t >= 2 else (W2cs, ('mod', 2 + s_))
                for hf in range(2):
                    cs = slice(hf * 512, (hf + 1) * 512)
                    for fc in range(2):
                        MM(ps[ob_ + hf], UT[:, up + fc, ti * 128:(ti + 1) * 128], wsel[:, fc, cs], fc == 0, fc == 1,
                           [('ut', up + fc), wkey], [('ps', ob_ + hf)])
                    if moe:
                        STT(XS[:, t, cs], ps[ob_ + hf], COMB[:, t, e_:e_ + 1], XS[:, t, cs], ALU.mult, ALU.add,
                            [('ps', ob_ + hf), 'comb', ('x', t)], [('x', t)])
                    else:
                        TT('dve', XS[:, t, cs], ps[ob_ + hf], XS[:, t, cs], ALU.add,
                           [('ps', ob_ + hf), ('x', t)], [('x', t)])

        loads(0)
        if len(groups) > 1:
            loads(1)
        ab(0)
        for six in range(len(steps)):
            if six + 1 < len(steps):
                ab(six + 1)
            w2s(six)
            gix, cix = steps[six]
            if cix == len(chunks) - 1 and gix + 2 < len(groups):
                loads(gix + 2)
        P.barrier()
        if dbg == ('xf', l):
            for t in q_tiles:
                DMA(dbg_out[t * 128:(t + 1) * 128, :], XS[:, t, :], [('x', t)], ['dbgo'])
            return True
        if not lastl:
            for t in range(NT):
                DMA(xd[t * 128:(t + 1) * 128, :], XS[:, t, :], [('x', t)], [('xd', t)])
            P.barrier()
        return False

    stopped = False
    try:
        for l in range(n_layers):
            stopped = layer(l)
            if stopped:
                break
    except _Stop:
        stopped = True
    if not stopped and n_layers == DEPTH:
        DMA(MOD[:, 0, :], final_g.partition_broadcast(128), [], [('mod', 0)])
        for t in range(2, NT):
            src = XS[:, t, :]
            ACT(HB, src, AF.Square, [('x', t)], ['hb'], accum_out=STAT[:, 0:1])
            ACT(STAT[:, 1:2], STAT[:, 0:1], AF.Sqrt, ['hb'], ['stat'], bias=EPSB, scale=1.0 / D)
            RCP(STAT[:, 2:3], STAT[:, 1:2], ['stat'], ['stat'])
            STT(HB, src, STAT[:, 2:3], MOD[:, 0, :], ALU.mult, ALU.mult, [('x', t), 'stat', ('mod', 0)], ['hb'])
            DMA(out[(t - 2) * 128:(t - 1) * 128, :], HB, ['hb'], ['out'])
    elif not stopped:
        DMA(out[0:128, :], HB, [], ['out'])
    P.barrier()
    P.emit()
    print("instructions:", P.n, {k: len(v) for k, v in P.streams.items()})


_CACHE = {}


def make_in_maps(inp, n_cores=8):
    f = lambda a: np.ascontiguousarray(np.asarray(a), dtype=np.float32)
    cosH, sinH, cosR, sinR = _rope_tables()
    shared = {
        "norm1_g": f(inp["norm1_g"]), "norm2_g": f(inp["norm2_g"]), "w_ada": f(inp["w_ada"]),
        "b_ada": f(inp["b_ada"]), "w_in": f(inp["w_in"]), "biasA": _natten_bias(f(inp["na_rpb"])),
        "smallp": np.ascontiguousarray(np.concatenate(
            [f(inp["gb_qnorm"]), f(inp["gb_knorm"]), f(inp["mla_qnorm"]), f(inp["mla_kvnorm"]), f(inp["wc_sink"])],
            axis=1)),
        "mla_wqb": f(inp["mla_wqb"]), "mla_wkvb": f(inp["mla_wkvb"]), "w_branch": f(inp["w_branch"]),
        "w_out": f(inp["w_out"]), "ffn_w1": f(inp["ffn_w1"]), "ffn_w3": f(inp["ffn_w3"]), "ffn_w2": f(inp["ffn_w2"]),
        "moe_router": f(inp["moe_router"]), "moe_w1": f(inp["moe_w1"]), "moe_w3": f(inp["moe_w3"]),
        "moe_w2": f(inp["moe_w2"]), "final_g": f(inp["final_g"]).reshape(1, D),
        "c_ident": np.eye(128, dtype=np.float32),
        "c_ropeH": np.ascontiguousarray(np.stack([cosH, sinH])),
        "c_ropeR": np.ascontiguousarray(np.stack([cosR, sinR])),
        "c_maskC": _maskC(),
    }
    x = f(inp["x"]); ctx = f(inp["ctx"]); c = f(inp["c"]); cc = f(inp["c_ctx"])
    maps = []
    for b in range(n_cores):
        m = dict(shared)
        m["xin"] = np.ascontiguousarray(np.concatenate([ctx[b], x[b]], axis=0))
        m["cvec"] = np.ascontiguousarray(np.stack([c[b], cc]))
        maps.append(m)
    return maps


def kernel(**inputs):
    if "nc" not in _CACHE:
        _CACHE["nc"] = build(DEPTH)
    nc = _CACHE["nc"]
    maps = make_in_maps(inputs, 8)
    res = run_bass_kernel_spmd(nc, maps, core_ids=list(range(8)))
    return np.stack([np.asarray(r["out"], dtype=np.float32) for r in res.results], axis=0)
```

```python
import numpy as np
from contextlib import ExitStack
import concourse.bass as bass
import concourse.mybir as mybir
from concourse.bass_utils import run_bass_kernel_spmd

F32 = mybir.dt.float32
BF16 = mybir.dt.bfloat16
AF = mybir.ActivationFunctionType
ALU = mybir.AluOpType
AX = mybir.AxisListType

D = 1024
S = 2048
C = 256
T = S + C
NT = T // 128
DEPTH = 4
IN_W = 6304
DFF = 3584
NEXP = 8
EPS = 1e-6
NEG = -30000.0
NDMA = 12
NSTG = 3

OFF = dict(aq=0, ak=256, av=512, bq=768, bk=1024, bv=1152, cq=1280, ck=1536, cv=1664,
           dqa=1792, dkva=2048, gate=2208)


class Prog:
    CE = ['pe', 'act', 'dve', 'pool']

    def __init__(self, nc, es):
        self.nc = nc
        self.streams = {e: [] for e in self.CE + ['sp']}
        self.sems = {}
        for e in self.CE:
            self.sems[e] = es.enter_context(nc.semaphore("s_" + e))
        for i in range(NDMA):
            self.sems['d%d' % i] = es.enter_context(nc.semaphore("s_d%d" % i))
        self.cnt = {k: 0 for k in self.sems}
        self.known = {e: {k: 0 for k in self.sems} for e in self.streams}
        self.lastw = {}
        self.readers = {}
        self.dma_rr = 0
        self.n = 0
        self.last_real = {e: None for e in self.CE}
        self.pend = {e: False for e in self.CE}

    def _materialize(self, s):
        if s in self.pend and self.pend[s]:
            self.streams[s][self.last_real[s]][2] = s
            self.cnt[s] += 1
            self.pend[s] = False

    def _deps(self, eng, reads, writes):
        deps = {}

        def add(d):
            if d is not None:
                if d[0] == 'pe' and eng == 'pe':
                    return
                if deps.get(d[0], 0) < d[1]:
                    deps[d[0]] = d[1]
        for k in reads:
            add(self.lastw.get(k))
        for k in writes:
            add(self.lastw.get(k))
            for r in self.readers.get(k, ()):
                add(r)
        waits = []
        kn = self.known[eng]
        for s, i in deps.items():
            if kn[s] < i:
                if i > self.cnt[s]:
                    self._materialize(s)
                assert i <= self.cnt[s], (s, i, self.cnt[s])
                kn[s] = i
                waits.append((s, i))
        return waits

    def _commit(self, tok, reads, writes):
        for k in writes:
            self.lastw[k] = tok
            self.readers[k] = []
        for k in reads:
            self.readers.setdefault(k, []).append(tok)

    def op(self, eng, fn, reads=(), writes=()):
        waits = self._deps(eng, reads, writes)
        tok = (eng, self.cnt[eng] + 1)
        self.streams[eng].append([waits, fn, None])
        self.last_real[eng] = len(self.streams[eng]) - 1
        self.pend[eng] = True
        self._commit(tok, reads, writes)
        self.n += 1

    def dma(self, fn, reads=(), writes=()):
        s = 'd%d' % self.dma_rr
        self.dma_rr = (self.dma_rr + 1) % NDMA
        waits = self._deps('sp', reads, writes)
        if self.known['sp'][s] < self.cnt[s]:
            self.known['sp'][s] = self.cnt[s]
            waits.append((s, self.cnt[s]))
        self.cnt[s] += 1
        tok = (s, self.cnt[s])
        self.streams['sp'].append([waits, fn, s])
        self._commit(tok, reads, writes)
        self.n += 1

    def barrier(self):
        for e in self.CE:
            self._materialize(e)
        snap = dict(self.cnt)
        for e in self.streams:
            waits = []
            for s, i in snap.items():
                if self.known[e][s] < i:
                    self.known[e][s] = i
                    waits.append((s, i))
            if waits:
                self.streams[e].append([waits, None, None])

    def emit(self):
        nc = self.nc
        sems = self.sems

        def mult(s):
            return 16 if s[0] == 'd' else 1

        def run(engine, stream):
            for waits, fn, inc in stream:
                for s, i in waits:
                    engine.wait_ge(sems[s], i * mult(s))
                if fn is None:
                    continue
                ins = fn(engine)
                if inc is not None:
                    ins.then_inc(sems[inc], mult(inc))

        with nc.Block() as block:
            @block.tensor
            def _(e):
                run(e, self.streams['pe'])

            @block.scalar
            def _(e):
                run(e, self.streams['act'])

            @block.vector
            def _(e):
                run(e, self.streams['dve'])

            @block.gpsimd
            def _(e):
                run(e, self.streams['pool'])

            @block.sync
            def _(e):
                run(e, self.streams['sp'])


def _rope_tables():
    def ang(pos, dim):
        inv = (10000.0 ** (-np.arange(0, dim, 2, dtype=np.float32) / dim)).astype(np.float32)
        return pos.astype(np.float32)[:, None] * inv[None, :]
    t = np.arange(S)
    out = []
    for rot in (64, 32):
        half = rot // 2
        a = np.concatenate([ang(t // 64, half), ang(t % 64, half)], axis=-1).astype(np.float32)
        out += [np.cos(a).astype(np.float32), np.sin(a).astype(np.float32)]
    return out


def _natten_plan():
    rows = S // 64
    kh = 8
    pats = {}
    plan = []
    for i in range(16):
        lst = []
        for j in range(16):
            sig = []
            anyv = False
            for a in range(2):
                for b in range(2):
                    r = 2 * i + b
                    rk = 2 * j + a
                    st = min(max(r - kh // 2, 0), rows - kh)
                    if st <= rk < st + kh:
                        sig.append(rk - r + 7)
                        anyv = True
                    else:
                        sig.append(-1)
            if anyv:
                sig = tuple(sig)
                if sig not in pats:
                    pats[sig] = len(pats)
                lst.append((j, pats[sig]))
        plan.append(lst)
    return plan, pats


NAT_PLAN, NAT_PATS = _natten_plan()
NPAT = len(NAT_PATS)


def _natten_bias(rpb):
    col = np.arange(64)
    cstart = np.clip(col - 8, 0, 48)
    col_ok = (col[None, :] >= cstart[:, None]) & (col[None, :] < cstart[:, None] + 16)
    dc = np.clip(col[None, :] - col[:, None] + 15, 0, 30)
    out = np.full((DEPTH, 128, NPAT * 4, 128), NEG, dtype=np.float32)
    for sig, pid in NAT_PATS.items():
        for a in range(2):
            for b in range(2):
                dr = sig[a * 2 + b]
                if dr < 0:
                    continue
                g = rpb[:, :, dr, :][:, :, dc]
                g = np.where(col_ok[None, None], g, np.float32(NEG))
                g = np.transpose(g, (0, 3, 1, 2))
                out[:, a * 64:(a + 1) * 64, pid * 4:(pid + 1) * 4, b * 64:(b + 1) * 64] = g
    return out


def _maskC():
    kk = np.arange(128)[:, None]
    qq = np.arange(128)[None, :]
    m0 = np.where(qq <= kk, 0.0, NEG).astype(np.float32)
    m1 = np.where(kk <= qq, 0.0, NEG).astype(np.float32)
    return np.stack([m0, m1], axis=1)


def build(n_layers=DEPTH, dbg=None):
    nc = bass.Bass("TRN2", target_bir_lowering=False, dynamic_dma_scratch_size=256)
    es = ExitStack()
    with es:
        _build_body(nc, es, n_layers, dbg)
    return nc


def _build_body(nc, es, n_layers, dbg):
    def din(name, shape):
        return nc.dram_tensor(name, list(shape), F32, kind="ExternalInput").ap()

    xin = din("xin", [T, D])
    cvec = din("cvec", [2, D])
    norm1_g = din("norm1_g", [DEPTH, D])
    norm2_g = din("norm2_g", [DEPTH, D])
    w_ada = din("w_ada", [DEPTH, D, 6 * D])
    b_ada = din("b_ada", [DEPTH, 6 * D])
    w_in = din("w_in", [DEPTH, D, IN_W])
    biasA = din("biasA", [DEPTH, 128, NPAT * 4, 128])
    smallp = din("smallp", [DEPTH, 516])
    mla_wqb = din("mla_wqb", [DEPTH, 256, 384])
    mla_wkvb = din("mla_wkvb", [DEPTH, 128, 512])
    w_branch = din("w_branch", [DEPTH, 4, 256, D])
    w_out = din("w_out", [DEPTH, D, D])
    ffn_w1 = din("ffn_w1", [2, D, DFF])
    ffn_w3 = din("ffn_w3", [2, D, DFF])
    ffn_w2 = din("ffn_w2", [2, DFF, D])
    moe_router = din("moe_router", [2, D, NEXP])
    moe_w1 = din("moe_w1", [2, NEXP, D, DFF])
    moe_w3 = din("moe_w3", [2, NEXP, D, DFF])
    moe_w2 = din("moe_w2", [2, NEXP, DFF, D])
    final_g = din("final_g", [1, D])
    c_ident = din("c_ident", [128, 128])
    c_ropeH = din("c_ropeH", [2, S, 32])
    c_ropeR = din("c_ropeR", [2, S, 16])
    c_maskC = din("c_maskC", [128, 2, 128])
    out = nc.dram_tensor("out", [S, D], F32, kind="ExternalOutput").ap()
    xd = nc.dram_tensor("xd", [T, D], F32, kind="Internal").ap()
    md = nc.dram_tensor("md", [4, T, D], BF16, kind="Internal").ap()
    dbg_out = None
    if dbg is not None:
        dbg_out = nc.dram_tensor("dbg", [T, D], F32, kind="ExternalOutput").ap()

    P = Prog(nc, es)

    def sb(name, shape, dt):
        return es.enter_context(nc.sbuf_tensor(name, list(shape), dt))[:]

    ps = [es.enter_context(nc.psum_tensor("ps%d" % i, [128, 512], F32))[:] for i in range(8)]

    IDF = sb("idf", [128, 128], F32)
    IDB = sb("idb", [128, 128], BF16)
    ROPEH = sb("ropeh", [128, 2, 16, 32], F32)
    ROPER = sb("roper", [128, 2, 16, 16], F32)
    MASKC = sb("maskc", [128, 2, 128], BF16)
    CV = sb("cv", [128, 2, 8], F32)
    SL = sb("sl", [128, 2, 8, 128], F32)
    MOD = sb("mod", [128, 4, D], F32)
    SMP = sb("smp", [128, 516], F32)
    SKE = sb("ske", [128, 4], F32)
    HT = sb("ht", [128, 8, T], BF16)
    STG = sb("stg", [128, NSTG, 2048], F32)
    XT = sb("xt", [128, D], F32)
    HB = sb("hb", [128, D], F32)
    PB = HB
    STAT = sb("stat", [128, 32], F32)
    PBUF = sb("pbuf", [128, D], F32)
    TMPF = sb("tmpf", [128, 4, 256], F32)
    PB16 = sb("pb16", [128, 512], BF16)
    QT = sb("qt", [128, 2, 4, 128], BF16)
    OB = sb("ob", [128, 256], BF16)
    OT = sb("ot", [128, 2, 128], BF16)
    DTT = sb("dtt", [128, 2, 128], BF16)
    MT8 = sb("mt8", [128, 8, 128], BF16)
    RT = sb("rt", [128, 8, NEXP], F32)
    RTB = sb("rtb", [128, 8, 16], BF16)
    COMB = sb("comb", [128, NT, NEXP], F32)
    RS = sb("rs", [128, 64], F32)
    UT = sb("ut", [128, 4, 512], BF16)
    SA = sb("sa", [128, 2, 512], BF16)
    R4 = sb("r4", [128, 12288], BF16)
    R2 = sb("r2", [128, 36864], BF16)

    KT = R2[:, 0:9216].rearrange("p (h t) -> p h t", h=4)
    VA = R2[:, 9216:13896].rearrange("p (t h d) -> p t h d", t=NT, h=4)
    BIAS = R2[:, 13896:13896 + NPAT * 4 * 128].rearrange("p (n q) -> p n q", q=128)
    XS = R2.bitcast(F32).rearrange("p (t d) -> p t d", t=NT)
    b0 = 13896 + NPAT * 4 * 128
    WG = R2[:, b0:b0 + 8192].rearrange("p (k c) -> p k c", k=8)
    WKV = WG
    WQ = R2[:, b0 + 8192:b0 + 10240].rearrange("p (k c) -> p k c", k=8)
    WBR = R2[:, b0 + 10240:b0 + 12288].rearrange("p (k c) -> p k c", k=2)
    WQB = R2[:, b0 + 12288:b0 + 13056].rearrange("p (k c) -> p k c", k=2)
    WKVB = R2[:, b0 + 13056:b0 + 13568]
    SIG = R2[:, b0 + 13568:b0 + 14592]
    MTL = R2[:, b0 + 14592:b0 + 15616]
    PT = R2[:, b0 + 15616:b0 + 17152].rearrange("p (a g q) -> p a g q", a=3, g=4)
    assert b0 + 17152 <= 36864
    WO = R4[:, 0:8192].rearrange("p (k c) -> p k c", k=8)
    M4 = MOD[:, 2:4, :].rearrange("p a d -> p (a d)").bitcast(BF16).rearrange("p (a d) -> p a d", a=4)
    H2F = PBUF.rearrange("p (k q) -> p k q", k=8)
    W1B = R4[:, 0:4096].rearrange("p (k c) -> p k c", k=8)
    W3B = R4[:, 4096:8192].rearrange("p (k c) -> p k c", k=8)
    W2B = R4[:, 8192:12288].rearrange("p (k c) -> p k c", k=4)

    def MM(o, lhsT, rhs, start, stop, r, w):
        P.op('pe', lambda e: e.matmul(o, lhsT, rhs, start=start, stop=stop), r, w)

    def TR(o, i, ident, r, w):
        P.op('pe', lambda e: e.transpose(o, i, ident), r, w)

    def ACT(o, i, func, r, w, **kw):
        P.op('act', lambda e: e.activation(o, i, func, **kw), r, w)

    def TT(eng, o, a, b, op, r, w):
        P.op(eng, lambda e: e.tensor_tensor(o, a, b, op), r, w)

    def TS(eng, o, a, s1, s2, op0, op1, r, w):
        if op1 is None:
            P.op(eng, lambda e: e.tensor_scalar(o, a, s1, None, op0), r, w)
        else:
            P.op(eng, lambda e: e.tensor_scalar(o, a, s1, s2, op0, op1), r, w)

    def STT(o, a, s, b, op0, op1, r, w):
        P.op('dve', lambda e: e.scalar_tensor_tensor(o, a, s, b, op0, op1), r, w)

    def CP(eng, o, i, r, w):
        if eng == 'act':
            P.op('act', lambda e: e.activation(o, i, AF.Copy), r, w)
        else:
            P.op(eng, lambda e: e.tensor_copy(o, i), r, w)

    def RED(o, i, op, r, w):
        P.op('dve', lambda e: e.tensor_reduce(o, i, AX.X, op), r, w)

    def RCP(o, i, r, w):
        P.op('dve', lambda e: e.reciprocal(o, i), r, w)

    def DMA(o, i, r, w):
        P.dma(lambda e: e.dma_start(out=o, in_=i), r, w)

    stg_rr = [0]

    def load_w(src, kc, ncols, dst, dkey, cast=True, use=None):
        pcw = 2048 // kc
        c0 = 0
        while c0 < ncols:
            cw = min(pcw, ncols - c0)
            s = stg_rr[0]
            stg_rr[0] = (s + 1) % NSTG
            sv = STG[:, s, 0:kc * cw].rearrange("p (k c) -> p k c", k=kc)
            DMA(sv, src[:, c0:c0 + cw].rearrange("(k p) c -> p k c", p=128), [], [('stg', s)])
            if cast:
                CP('pool', dst[:, :, c0:c0 + cw], sv, [('stg', s)], [dkey])
            else:
                use(sv, c0, cw, ('stg', s))
            c0 += cw

    import os
    KS = int(os.environ.get("KSETUP", "99"))
    if KS > 0:
        DMA(IDF, c_ident, [], ['idf'])
        CP('dve', IDB, IDF, ['idf'], ['idb'])
    if KS > 1:
        DMA(ROPEH[:, 0], c_ropeH[0].rearrange("(t p) d -> p t d", p=128), [], ['rope'])
        DMA(ROPEH[:, 1], c_ropeH[1].rearrange("(t p) d -> p t d", p=128), [], ['rope'])
        DMA(ROPER[:, 0], c_ropeR[0].rearrange("(t p) d -> p t d", p=128), [], ['rope'])
        DMA(ROPER[:, 1], c_ropeR[1].rearrange("(t p) d -> p t d", p=128), [], ['rope'])
    if KS > 2:
        DMA(PBUF[:, 0:256].rearrange("p (a q) -> p a q", a=2), c_maskC, [], ['pbuf'])
        CP('dve', MASKC, PBUF[:, 0:256].rearrange("p (a q) -> p a q", a=2), ['pbuf'], ['maskc'])
    if KS > 3:
        P.dma(lambda e: e.dma_start(out=CV, in_=cvec.rearrange("w (k p) -> p w k", p=128),
                                    allow_slow_non_contiguous=True), [], ['cv'])
    if KS > 4:
        ACT(CV, CV, AF.Silu, ['cv'], ['cv'])
    if KS > 5:
        for wch in range(2):
            CP('dve', SL[:, wch], CV[:, wch, :].rearrange("p (k o) -> p k o", o=1).to_broadcast([128, 8, 128]),
               ['cv'], ['sl'])

    def rope_view(tab, which, t, nh, half):
        return tab[:, which, t - 2, :].rearrange("p (o d) -> p o d", o=1).to_broadcast([128, nh, half])

    def rope(dst, src, tab, t, nh, half, skey, dkey):
        c = rope_view(tab, 0, t, nh, half)
        s = rope_view(tab, 1, t, nh, half)
        x1 = src[:, :, 0:half]
        x2 = src[:, :, half:2 * half]
        t1 = TMPF[:, 0, 0:nh * half].rearrange("p (h d) -> p h d", h=nh)
        t2 = TMPF[:, 1, 0:nh * half].rearrange("p (h d) -> p h d", h=nh)
        t3 = TMPF[:, 2, 0:nh * half].rearrange("p (h d) -> p h d", h=nh)
        t4 = TMPF[:, 3, 0:nh * half].rearrange("p (h d) -> p h d", h=nh)
        TT('dve', t1, x1, c, ALU.mult, [skey, 'rope'], ['tf0'])
        TT('pool', t2, x2, s, ALU.mult, [skey, 'rope'], ['tf1'])
        TT('dve', t3, x1, s, ALU.mult, [skey, 'rope'], ['tf2'])
        TT('pool', t4, x2, c, ALU.mult, [skey, 'rope'], ['tf3'])
        TT('dve', dst[:, :, 0:half], t1, t2, ALU.subtract, ['tf0', 'tf1'], [dkey])
        TT('dve', dst[:, :, half:2 * half], t3, t4, ALU.add, ['tf2', 'tf3'], [dkey])

    def head_rms(src, nh, hd, gain, skey):
        sq = TMPF[:, 0:1, :].rearrange("p a c -> p (a c)")[:, 0:nh * hd].rearrange("p (h d) -> p h d", h=nh)
        TT('dve', sq, src, src, ALU.mult, [skey], ['tf0'])
        RED(RS[:, 0:nh], sq, ALU.add, ['tf0'], ['rs'])
        TS('dve', RS[:, 8:8 + nh], RS[:, 0:nh], 1.0 / hd, EPS, ALU.mult, ALU.add, ['rs'], ['rs'])
        TT('pool', RS[:, 16:16 + nh], RS[:, 8:8 + nh], NHALF[:, 0:nh], ALU.pow, ['rs', 'nhalf'], ['rs'])
        TT('dve', src, src, RS[:, 16:16 + nh].rearrange("p (h o) -> p h o", o=1).to_broadcast([128, nh, hd]),
           ALU.mult, [skey, 'rs'], [skey])
        TT('dve', src, src, gain.rearrange("p (o d) -> p o d", o=1).to_broadcast([128, nh, hd]),
           ALU.mult, [skey, 'smp'], [skey])

    EPSB = sb("epsb", [128, 1], F32)
    P.op('dve', lambda e: e.memset(EPSB, EPS), [], ['epsb'])
    NHALF = sb("nhalf", [128, 8], F32)
    P.op('dve', lambda e: e.memset(NHALF, -0.5), [], ['nhalf'])

    def mod_tiles(l, col0, kind, slot, gain_src=None, which=(0, 1)):
        DMA(PB, b_ada[l:l + 1, col0:col0 + D].partition_broadcast(128), [], ['hb'])

        def use(sv, c0, cw, skey):
            for w in which:
                bank = ps[6 + w]
                for k in range(8):
                    MM(bank[:, 0:cw], SL[:, w, k, :], sv[:, k, :], k == 0, k == 7,
                       ['sl', skey], [('ps', 6 + w)])
                TT('dve', MOD[:, slot + w, c0:c0 + cw], bank[:, 0:cw], PB[:, c0:c0 + cw], ALU.add,
                   [('ps', 6 + w), 'hb'], [('mod', slot + w)])
        load_w(w_ada[l][:, col0:col0 + D], 8, D, None, None, cast=False, use=use)
        if kind == 'scale':
            DMA(PB, gain_src.partition_broadcast(128), [], ['hb'])
            for w in which:
                STT(MOD[:, slot + w], MOD[:, slot + w], 1.0, PB, ALU.add, ALU.mult,
                    [('mod', slot + w), 'hb'], [('mod', slot + w)])

    def norm_tile(t, src_ap, src_keys, gslot, sslot, router=False, xkey=None):
        w = 0 if t >= 2 else 1
        ACT(HB, src_ap, AF.Square, src_keys, ['hb'], accum_out=STAT[:, 0:1])
        ACT(STAT[:, 1:2], STAT[:, 0:1], AF.Sqrt, ['hb'], ['stat'], bias=EPSB, scale=1.0 / D)
        RCP(STAT[:, 2:3], STAT[:, 1:2], ['stat'], ['stat'])
        STT(HB, src_ap, STAT[:, 2:3], MOD[:, gslot + w], ALU.mult, ALU.mult,
            src_keys + ['stat', ('mod', gslot + w)], ['hb'])
        TT('dve', HB, HB, MOD[:, sslot + w], ALU.add, ['hb', ('mod', sslot + w)], ['hb'])
        for half in range(2):
            bank = ps[half]
            for kk in range(4):
                k = half * 4 + kk
                TR(bank[:, kk * 128:(kk + 1) * 128], HB[:, k * 128:(k + 1) * 128], IDF,
                   ['hb', 'idf'], [('ps', half)])
            pv = bank.rearrange("p (k q) -> p k q", k=4)
            CP('act' if half == 0 else 'dve', HT[:, half * 4:half * 4 + 4, t * 128:(t + 1) * 128], pv,
               [('ps', half)], [('ht', t)])
            if router:
                CP('dve' if half == 0 else 'act', H2F[:, half * 4:half * 4 + 4, :], pv,
                   [('ps', half)], ['h2f'])


    class _Stop(Exception):
        pass
    KSTOP = int(os.environ.get('KSTOP', '0'))

    def ck(n):
        if KSTOP == n:
            raise _Stop()

    def x_src(l):
        return xin if l == 0 else xd

    def kv_tiles(m, t, lastl):
        if t < 2:
            return [(0, None), (1, None)]
        i = t - 2
        if m == 0:
            return [(j + 2, ('A', pid)) for (j, pid) in NAT_PLAN[i]] + [(0, None), (1, None)]
        if m == 2:
            lst = []
            if i - 1 >= 0:
                lst.append((t - 1, ('C', 0)))
            lst.append((t, None))
            if i + 1 < 16:
                lst.append((t + 1, ('C', 1)))
            return lst + [(0, None), (1, None)]
        return [(j, None) for j in range(NT)]

    GQN = SMP[:, 0:64]
    GKN = SMP[:, 64:128]
    MQN = SMP[:, 128:384]
    MKVN = SMP[:, 384:512]

    def evac_scaled(dst, src_ps, pkey, dkey, scale):
        P.op('dve', lambda e: e.tensor_scalar(dst, src_ps, scale, None, ALU.mult), [pkey], [dkey])

    def transposes_to(dst_fn, src16, nblk, width, skey, dkey, bank_i=1, half=False):
        pb = ps[bank_i].bitcast(BF16)
        pkey = ('ps', bank_i)
        if half:
            pb = ps[0].bitcast(BF16)[:, 512:1024]
            pkey = ('ps', '0b')
        for b in range(nblk):
            TR(pb[0:width, b * 128:(b + 1) * 128], src16[:, b * width:(b + 1) * width], IDB,
               [skey, 'idb'], [pkey])
        for b in range(nblk):
            CP('dve' if (half or b % 2 == 1) else 'act', dst_fn(b), pb[0:width, b * 128:(b + 1) * 128],
               [pkey], [dkey])

    def proj(bank_i, t, wview, ncols, wkey):
        for k in range(8):
            MM(ps[bank_i][:, 0:ncols], HT[:, k, t * 128:(t + 1) * 128], wview[:, k, 0:ncols], k == 0, k == 7,
               [('ht', t), wkey], [('ps', bank_i)])

    att_i = [0]

    def layer(l):
        lastl = (l == DEPTH - 1)
        q_tiles = list(range(2, NT)) if lastl else list(range(NT))
        DMA(SMP, smallp[l:l + 1, :].partition_broadcast(128), [], ['smp'])
        ACT(SKE, SMP[:, 512:516], AF.Exp, ['smp'], ['ske'])
        mod_tiles(l, 1 * D, 'scale', 0, norm1_g[l:l + 1, :])
        mod_tiles(l, 0 * D, 'shift', 2)
        ck(1)
        for t in range(NT):
            DMA(XT, x_src(l)[t * 128:(t + 1) * 128, :], [], ['xt'])
            norm_tile(t, XT, ['xt'], 0, 2)
        P.barrier()
        ck(2)

        for m in [int(c_) for c_ in os.environ.get('KMIX', '0123')]:
            nh = 4
            nkv = 4 if m in (0, 3) else 2
            hd = 128 if m == 3 else 64
            if m == 0:
                load_w(w_in[l][:, OFF['ak']:OFF['ak'] + 512], 8, 512, WKV, 'wg')
                DMA_bias = True
                for c0 in range(0, NPAT * 4, 16):
                    c1 = min(c0 + 16, NPAT * 4)
                    s_ = stg_rr[0]
                    stg_rr[0] = (s_ + 1) % NSTG
                    sv = STG[:, s_, 0:(c1 - c0) * 128].rearrange("p (n q) -> p n q", q=128)
                    DMA(sv, biasA[l][:, c0:c1, :], [], [('stg', s_)])
                    CP('pool', BIAS[:, c0:c1, :], sv, [('stg', s_)], ['bias'])
                kvw = 512
            elif m == 1:
                load_w(w_in[l][:, OFF['bk']:OFF['bk'] + 256], 8, 256, WKV, 'wg')
                kvw = 256
            elif m == 2:
                load_w(w_in[l][:, OFF['ck']:OFF['ck'] + 256], 8, 256, WKV, 'wg')
                kvw = 256
            else:
                load_w(w_in[l][:, OFF['dkva']:OFF['dkva'] + 160], 8, 160, WKV, 'wg')
                load_w(mla_wkvb[l], 1, 512, WKVB.rearrange("p (k c) -> p k c", k=1), 'wkvb')
                kvw = 160
            if m == 0:
                ck(3)
            P.op('dve', lambda e: e.memset(VA[:, :, :, 64:65], 1.0), [], ['va'])
            for t in range(NT):
                lat = t >= 2
                proj(0, t, WKV, kvw, 'wg')
                CP('act', PBUF[:, 0:kvw], ps[0][:, 0:kvw], [('ps', 0)], ['pbuf'])
                if m == 0:
                    CP('dve', PB16[:, 0:256], PBUF[:, 0:256], ['pbuf'], ['pb16'])
                    CP('pool', VA[:, t, :, 0:64], PBUF[:, 256:512].rearrange("p (h d) -> p h d", h=4), ['pbuf'], ['va'])
                    transposes_to(lambda b: KT[0:64, b, t * 128:(t + 1) * 128], PB16, 4, 64, 'pb16', 'kt')
                elif m in (1, 2):
                    kview = PBUF[:, 0:128].rearrange("p (h d) -> p h d", h=2)
                    if m == 1:
                        head_rms(kview, 2, 64, GKN, 'pbuf')
                    k16 = PB16[:, 0:128].rearrange("p (h d) -> p h d", h=2)
                    if lat:
                        rope(k16, kview, ROPEH, t, 2, 32, 'pbuf', 'pb16')
                    else:
                        CP('dve', k16, kview, ['pbuf'], ['pb16'])
                    CP('pool', VA[:, t, 0:2, 0:64], PBUF[:, 128:256].rearrange("p (h d) -> p h d", h=2), ['pbuf'], ['va'])
                    transposes_to(lambda b: KT[0:64, b, t * 128:(t + 1) * 128], PB16, 2, 64, 'pb16', 'kt')
                else:
                    cview = PBUF[:, 0:128].rearrange("p (h d) -> p h d", h=1)
                    head_rms(cview, 1, 128, MKVN, 'pbuf')
                    CP('dve', PB16[:, 0:128], PBUF[:, 0:128], ['pbuf'], ['pb16'])
                    transposes_to(lambda b: DTT[:, 0, :], PB16, 1, 128, 'pb16', 'dtt')
                    ck(14)
                    MM(ps[2][:, 0:512], DTT[:, 0, :], WKVB, True, True, ['dtt', 'wkvb'], [('ps', 2)])
                    ck(15)
                    kf = PB16[:, 0:512].rearrange("p (h d) -> p h d", h=4)
                    P.op('dve', lambda e: e.memset(PB16[:, 0:512], 0.0), [], ['pb16'])
                    CP('act', PBUF[:, 512:1024], ps[2], [('ps', 2)], ['pbuf2'])
                    dkv = PBUF[:, 512:1024].rearrange("p (h d) -> p h d", h=4)
                    CP('pool', kf[:, :, 0:64], dkv[:, :, 0:64], ['pbuf2'], ['pb16'])
                    CP('pool', VA[:, t, :, 0:64], dkv[:, :, 64:128], ['pbuf2'], ['va'])
                    ck(16)
                    pe_src = PBUF[:, 128:160].rearrange("p (h d) -> p h d", h=1)
                    pe_dst = PBUF[:, 160:192].rearrange("p (h d) -> p h d", h=1)
                    if lat:
                        rope(pe_dst, pe_src, ROPER, t, 1, 16, 'pbuf', 'pbuf')
                    else:
                        CP('dve', pe_dst, pe_src, ['pbuf'], ['pbuf'])
                    ck(17)
                    CP('dve', kf[:, :, 64:96], pe_dst.to_broadcast([128, 4, 32]), ['pbuf'], ['pb16'])
                    ck(18)
                    transposes_to(lambda b: KT[:, b, t * 128:(t + 1) * 128], PB16, 4, 128, 'pb16', 'kt')
            if m == 0:
                ck(4)
            if m == 3:
                ck(10)
            qoff = [OFF['aq'], OFF['bq'], OFF['cq'], OFF['dqa']][m]
            load_w(w_in[l][:, qoff:qoff + 256], 8, 256, WQ, 'wq')
            if m == 3:
                load_w(mla_wqb[l], 2, 384, WQB, 'wqb')
            load_w(w_in[l][:, OFF['gate'] + m * D:OFF['gate'] + (m + 1) * D], 8, D, WG, 'wg')
            load_w(w_branch[l, m], 2, D, None, None, cast=False,
                   use=lambda sv, c0, cw, skey: TS('dve', WBR[:, :, c0:c0 + cw], sv, 0.5, None, ALU.mult, None, [skey], ['wbr']))
            def q_prep(t, QTv, qkey):
                lat = t >= 2
                proj(0, t, WQ, 256, 'wq')
                qscale = 0.125 if m != 3 else float(96 ** -0.5)
                if m == 3:
                    evac_scaled(PBUF[:, 0:256], ps[0][:, 0:256], ('ps', 0), 'pbuf', 1.0)
                    qv = PBUF[:, 0:256].rearrange("p (h d) -> p h d", h=1)
                    head_rms(qv, 1, 256, MQN, 'pbuf')
                    CP('dve', PB16[:, 0:256], PBUF[:, 0:256], ['pbuf'], ['pb16'])
                    yield
                    transposes_to(lambda b: DTT[:, b, :], PB16, 2, 128, 'pb16', 'dtt', half=True)
                    for k in range(2):
                        MM(ps[5][:, 0:384], DTT[:, k, :], WQB[:, k, :], k == 0, k == 1, ['dtt', 'wqb'], [('ps', 5)])
                    evac_scaled(PBUF[:, 0:384], ps[5][:, 0:384], ('ps', 5), 'pbuf', qscale)
                    q4 = PBUF[:, 0:384].rearrange("p (h d) -> p h d", h=4)
                    q16 = PB16[:, 0:512].rearrange("p (h d) -> p h d", h=4)
                    P.op('dve', lambda e: e.memset(PB16[:, 0:512], 0.0), [], ['pb16'])
                    CP('pool', q16[:, :, 0:64], q4[:, :, 0:64], ['pbuf'], ['pb16'])
                    if lat:
                        rope(q16[:, :, 64:96], q4[:, :, 64:96], ROPER, t, 4, 16, 'pbuf', 'pb16')
                    else:
                        CP('dve', q16[:, :, 64:96], q4[:, :, 64:96], ['pbuf'], ['pb16'])
                    yield
                    transposes_to(lambda b: QTv[:, b, :], PB16, 4, 128, 'pb16', qkey, half=True)
                else:
                    evac_scaled(PBUF[:, 0:256], ps[0][:, 0:256], ('ps', 0), 'pbuf', qscale)
                    q4 = PBUF[:, 0:256].rearrange("p (h d) -> p h d", h=4)
                    q16 = PB16[:, 0:256].rearrange("p (h d) -> p h d", h=4)
                    if m == 1:
                        head_rms(q4, 4, 64, GQN, 'pbuf')
                        P.op('dve', lambda e: e.tensor_scalar(PBUF[:, 0:256], PBUF[:, 0:256], 0.125, None, ALU.mult),
                             ['pbuf'], ['pbuf'])
                    if lat and m in (1, 2):
                        rope(q16, q4, ROPEH, t, 4, 32, 'pbuf', 'pb16')
                    else:
                        CP('dve', q16, q4, ['pbuf'], ['pb16'])
                    yield
                    transposes_to(lambda b: QTv[0:64, b, :], PB16, 4, 64, 'pb16', qkey, half=True)

            def q_body(t, QTv, qkey, nxt):
                lat = t >= 2
                kts = kv_tiles(m, t, lastl)
                o_i = 4 if (att_i[0] % 2 == 0) else 7
                att_i[0] += 1
                OPS = ps[o_i][:, 0:260].rearrange("p (h d) -> p h d", h=4)
                items = []
                for h in range(4):
                    for g0 in range(0, len(kts), 4):
                        items.append((h, g0, kts[g0:g0 + 4]))

                def emit_qk(ix):
                    h, g0, grp = items[ix]
                    kvh = h if nkv == 4 else h // 2
                    sb_i = 1 + (ix % 3)
                    Sv = ps[sb_i].rearrange("p (g q) -> p g q", g=4)
                    for gi_, (kt, bspec) in enumerate(grp):
                        MM(Sv[:, gi_, :], KT[0:hd, kvh, kt * 128:(kt + 1) * 128], QTv[0:hd, h, :], True, bspec is None,
                           ['kt', qkey], [('ps', sb_i)])
                        if bspec is not None:
                            brhs = BIAS[:, bspec[1] * 4 + h, :] if bspec[0] == 'A' else MASKC[:, bspec[1], :]
                            MM(Sv[:, gi_, :], IDB, brhs, False, True, ['idb', 'bias', 'maskc'], [('ps', sb_i)])

                def emit_exp_pv(ix):
                    h, g0, grp = items[ix]
                    kvh = h if nkv == 4 else h // 2
                    sb_i = 1 + (ix % 3)
                    pt_i = ix % 3
                    Sv = ps[sb_i].rearrange("p (g q) -> p g q", g=4)
                    ng = len(grp)
                    ACT(PT[:, pt_i, 0:ng, :], Sv[:, 0:ng, :], AF.Exp, [('ps', sb_i)], [('pt', pt_i)])
                    for gi_, (kt, bspec) in enumerate(grp):
                        first = (g0 == 0 and gi_ == 0)
                        last = (g0 + gi_ == len(kts) - 1)
                        MM(OPS[:, h, :], PT[:, pt_i, gi_, :], VA[:, kt, kvh, :], first, last,
                           [('pt', pt_i), 'va'], [('ps', o_i)])

                step_pts = set([0, len(items) // 3, (2 * len(items)) // 3])
                emit_qk(0)
                if len(items) > 1:
                    emit_qk(1)
                for ix in range(len(items)):
                    if nxt is not None and ix in step_pts:
                        next(nxt, None)
                    if ix + 2 < len(items):
                        emit_qk(ix + 2)
                    emit_exp_pv(ix)
                if nxt is not None:
                    for _ in nxt:
                        pass
                den = RS[:, 32:36]
                if m == 2:
                    TT('dve', den, OPS[:, :, 64], SKE, ALU.add, [('ps', o_i), 'ske'], ['rs2'])
                else:
                    CP('dve', den, OPS[:, :, 64], [('ps', o_i)], ['rs2'])
                RCP(RS[:, 36:40], den, ['rs2'], ['rs2'])
                TT('dve', OB.rearrange("p (h d) -> p h d", h=4), OPS[:, :, 0:64],
                   RS[:, 36:40].rearrange("p (h o) -> p h o", o=1).to_broadcast([128, 4, 64]), ALU.mult,
                   [('ps', o_i), 'rs2'], ['ob'])
                transposes_to(lambda b: OT[:, b, :], OB, 2, 128, 'ob', 'ot', half=True)
                for hf in range(2):
                    cs = slice(hf * 512, (hf + 1) * 512)
                    for k in range(8):
                        MM(ps[5], HT[:, k, t * 128:(t + 1) * 128], WG[:, k, cs], k == 0, k == 7,
                           [('ht', t), 'wg'], [('ps', 5)])
                    for k in range(2):
                        MM(ps[6], OT[:, k, :], WBR[:, k, cs], k == 0, k == 1, ['ot', 'wbr'], [('ps', 6)])
                    ACT(SIG[:, cs], ps[5], AF.Tanh, [('ps', 5)], ['sig'], scale=0.5)
                    STT(MTL[:, cs], SIG[:, cs], 1.0, ps[6], ALU.add, ALU.mult, [('ps', 6), 'sig'], ['mtl'])
                DMA(md[m, t * 128:(t + 1) * 128, :], MTL, ['mtl'], [('md', m, t)])
                if m == 0 and t == 0:
                    ck(5)
                if m == 3 and t == 0:
                    ck(11)
                if m == 3 and t == 2:
                    ck(12)
            g0_ = q_prep(q_tiles[0], QT[:, 0], ('qt', 0))
            for _ in g0_:
                pass
            for idx_, t_ in enumerate(q_tiles):
                nx_ = None
                if idx_ + 1 < len(q_tiles):
                    nx_ = q_prep(q_tiles[idx_ + 1], QT[:, (idx_ + 1) % 2], ('qt', (idx_ + 1) % 2))
                q_body(t_, QT[:, idx_ % 2], ('qt', idx_ % 2), nx_)
            ck(6 + m)
        P.barrier()

        mod_tiles(l, 2 * D, 'gate', 0, which=(0,) if lastl else (0, 1))
        load_w(w_out[l], 8, D, WO, 'wo')
        for t in q_tiles:
            w = 0 if t >= 2 else 1
            DMA(M4, md[:, t * 128:(t + 1) * 128, :].rearrange("m p d -> p m d"),
                [('md', mm_, t) for mm_ in range(4)], ['m4'])
            DMA(XT, x_src(l)[t * 128:(t + 1) * 128, :], [], ['xt'])
            TT('dve', M4[:, 0, :], M4[:, 0, :], M4[:, 1, :], ALU.add, ['m4'], ['m4'])
            TT('pool', M4[:, 2, :], M4[:, 2, :], M4[:, 3, :], ALU.add, ['m4'], ['m4b'])
            TT('dve', M4[:, 0, :], M4[:, 0, :], M4[:, 2, :], ALU.add, ['m4', 'm4b'], ['m4'])
            pb = ps[1].bitcast(BF16)
            for k in range(8):
                TR(pb[:, k * 128:(k + 1) * 128], M4[:, 0, k * 128:(k + 1) * 128], IDB, ['m4', 'idb'], [('ps', 1)])
            CP('act', MT8, pb.rearrange("p (k q) -> p k q", k=8), [('ps', 1)], ['mt8'])
            for hf in range(2):
                cs = slice(hf * 512, (hf + 1) * 512)
                for k in range(8):
                    MM(ps[5 + hf], MT8[:, k, :], WO[:, k, cs], k == 0, k == 7, ['mt8', 'wo'], [('ps', 5 + hf)])
                TT('dve', HB[:, cs], ps[5 + hf], MOD[:, w, cs], ALU.mult, [('ps', 5 + hf), ('mod', w)], ['hb'])
                TT('pool', XS[:, t, cs], HB[:, cs], XT[:, cs], ALU.add, ['hb', 'xt'], [('x', t)])
        P.barrier()
        if dbg == ('xm', l):
            for t in q_tiles:
                DMA(dbg_out[t * 128:(t + 1) * 128, :], XS[:, t, :], [('x', t)], ['dbgo'])
            return True

        moe = (l % 2 == 1)
        mod_tiles(l, 4 * D, 'scale', 0, norm2_g[l:l + 1, :], which=(0,) if lastl else (0, 1))
        mod_tiles(l, 3 * D, 'shift', 2, which=(0,) if lastl else (0, 1))
        if moe:
            DMA(RT, moe_router[l // 2].rearrange("(k p) e -> p k e", p=128), [], ['rt'])
            P.op('dve', lambda e: e.memset(RTB, 0.0), [], ['rtb'])
            CP('dve', RTB[:, :, 0:8], RT, ['rt', 'rtb'], ['rtb'])
        for t in q_tiles:
            norm_tile(t, XS[:, t, :], [('x', t)], 0, 2, router=False)
            if moe:
                lg = ps[7][:, 0:NEXP]
                for k in range(8):
                    MM(ps[7][:, 0:16], HT[:, k, t * 128:(t + 1) * 128], RTB[:, k, :], k == 0, k == 7, [('ht', t), 'rtb'], [('ps', 7)])
                L = RS[:, 0:8]
                CP('dve', L, lg, [('ps', 7)], ['rs'])
                RED(RS[:, 8:9], L, ALU.max, ['rs'], ['rs'])
                TT('dve', RS[:, 16:24], L, RS[:, 8:9].to_broadcast([128, 8]), ALU.is_equal, ['rs'], ['rs'])
                STT(RS[:, 24:32], RS[:, 16:24], -1e30, L, ALU.mult, ALU.add, ['rs'], ['rs'])
                RED(RS[:, 9:10], RS[:, 24:32], ALU.max, ['rs'], ['rs'])
                TT('dve', RS[:, 40:48], RS[:, 24:32], RS[:, 9:10].to_broadcast([128, 8]), ALU.is_equal, ['rs'], ['rs'])
                TT('dve', RS[:, 10:11], RS[:, 9:10], RS[:, 8:9], ALU.subtract, ['rs'], ['rs'])
                ACT(RS[:, 11:12], RS[:, 10:11], AF.Exp, ['rs'], ['rs'])
                TS('dve', RS[:, 12:13], RS[:, 11:12], 1.0, None, ALU.add, None, ['rs'], ['rs'])
                RCP(RS[:, 13:14], RS[:, 12:13], ['rs'], ['rs'])
                TT('dve', RS[:, 14:15], RS[:, 11:12], RS[:, 13:14], ALU.mult, ['rs'], ['rs'])
                TT('dve', RS[:, 48:56], RS[:, 16:24], RS[:, 13:14].to_broadcast([128, 8]), ALU.mult, ['rs'], ['rs'])
                STT(COMB[:, t, :], RS[:, 40:48], RS[:, 14:15], RS[:, 48:56], ALU.mult, ALU.add, ['rs'], ['comb'])
                if t == 0:
                    ck(20)
        mod_tiles(l, 5 * D, 'gate', 0, which=(0,) if lastl else (0, 1))

        chunks = []
        if not lastl:
            chunks.append([0, 1])
        for c in range(4):
            chunks.append([2 + 4 * c + i for i in range(4)])
        nexp = NEXP if moe else 1

        def wset(s_):
            o = s_ * 6144
            return (R4[:, o:o + 2048].rearrange("p (k c) -> p k c", k=8),
                    R4[:, o + 2048:o + 4096].rearrange("p (k c) -> p k c", k=8),
                    R4[:, o + 4096:o + 6144].rearrange("p (k c) -> p k c", k=2),
                    MOD[:, 2 + s_, :].bitcast(BF16).rearrange("p (k c) -> p k c", k=2))

        def g2b(w):
            return MOD[:, w, :].rearrange("p (o d) -> p o d", o=1).to_broadcast([128, 2, D])

        groups = [(e_, g) for e_ in range(nexp) for g in range(DFF // 256)]

        def wsrc(e_):
            if moe:
                return moe_w1[l // 2, e_], moe_w3[l // 2, e_], moe_w2[l // 2, e_]
            return ffn_w1[l // 2], ffn_w3[l // 2], ffn_w2[l // 2]

        def loads(gix):
            e_, g = groups[gix]
            s_ = gix % 2
            w1, w3, w2 = wsrc(e_)
            W1s, W3s, W2s, W2cs = wset(s_)
            load_w(w1[:, g * 256:(g + 1) * 256], 8, 256, None, None, cast=False,
                   use=lambda sv, c0, cw, skey: CP('act', W1s, sv, [skey], [('w1b', s_)]))
            load_w(w3[:, g * 256:(g + 1) * 256], 8, 256, None, None, cast=False,
                   use=lambda sv, c0, cw, skey: CP('act', W3s, sv, [skey], [('w3b', s_)]))

            def use_w2(sv, c0, cw, skey):
                TT('pool', W2s, sv, g2b(0), ALU.mult, [skey, ('mod', 0)], [('w2b', s_)])
                if not lastl:
                    TT('pool', W2cs, sv, g2b(1), ALU.mult, [skey, ('mod', 1)], [('mod', 2 + s_)])
            load_w(w2[g * 256:(g + 1) * 256, :], 2, D, None, None, cast=False, use=use_w2)

        steps = [(gix, cix) for gix in range(len(groups)) for cix in range(len(chunks))]

        def ab_gen(six):
            gix, cix = steps[six]
            s_ = gix % 2
            ch = chunks[cix]
            n = len(ch) * 128
            t0 = ch[0] * 128
            up = (six % 2) * 2
            W1s, W3s, _w2, _w2c = wset(s_)
            for fc in range(2):
                for k in range(8):
                    MM(ps[fc][:, 0:n], W1s[:, k, fc * 128:(fc + 1) * 128], HT[:, k, t0:t0 + n], k == 0, k == 7,
                       [('w1b', s_)] + [('ht', t) for t in ch], [('ps', fc)])
                yield
                for k in range(8):
                    MM(ps[2 + fc][:, 0:n], W3s[:, k, fc * 128:(fc + 1) * 128], HT[:, k, t0:t0 + n], k == 0, k == 7,
                       [('w3b', s_)] + [('ht', t) for t in ch], [('ps', 2 + fc)])
                ACT(SA[:, fc, 0:n], ps[fc][:, 0:n], AF.Silu, [('ps', fc)], [('sa', fc)])
                TT('dve', UT[:, up + fc, 0:n], ps[2 + fc][:, 0:n], SA[:, fc, 0:n], ALU.mult,
                   [('ps', 2 + fc), ('sa', fc)], [('ut', up + fc)])
                yield

        def w2_gen(six):
            gix, cix = steps[six]
            e_ = groups[gix][0]
            s_ = gix % 2
            ch = chunks[cix]
            up = (six % 2) * 2
            _w1, _w3, W2s, W2cs = wset(s_)
            for ti, t in enumerate(ch):
                ob_ = 4 + 2 * (ti % 2)
                wsel, wkey = (W2s, ('w2b', s_)) if t >= 2 else (W2cs, ('mod', 2 + s_))
                for hf in range(2):
                    cs = slice(hf * 512, (hf + 1) * 512)
                    for fc in range(2):
                        MM(ps[ob_ + hf], UT[:, up + fc, ti * 128:(ti + 1) * 128], wsel[:, fc, cs], fc == 0, fc == 1,
                           [('ut', up + fc), wkey], [('ps', ob_ + hf)])
                    if moe:
                        STT(XS[:, t, cs], ps[ob_ + hf], COMB[:, t, e_:e_ + 1], XS[:, t, cs], ALU.mult, ALU.add,
                            [('ps', ob_ + hf), 'comb', ('x', t)], [('x', t)])
                    else:
                        TT('dve', XS[:, t, cs], ps[ob_ + hf], XS[:, t, cs], ALU.add,
                           [('ps', ob_ + hf), ('x', t)], [('x', t)])
                yield

        loads(0)
        if len(groups) > 1:
            loads(1)
        for _ in ab_gen(0):
            pass
        for six in range(len(steps)):
            ga = ab_gen(six + 1) if six + 1 < len(steps) else None
            gw = w2_gen(six)
            while ga is not None or gw is not None:
                if ga is not None:
                    try:
                        next(ga)
                    except StopIteration:
                        ga = None
                if gw is not None:
                    try:
                        next(gw)
                    except StopIteration:
                        gw = None
            gix, cix = steps[six]
            if cix == len(chunks) - 1 and gix + 2 < len(groups):
                loads(gix + 2)
        P.barrier()
        if dbg == ('xf', l):
            for t in q_tiles:
                DMA(dbg_out[t * 128:(t + 1) * 128, :], XS[:, t, :], [('x', t)], ['dbgo'])
            return True
        if not lastl:
            for t in range(NT):
                DMA(xd[t * 128:(t + 1) * 128, :], XS[:, t, :], [('x', t)], [('xd', t)])
            P.barrier()
        return False

    stopped = False
    try:
        for l in range(n_layers):
            stopped = layer(l)
            if stopped:
                break
    except _Stop:
        stopped = True
    if not stopped and n_layers == DEPTH:
        DMA(MOD[:, 0, :], final_g.partition_broadcast(128), [], [('mod', 0)])
        for t in range(2, NT):
            src = XS[:, t, :]
            ACT(HB, src, AF.Square, [('x', t)], ['hb'], accum_out=STAT[:, 0:1])
            ACT(STAT[:, 1:2], STAT[:, 0:1], AF.Sqrt, ['hb'], ['stat'], bias=EPSB, scale=1.0 / D)
            RCP(STAT[:, 2:3], STAT[:, 1:2], ['stat'], ['stat'])
            STT(HB, src, STAT[:, 2:3], MOD[:, 0, :], ALU.mult, ALU.mult, [('x', t), 'stat', ('mod', 0)], ['hb'])
            DMA(out[(t - 2) * 128:(t - 1) * 128, :], HB, ['hb'], ['out'])
    elif not stopped:
        DMA(out[0:128, :], HB, [], ['out'])
    P.barrier()
    P.emit()
    print("instructions:", P.n, {k: len(v) for k, v in P.streams.items()})


_CACHE = {}


def make_in_maps(inp, n_cores=8):
    f = lambda a: np.ascontiguousarray(np.asarray(a), dtype=np.float32)
    cosH, sinH, cosR, sinR = _rope_tables()
    shared = {
        "norm1_g": f(inp["norm1_g"]), "norm2_g": f(inp["norm2_g"]), "w_ada": f(inp["w_ada"]),
        "b_ada": f(inp["b_ada"]), "w_in": f(inp["w_in"]), "biasA": _natten_bias(f(inp["na_rpb"])),
        "smallp": np.ascontiguousarray(np.concatenate(
            [f(inp["gb_qnorm"]), f(inp["gb_knorm"]), f(inp["mla_qnorm"]), f(inp["mla_kvnorm"]), f(inp["wc_sink"])],
            axis=1)),
        "mla_wqb": f(inp["mla_wqb"]), "mla_wkvb": f(inp["mla_wkvb"]), "w_branch": f(inp["w_branch"]),
        "w_out": f(inp["w_out"]), "ffn_w1": f(inp["ffn_w1"]), "ffn_w3": f(inp["ffn_w3"]), "ffn_w2": f(inp["ffn_w2"]),
        "moe_router": f(inp["moe_router"]), "moe_w1": f(inp["moe_w1"]), "moe_w3": f(inp["moe_w3"]),
        "moe_w2": f(inp["moe_w2"]), "final_g": f(inp["final_g"]).reshape(1, D),
        "c_ident": np.eye(128, dtype=np.float32),
        "c_ropeH": np.ascontiguousarray(np.stack([cosH, sinH])),
        "c_ropeR": np.ascontiguousarray(np.stack([cosR, sinR])),
        "c_maskC": _maskC(),
    }
    x = f(inp["x"]); ctx = f(inp["ctx"]); c = f(inp["c"]); cc = f(inp["c_ctx"])
    maps = []
    for b in range(n_cores):
        m = dict(shared)
        m["xin"] = np.ascontiguousarray(np.concatenate([ctx[b], x[b]], axis=0))
        m["cvec"] = np.ascontiguousarray(np.stack([c[b], cc]))
        maps.append(m)
    return maps


def kernel(**inputs):
    if "nc" not in _CACHE:
        _CACHE["nc"] = build(DEPTH)
    nc = _CACHE["nc"]
    maps = make_in_maps(inputs, 8)
    res = run_bass_kernel_spmd(nc, maps, core_ids=list(range(8)))
    return np.stack([np.asarray(r["out"], dtype=np.float32) for r in res.results], axis=0)
```
